# Optimizing a Trainium2 kernel written in Bass

```python
import math
import jax, jax.numpy as jnp
from jax import lax
import numpy as np

D_MODEL = 4096
BATCH = 8
SEQ = 2048
DEPTH = 2

CHUNK = 64
MEM_LEN = 256
EPS = 1e-6
ROPE_THETA = 10000.0
FFN_DIM = 8192

POOL_WINDOWS = (2, 4, 8, 16)
POOL_GROUPS = 4
POOL_WIDTH = 2048
POOL_GW = POOL_WIDTH // POOL_GROUPS
SG_WIDTH = 1024
SG_BLOCK = 128
SG_GROUPS = 4
SG_GW = SG_WIDTH // SG_GROUPS
SSM_HEADS = 16
SSM_HEADDIM = 64
SSM_INNER = SSM_HEADS * SSM_HEADDIM
SSM_GROUPS = 4
SSM_STATE = 128
SSM_CONV = 4
SSM_CONV_CH = SSM_INNER + 2 * SSM_GROUPS * SSM_STATE
SSM_CHUNK = CHUNK
ATT_HEADS = 8
ATT_KV_HEADS = 2
ATT_HEADDIM = 128
IDX_HEADS = 8
IDX_HEADDIM = 64
IDX_TOPK = 256
Q_BLOCK = 128
MEM_HEADS = 4
MEM_HEADDIM = 128

IN_SPLITS = (POOL_WIDTH, SG_WIDTH, SG_WIDTH, SSM_INNER, SSM_CONV_CH, SSM_HEADS,
             ATT_HEADS * ATT_HEADDIM, ATT_KV_HEADS * ATT_HEADDIM, ATT_KV_HEADS * ATT_HEADDIM,
             IDX_HEADS * IDX_HEADDIM, IDX_HEADDIM, IDX_HEADS)
IN_WIDTH = 9304
BRANCH_WIDTHS = (POOL_WIDTH, SG_WIDTH, SSM_INNER, ATT_HEADS * ATT_HEADDIM)
BRANCH_TOTAL = 5120
N_BRANCH = 4

kernel_name = 'hybrid_streaming_gated_block'


def rmsnorm(x, g):
    x32 = x.astype(jnp.float32)
    y = x32 * lax.rsqrt(jnp.mean(x32 * x32, axis=-1, keepdims=True) + EPS)
    return (y * g.astype(jnp.float32)).astype(x.dtype)


def layernorm(x, g, b):
    x32 = x.astype(jnp.float32)
    mu = jnp.mean(x32, axis=-1, keepdims=True)
    xc = x32 - mu
    y = xc * lax.rsqrt(jnp.mean(xc * xc, axis=-1, keepdims=True) + EPS)
    return (y * g.astype(jnp.float32) + b.astype(jnp.float32)).astype(x.dtype)


def rope(x, pos):
    half = x.shape[-1] // 2
    inv = ROPE_THETA ** (-jnp.arange(half, dtype=jnp.float32) / half)
    ang = pos.astype(jnp.float32)[:, None] * inv[None, :]
    shape = (1, pos.shape[0]) + (1,) * (x.ndim - 3) + (half,)
    cos = jnp.cos(ang).reshape(shape)
    sin = jnp.sin(ang).reshape(shape)
    xf = x.astype(jnp.float32)
    x1, x2 = xf[..., :half], xf[..., half:]
    return jnp.concatenate([x1 * cos - x2 * sin, x2 * cos + x1 * sin], axis=-1).astype(x.dtype)


def swiglu_ffn(h, w_in, w_out):
    gate, up = jnp.split(h @ w_in, 2, axis=-1)
    return (jax.nn.silu(gate) * up) @ w_out


def pool_mixer(a, pool_w, pool_scale):
    bsz, L, _ = a.shape
    a4 = a.reshape(bsz, L, POOL_GROUPS, POOL_GW)
    cs = jnp.cumsum(a4.astype(jnp.float32), axis=1)
    cs = jnp.pad(cs, ((0, 0), (1, 0), (0, 0), (0, 0)))
    t1 = jnp.arange(1, L + 1)[:, None]
    lo = jnp.maximum(t1 - jnp.array(POOL_WINDOWS)[None, :], 0)
    cs_lo = cs[:, lo, jnp.arange(POOL_GROUPS)[None, :], :]
    pooled = (cs[:, 1:] - cs_lo) / (t1 - lo).astype(jnp.float32)[None, :, :, None]
    mixed = (pooled - a4.astype(jnp.float32)).astype(a.dtype)
    out = jnp.einsum('blgc,gcd->blgd', mixed, pool_w) * pool_scale
    return out.reshape(bsz, L, POOL_WIDTH)


def spatial_gating_mixer(u, v, ln_g, ln_b, w_s, b_s):
    u = jax.nn.gelu(u, approximate=False)
    v = layernorm(jax.nn.gelu(v, approximate=False), ln_g, ln_b)
    bsz, L, _ = v.shape
    nb = L // SG_BLOCK
    cid = jnp.arange(SG_BLOCK) // CHUNK
    mask = cid[:, None] >= cid[None, :]
    w = jnp.where(mask[None], w_s, 0.0)
    vb = v.reshape(bsz, nb, SG_BLOCK, SG_GROUPS, SG_GW)
    sv = jnp.einsum('gij,bnjgc->bnigc', w, vb) + b_s.T[None, None, :, :, None]
    return u * sv.reshape(bsz, L, SG_WIDTH)


def ssd_scan(xs, dt, A, Bm, Cm):
    bsz, L, H, P = xs.shape
    G, N = Bm.shape[2], Bm.shape[3]
    Hg = H // G
    nc = L // SSM_CHUNK
    xd = (xs * dt[..., None]).reshape(bsz, nc, SSM_CHUNK, G, Hg, P)
    a_cs = jnp.cumsum((dt * A).reshape(bsz, nc, SSM_CHUNK, G, Hg), axis=2)
    Bc = Bm.reshape(bsz, nc, SSM_CHUNK, G, N)
    Cc = Cm.reshape(bsz, nc, SSM_CHUNK, G, N)
    tri = jnp.tril(jnp.ones((SSM_CHUNK, SSM_CHUNK), dtype=bool))
    seg = a_cs[:, :, :, None] - a_cs[:, :, None, :]
    decay = jnp.exp(jnp.where(tri[None, None, :, :, None, None], seg, -jnp.inf))
    cb = jnp.einsum('bclgn,bcsgn->bclsg', Cc, Bc)
    y_diag = jnp.einsum('bclsg,bclsgh,bcsghp->bclghp', cb, decay, xd)
    to_end = jnp.exp(a_cs[:, :, -1:] - a_cs)
    chunk_states = jnp.einsum('bclgn,bclgh,bclghp->bcghpn', Bc, to_end, xd)
    chunk_decay = jnp.exp(a_cs[:, :, -1])

    def step(state, inp):
        dec, new = inp
        return state * dec[..., None, None] + new, state

    init = jnp.zeros((bsz, G, Hg, P, N), xs.dtype)
    _, prev = lax.scan(step, init, (jnp.moveaxis(chunk_decay, 1, 0), jnp.moveaxis(chunk_states, 1, 0)))
    prev = jnp.moveaxis(prev, 0, 1)
    y_off = jnp.einsum('bclgn,bcghpn,bclgh->bclghp', Cc, prev, jnp.exp(a_cs))
    return (y_diag + y_off).reshape(bsz, L, H, P)


def mamba2_mixer(z, xbc, dt_raw, conv_w, conv_b, a_log, dt_bias, d_skip, norm_g):
    bsz, L, _ = xbc.shape
    xbc = lax.conv_general_dilated(xbc, conv_w[:, None, :], window_strides=(1,),
                                   padding=[(SSM_CONV - 1, 0)],
                                   dimension_numbers=('NWC', 'WIO', 'NWC'),
                                   feature_group_count=SSM_CONV_CH) + conv_b
    xbc = jax.nn.silu(xbc)
    xs, Bm, Cm = jnp.split(xbc, [SSM_INNER, SSM_INNER + SSM_GROUPS * SSM_STATE], axis=-1)
    xs = xs.reshape(bsz, L, SSM_HEADS, SSM_HEADDIM).astype(jnp.float32)
    Bm = Bm.reshape(bsz, L, SSM_GROUPS, SSM_STATE).astype(jnp.float32)
    Cm = Cm.reshape(bsz, L, SSM_GROUPS, SSM_STATE).astype(jnp.float32)
    dt = jax.nn.softplus(dt_raw.astype(jnp.float32) + dt_bias.astype(jnp.float32))
    A = -jnp.exp(a_log.astype(jnp.float32))
    y = ssd_scan(xs, dt, A, Bm, Cm) + xs * d_skip.astype(jnp.float32)[:, None]
    y = y.reshape(bsz, L, SSM_INNER).astype(z.dtype) * jax.nn.silu(z)
    y = rmsnorm(y.reshape(bsz, L, SSM_GROUPS, SSM_INNER // SSM_GROUPS),
                norm_g.reshape(SSM_GROUPS, SSM_INNER // SSM_GROUPS))
    return y.reshape(bsz, L, SSM_INNER)


def dsa_mixer(q, k, v, q_idx, k_idx, w_idx):
    bsz, L, _ = q.shape
    pos = jnp.arange(L)
    cid = pos // CHUNK
    grp = ATT_HEADS // ATT_KV_HEADS
    q = rope(q.reshape(bsz, L, ATT_HEADS, ATT_HEADDIM), pos).reshape(bsz, L, ATT_KV_HEADS, grp, ATT_HEADDIM)
    k = rope(k.reshape(bsz, L, ATT_KV_HEADS, ATT_HEADDIM), pos)
    v = v.reshape(bsz, L, ATT_KV_HEADS, ATT_HEADDIM)
    q_idx = rope(q_idx.reshape(bsz, L, IDX_HEADS, IDX_HEADDIM), pos)
    k_idx = rope(k_idx, pos)
    w_idx = w_idx * (IDX_HEADS ** -0.5)
    topk = min(IDX_TOPK, L // 4)
    scale = ATT_HEADDIM ** -0.5

    def block(i):
        t0 = i * Q_BLOCK
        qb = lax.dynamic_slice_in_dim(q, t0, Q_BLOCK, axis=1)
        qib = lax.dynamic_slice_in_dim(q_idx, t0, Q_BLOCK, axis=1)
        wib = lax.dynamic_slice_in_dim(w_idx, t0, Q_BLOCK, axis=1)
        cq = (t0 + jnp.arange(Q_BLOCK)) // CHUNK
        logits = jax.nn.relu(jnp.einsum('bqhd,bsd->bqhs', qib, k_idx))
        iscore = jnp.einsum('bqhs,bqh->bqs', logits, wib).astype(jnp.float32)
        adm = cid[None, None, :] <= cq[None, :, None]
        iscore = jnp.where(adm, iscore, -jnp.inf)
        _, idx = lax.top_k(iscore, topk)
        kg = jax.vmap(lambda kk, ii: kk[ii])(k, idx)
        vg = jax.vmap(lambda vv, ii: vv[ii])(v, idx)
        valid = cid[idx] <= cq[None, :, None]
        s = jnp.einsum('bqhgd,bqkhd->bqhgk', qb, kg).astype(jnp.float32) * scale
        s = jnp.where(valid[:, :, None, None, :], s, -jnp.inf)
        p = jax.nn.softmax(s, axis=-1).astype(v.dtype)
        o = jnp.einsum('bqhgk,bqkhd->bqhgd', p, vg)
        return o.reshape(bsz, Q_BLOCK, ATT_HEADS * ATT_HEADDIM)

    out = lax.map(block, jnp.arange(L // Q_BLOCK))
    return jnp.transpose(out, (1, 0, 2, 3)).reshape(bsz, L, ATT_HEADS * ATT_HEADDIM)


def gated_merge(h, branches, w_branch, w_gate):
    out = None
    row = 0
    for i, y in enumerate(branches):
        width = y.shape[-1]
        term = jax.nn.sigmoid(h @ w_gate[i]) * (y @ w_branch[row:row + width])
        row += width
        out = term if out is None else out + term
    return out


def memory_cross_attention(h, mem_n, w_q, w_kv, w_o):
    bsz, L, _ = h.shape
    q = (h @ w_q).reshape(bsz, L, MEM_HEADS, MEM_HEADDIM)
    kv = (mem_n @ w_kv).reshape(bsz, mem_n.shape[1], 2, MEM_HEADS, MEM_HEADDIM)
    k, v = kv[:, :, 0], kv[:, :, 1]
    s = jnp.einsum('blhd,bmhd->bhlm', q, k).astype(jnp.float32) * (MEM_HEADDIM ** -0.5)
    p = jax.nn.softmax(s, axis=-1).astype(h.dtype)
    o = jnp.einsum('bhlm,bmhd->blhd', p, v).reshape(bsz, L, MEM_HEADS * MEM_HEADDIM)
    return o @ w_o


def setup_inputs(seed: int = 0) -> dict:
    key = jax.random.key(seed)
    ks = jax.random.split(key, 32)
    f32 = jnp.float32
    D = D_MODEL

    def nrm(k, shape, scale):
        return scale * jax.random.normal(k, shape, f32)

    def gain(k, shape):
        return 1.0 + 0.02 * jax.random.normal(k, shape, f32)

    dt = jnp.exp(jax.random.uniform(ks[16], (DEPTH, SSM_HEADS), f32, math.log(1e-3), math.log(1e-1)))
    row_scale = jnp.concatenate([jnp.full((w,), w ** -0.5, f32) for w in BRANCH_WIDTHS])
    return {
        'x': nrm(ks[0], (BATCH, SEQ, D), 1.0),
        'mem': nrm(ks[1], (BATCH, MEM_LEN, D), 1.0),
        'g_ffn1': gain(ks[2], (DEPTH, D)),
        'w_ffn1_in': nrm(ks[3], (DEPTH, D, 2 * FFN_DIM), D ** -0.5),
        'w_ffn1_out': nrm(ks[4], (DEPTH, FFN_DIM, D), FFN_DIM ** -0.5),
        'g_mix': gain(ks[5], (DEPTH, D)),
        'w_in': nrm(ks[6], (DEPTH, D, IN_WIDTH), D ** -0.5),
        'pool_w': nrm(ks[7], (DEPTH, POOL_GROUPS, POOL_GW, POOL_GW), POOL_GW ** -0.5),
        'pool_scale': 1.0 + nrm(ks[8], (DEPTH, POOL_GROUPS, POOL_GW), 0.1),
        'sg_ln_g': gain(ks[9], (DEPTH, SG_WIDTH)),
        'sg_ln_b': nrm(ks[10], (DEPTH, SG_WIDTH), 0.02),
        'sg_w': nrm(ks[11], (DEPTH, SG_GROUPS, SG_BLOCK, SG_BLOCK), SG_BLOCK ** -0.5),
        'sg_b': 1.0 + nrm(ks[12], (DEPTH, SG_GROUPS, SG_BLOCK), 0.1),
        'ssm_conv_w': nrm(ks[13], (DEPTH, SSM_CONV, SSM_CONV_CH), SSM_CONV ** -0.5),
        'ssm_conv_b': nrm(ks[14], (DEPTH, SSM_CONV_CH), 0.02),
        'ssm_a_log': jnp.log(jax.random.uniform(ks[15], (DEPTH, SSM_HEADS), f32, 1.0, 16.0)),
        'ssm_dt_bias': dt + jnp.log(-jnp.expm1(-dt)),
        'ssm_d': 1.0 + nrm(ks[17], (DEPTH, SSM_HEADS), 0.1),
        'ssm_norm_g': gain(ks[18], (DEPTH, SSM_INNER)),
        'w_branch': jax.random.normal(ks[19], (DEPTH, BRANCH_TOTAL, D), f32) * row_scale[None, :, None],
        'w_gate': nrm(ks[20], (DEPTH, N_BRANCH, D, D), D ** -0.5),
        'w_out': nrm(ks[21], (DEPTH, D, D), D ** -0.5),
        'g_mem': gain(ks[22], (D,)),
        'g_cross': gain(ks[23], (DEPTH, D)),
        'w_mem_q': nrm(ks[24], (DEPTH, D, MEM_HEADS * MEM_HEADDIM), D ** -0.5),
        'w_mem_kv': nrm(ks[25], (DEPTH, D, 2 * MEM_HEADS * MEM_HEADDIM), D ** -0.5),
        'w_mem_o': nrm(ks[26], (DEPTH, MEM_HEADS * MEM_HEADDIM, D), (MEM_HEADS * MEM_HEADDIM) ** -0.5),
        'g_ffn2': gain(ks[27], (DEPTH, D)),
        'w_ffn2_in': nrm(ks[28], (DEPTH, D, 2 * FFN_DIM), D ** -0.5),
        'w_ffn2_out': nrm(ks[29], (DEPTH, FFN_DIM, D), FFN_DIM ** -0.5),
        'g_final': gain(ks[30], (D,)),
    }


def reference(x, mem, g_ffn1, w_ffn1_in, w_ffn1_out, g_mix, w_in, pool_w, pool_scale,
              sg_ln_g, sg_ln_b, sg_w, sg_b, ssm_conv_w, ssm_conv_b, ssm_a_log, ssm_dt_bias,
              ssm_d, ssm_norm_g, w_branch, w_gate, w_out, g_mem, g_cross, w_mem_q, w_mem_kv,
              w_mem_o, g_ffn2, w_ffn2_in, w_ffn2_out, g_final):
    mem_n = rmsnorm(mem, g_mem)
    split_points = [int(p) for p in np.cumsum(IN_SPLITS)[:-1]]
    for l in range(DEPTH):
        x = x + 0.5 * swiglu_ffn(rmsnorm(x, g_ffn1[l]), w_ffn1_in[l], w_ffn1_out[l])
        h = rmsnorm(x, g_mix[l])
        (c_pool, c_u, c_v, c_z, c_xbc, c_dt, c_q, c_k, c_val, c_qi, c_ki, c_wi) = jnp.split(
            h @ w_in[l], split_points, axis=-1)
        y_a = pool_mixer(c_pool, pool_w[l], pool_scale[l])
        y_b = spatial_gating_mixer(c_u, c_v, sg_ln_g[l], sg_ln_b[l], sg_w[l], sg_b[l])
        y_c = mamba2_mixer(c_z, c_xbc, c_dt, ssm_conv_w[l], ssm_conv_b[l], ssm_a_log[l],
                           ssm_dt_bias[l], ssm_d[l], ssm_norm_g[l])
        y_d = dsa_mixer(c_q, c_k, c_val, c_qi, c_ki, c_wi)
        x = x + gated_merge(h, (y_a, y_b, y_c, y_d), w_branch[l], w_gate[l]) @ w_out[l]
        x = x + memory_cross_attention(rmsnorm(x, g_cross[l]), mem_n, w_mem_q[l], w_mem_kv[l], w_mem_o[l])
        x = x + 0.5 * swiglu_ffn(rmsnorm(x, g_ffn2[l]), w_ffn2_in[l], w_ffn2_out[l])
    return rmsnorm(x, g_final)
```

```python
import math
import numpy as np
import concourse.bass as bass
import concourse.mybir as mybir
from concourse.bass_utils import run_bass_kernel_spmd

F32 = mybir.dt.float32
BF16 = mybir.dt.bfloat16
AF = mybir.ActivationFunctionType
ALU = mybir.AluOpType
AX = mybir.AxisListType

D = 4096
T = 2048
FF = 8192
DC = D // 128
EPS = 1e-6
DEPTH = 2
MEM = 256
NIN = 9304
POOL_WINDOWS = (2, 4, 8, 16)
NEG = -1.0e30


class Res:
    __slots__ = ("name", "writers", "readers")

    def __init__(self, name=""):
        self.name = name
        self.writers = {}
        self.readers = {}


class KB:
    def __init__(self, nc):
        self.nc = nc
        self.E = dict(pe=nc.tensor, act=nc.scalar, dve=nc.vector, pool=nc.gpsimd, sp=nc.sync)
        self.sems = {}
        self.cnt = {}
        for e in ("pe", "act", "dve", "pool"):
            k = "e_" + e
            self.sems[k] = nc.alloc_semaphore(k)
            self.cnt[k] = 0
        self.seen = {e: {} for e in self.E}
        self.dq = {}
        for q, n in (("sp", 10), ("pool", 8), ("act", 4)):
            keys = []
            for i in range(n):
                k = "d_%s%d" % (q, i)
                self.sems[k] = nc.alloc_semaphore(k)
                self.cnt[k] = 0
                keys.append(k)
            self.dq[q] = [keys, 0]
        self.nwaits = 0
        self.ninst = 0

    def wait(self, eng, key, val):
        if self.seen[eng].get(key, 0) >= val:
            return
        self.E[eng].wait_ge(self.sems[key], val)
        self.seen[eng][key] = val
        self.nwaits += 1

    def _deps(self, eng, reads, writes, pwrites, own):
        for r in reads:
            for k, v in r.writers.items():
                self.wait(eng, k, v)
        for w in writes:
            for k, v in w.writers.items():
                if k != own:
                    self.wait(eng, k, v)
            for k, v in w.readers.items():
                if k != own:
                    self.wait(eng, k, v)
        for w in pwrites:
            if w.readers:
                for k, v in w.readers.items():
                    if k != own:
                        self.wait(eng, k, v)
                w.readers = {}
                w.writers = {}

    def _commit(self, key, val, reads, writes, pwrites):
        for r in reads:
            if r.readers.get(key, 0) < val:
                r.readers[key] = val
        for w in writes:
            w.writers = {key: val}
            w.readers = {}
        for w in pwrites:
            if w.writers.get(key, 0) < val:
                w.writers[key] = val

    def op(self, eng, fn, reads=(), writes=(), pwrites=()):
        own = "e_" + eng
        self._deps(eng, reads, writes, pwrites, own)
        inst = fn()
        self.cnt[own] += 1
        inst.then_inc(self.sems[own], 1)
        self._commit(own, self.cnt[own], reads, writes, pwrites)
        self.ninst += 1
        return inst

    def dma(self, q, out, in_, reads=(), writes=(), pwrites=()):
        self._deps(q, reads, writes, pwrites, None)
        keys, rr = self.dq[q]
        key = keys[rr % len(keys)]
        self.dq[q][1] = rr + 1
        if self.cnt[key] > 0:
            self.wait(q, key, self.cnt[key])
        inst = self.E[q].dma_start(out=out, in_=in_)
        self.cnt[key] += 16
        inst.then_inc(self.sems[key], 16)
        self._commit(key, self.cnt[key], reads, writes, pwrites)
        self.ninst += 1
        return inst

    def barrier(self):
        for eng in self.E:
            for key, val in self.cnt.items():
                if val > 0:
                    self.wait(eng, key, val)


class Ctx:
    pass


def rlist(name, n):
    return [Res("%s%d" % (name, i)) for i in range(n)]


def v3(ap, a=2):
    return ap.rearrange("p (a b) -> p a b", a=a)


SMW = [("g_ffn1", 32), ("g_mix", 32), ("g_cross", 32), ("g_ffn2", 32), ("pool_scale", 16), ("sg_ln_g", 8),
       ("sg_ln_b", 8), ("conv_w0", 16), ("conv_w1", 16), ("conv_w2", 16), ("conv_w3", 16), ("conv_b", 16),
       ("ssm_norm_g", 8), ("g_final", 32), ("g_mem", 32)]
SMO = {}
_o = 0
for _n, _w in SMW:
    SMO[_n] = _o
    _o += _w
NSM = _o
RWW = [("dt_bias", 16), ("a_log", 16), ("ssm_d", 16), ("sg_b", 512)]
RWO = {}
_o = 0
for _n, _w in RWW:
    RWO[_n] = _o
    _o += _w
NRW = _o


def host_tables(inp):
    sm = np.zeros((DEPTH, 128, NSM), np.float32)
    rw = np.zeros((DEPTH, 128, NRW), np.float32)

    def pp(v):
        v = np.asarray(v, np.float32).reshape(-1)
        return v.reshape(-1, 128).T

    for l in range(DEPTH):
        for n in ("g_ffn1", "g_mix", "g_cross", "g_ffn2", "sg_ln_g", "sg_ln_b", "ssm_norm_g"):
            a = pp(inp[n][l])
            sm[l, :, SMO[n]:SMO[n] + a.shape[1]] = a
        sm[l, :, SMO["pool_scale"]:SMO["pool_scale"] + 16] = pp(inp["pool_scale"][l])
        for j in range(4):
            sm[l, :, SMO["conv_w%d" % j]:SMO["conv_w%d" % j] + 16] = pp(inp["ssm_conv_w"][l, j])
        sm[l, :, SMO["conv_b"]:SMO["conv_b"] + 16] = pp(inp["ssm_conv_b"][l])
        sm[l, :, SMO["g_final"]:SMO["g_final"] + 32] = pp(inp["g_final"])
        sm[l, :, SMO["g_mem"]:SMO["g_mem"] + 32] = pp(inp["g_mem"])
        rw[l, :, RWO["dt_bias"]:RWO["dt_bias"] + 16] = np.asarray(inp["ssm_dt_bias"][l])[None, :]
        rw[l, :, RWO["a_log"]:RWO["a_log"] + 16] = np.asarray(inp["ssm_a_log"][l])[None, :]
        rw[l, :, RWO["ssm_d"]:RWO["ssm_d"] + 16] = np.asarray(inp["ssm_d"][l])[None, :]
        rw[l, :, RWO["sg_b"]:RWO["sg_b"] + 512] = np.asarray(inp["sg_b"][l]).reshape(-1)[None, :]
    return sm, rw


M_ONES, M_ID, M_R128, M_R64, M_SG, M_UT, M_SL, M_SEL = range(8)


def host_consts():
    mats = np.zeros((8, 128, 128), np.float32)
    mats[M_ONES] = 1.0
    mats[M_ID] = np.eye(128)
    r = np.zeros((128, 128), np.float32)
    for d in range(64):
        r[d + 64, d] = -1.0
        r[d, d + 64] = 1.0
    mats[M_R128] = r
    r = np.zeros((128, 128), np.float32)
    for b in range(2):
        for d in range(32):
            r[b * 64 + d + 32, b * 64 + d] = -1.0
            r[b * 64 + d, b * 64 + d + 32] = 1.0
    mats[M_R64] = r
    i = np.arange(128)
    mats[M_SG] = ((i[:, None] // 64) >= (i[None, :] // 64)).astype(np.float32)
    j = np.arange(64)
    mats[M_UT, :64, :64] = (j[:, None] <= j[None, :]).astype(np.float32)
    mats[M_SL, :64, :64] = (j[:, None] > j[None, :]).astype(np.float32)
    mats[M_SEL, 63, :] = 1.0
    pos = np.arange(T, dtype=np.float32)
    rope = np.zeros((4, 128, T), np.float32)
    inv = (10000.0 ** (-np.arange(64, dtype=np.float32) / 64)).astype(np.float32)
    ang = pos[None, :] * inv[:, None]
    rope[0, :64] = np.cos(ang)
    rope[0, 64:] = np.cos(ang)
    rope[1, :64] = np.sin(ang)
    rope[1, 64:] = np.sin(ang)
    inv = (10000.0 ** (-np.arange(32, dtype=np.float32) / 32)).astype(np.float32)
    ang = pos[None, :] * inv[:, None]
    for b in range(4):
        rope[2, b * 32:(b + 1) * 32] = np.cos(ang)
        rope[3, b * 32:(b + 1) * 32] = np.sin(ang)
    cf = np.zeros((128, 80), np.float32)
    cf[:, 0] = EPS
    cf[:, 1] = -1.0e29
    for g, w in enumerate(POOL_WINDOWS):
        cf[:, 16 + g * 16:32 + g * 16] = 1.0 / np.minimum(w, np.arange(16) + 1.0)
    return dict(c_mats=mats, c_rope=rope, c_f32=cf)


def setup(nc, kb):
    c = Ctx()
    c.nc = nc
    c.kb = kb
    c.XA = nc.alloc_sbuf_tensor("XA", [128, 65536], BF16)
    c.XA_res = Res("XA")
    c.NW = 3
    c.WB = [nc.alloc_sbuf_tensor("WB%d" % i, [128, 8192], BF16) for i in range(c.NW)]
    c.WB_res = rlist("WB", c.NW)
    c.wrr = 0
    c.NS = 5
    c.ST = [nc.alloc_sbuf_tensor("ST%d" % i, [128, 1024], F32) for i in range(c.NS)]
    c.ST_res = rlist("ST", c.NS)
    c.srr = 0
    c.PS = nc.alloc_psum_tensor("PS", [128, 8, 512], F32)
    c.PS_res = rlist("PS", 8)
    c.mb = nc.alloc_sbuf_tensor("mats_bf", [128, 4, 128], BF16)
    c.mf = nc.alloc_sbuf_tensor("mats_f", [128, 4, 128], F32)
    c.cf = nc.alloc_sbuf_tensor("cf", [128, 80], F32)
    c.const_res = Res("const")
    c.sm = nc.alloc_sbuf_tensor("smt", [128, NSM], F32)
    c.sm_res = Res("sm")
    c.rw = nc.alloc_sbuf_tensor("rwt", [128, NRW], F32)
    c.rw_res = Res("rw")
    c.kvm = nc.alloc_sbuf_tensor("kvm", [128, 2048], BF16)
    c.ones = c.mb[:, 0, :]
    c.ident = c.mb[:, 1, :]
    c.rstd = c.WB[0][:, 0:4096].bitcast(F32)
    c.rstd_res = c.WB_res[0]
    return c


def stage_tile(c):
    i = c.srr % c.NS
    c.srr += 1
    return c.ST[i], c.ST_res[i]


def xa(c, off, shape, dt, parts=128, p0=0):
    n = 1
    for s in shape:
        n *= s
    esz = 2 if dt == BF16 else 4
    assert off % 4 == 0 and off + n * esz <= 131072, (off, shape)
    base = c.XA[p0:p0 + parts, off // 2: off // 2 + n * esz // 2]
    ap = base if dt == BF16 else base.bitcast(dt)
    if len(shape) == 2:
        ap = ap.rearrange("p (a b) -> p a b", a=shape[0])
    elif len(shape) == 3:
        ap = ap.rearrange("p (a b c) -> p a b c", a=shape[0], b=shape[1])
    return ap


def load_x_norm(c, src, src_res, gcol, t0, Tn, out_dram=None):
    kb, nc = c.kb, c.nc
    X = c.XA[:, 0:DC * Tn].rearrange("p (k t) -> p k t", k=DC)
    TS = min(Tn, 1024)
    nsub = Tn // TS
    nbk = TS // 512 if TS >= 512 else 1
    bw = min(512, TS)

    def rd(ch):
        return [src_res[ch]] if src_res is not None else []

    fused = out_dram is None
    it = 0
    for ch in range(DC):
        for hf in range(nsub):
            st, st_r = stage_tile(c)
            kb.dma("sp", st[:, 0:TS], src[ch * 128:(ch + 1) * 128, t0 + hf * TS:t0 + (hf + 1) * TS],
                   reads=rd(ch), writes=[st_r])
            sq, sq_r = stage_tile(c)
            sqb = sq[:, 0:512].bitcast(BF16)
            kb.op("act", lambda: nc.scalar.activation(out=sqb[:, 0:TS], in_=st[:, 0:TS], func=AF.Square),
                  reads=[st_r], writes=[sq_r])
            if fused:
                eng = "dve" if it % 2 == 0 else "pool"
                E = nc.vector if eng == "dve" else nc.gpsimd
                it += 1
                kb.op(eng, lambda: E.tensor_scalar(out=X[:, ch, hf * TS:(hf + 1) * TS], in0=st[:, 0:TS], scalar1=c.sm[:, gcol + ch:gcol + ch + 1],
                                                   scalar2=None, op0=ALU.mult),
                      reads=[st_r, c.sm_res], pwrites=[c.XA_res])
            for j in range(nbk):
                b = hf * nbk + j
                kb.op("pe", lambda: nc.tensor.matmul(c.PS[:, b, 0:bw], c.ones, sqb[:, j * bw:(j + 1) * bw],
                                                     start=(ch == 0), stop=(ch == DC - 1)),
                      reads=[sq_r, c.const_res], writes=[c.PS_res[b]])
    for b in range(nsub * nbk):
        kb.op("act", lambda: nc.scalar.activation(out=c.rstd[:, b * bw:(b + 1) * bw], in_=c.PS[:, b, 0:bw], func=AF.Sqrt,
                                                  bias=c.cf[:, 0:1], scale=1.0 / D),
              reads=[c.PS_res[b], c.const_res], writes=[c.rstd_res])
    kb.op("dve", lambda: nc.vector.reciprocal(out=c.rstd[:, 0:Tn], in_=c.rstd[:, 0:Tn]),
          reads=[c.rstd_res], writes=[c.rstd_res])
    if fused:
        xw = Res("xnorm")
        it = 0
        for ch in range(DC):
            eng = "dve" if it % 2 == 0 else "pool"
            E = nc.vector if eng == "dve" else nc.gpsimd
            it += 1
            kb.op(eng, lambda: E.tensor_tensor(out=X[:, ch, :], in0=X[:, ch, :], in1=c.rstd[:, 0:Tn], op=ALU.mult),
                  reads=[c.rstd_res, c.XA_res], pwrites=[xw])
        c.XA_res.writers = dict(xw.writers)
        c.XA_res.readers = {}
        return X
    for ch in range(DC):
        for hf in range(nsub):
            st, st_r = stage_tile(c)
            kb.dma("sp", st[:, 0:TS], src[ch * 128:(ch + 1) * 128, t0 + hf * TS:t0 + (hf + 1) * TS],
                   reads=rd(ch), writes=[st_r])
            if out_dram is None:
                kb.op("dve", lambda: nc.vector.scalar_tensor_tensor(out=X[:, ch, hf * TS:(hf + 1) * TS], in0=st[:, 0:TS],
                                                                    scalar=c.sm[:, gcol + ch:gcol + ch + 1],
                                                                    in1=c.rstd[:, hf * TS:(hf + 1) * TS],
                                                                    op0=ALU.mult, op1=ALU.mult),
                      reads=[st_r, c.rstd_res, c.sm_res], pwrites=[c.XA_res])
            else:
                so, so_r = stage_tile(c)
                kb.op("dve", lambda: nc.vector.scalar_tensor_tensor(out=so[:, 0:TS], in0=st[:, 0:TS],
                                                                    scalar=c.sm[:, gcol + ch:gcol + ch + 1],
                                                                    in1=c.rstd[:, hf * TS:(hf + 1) * TS],
                                                                    op0=ALU.mult, op1=ALU.mult),
                      reads=[st_r, c.rstd_res, c.sm_res], writes=[so_r])
                kb.dma("sp", out_dram[ch * 128:(ch + 1) * 128, t0 + hf * TS:t0 + (hf + 1) * TS], so[:, 0:TS],
                       reads=[so_r])
    return X


def load_x_plain(c, src, src_res, KC, t0, Tn, off=0):
    kb = c.kb
    X = xa(c, off, [KC, Tn], BF16)
    for k in range(KC):
        kb.dma("sp", X[:, k, :], src[k * 128:(k + 1) * 128, t0:t0 + Tn],
               reads=[src_res[k]] if src_res is not None else [], pwrites=[c.XA_res])
    return X


def gemm(c, wloads, units, wbufs=None):
    for _ in gemm_iter(c, wloads, units, wbufs):
        pass


def gemm_iter(c, wloads, units, wbufs=None):
    kb, nc = c.kb, c.nc
    W = {}
    priv = {"rr": 0}
    NWB = c.NW if wbufs is None else len(wbufs)

    def issue_w(wi):
        if wbufs is None:
            i = c.wrr % c.NW
            c.wrr += 1
            wb, wr = c.WB[i], c.WB_res[i]
        else:
            wb, wr = wbufs[priv["rr"] % len(wbufs)]
            priv["rr"] += 1
        off = 0
        views = []
        first = True
        for (wd, r0, kc, c0, cw) in wloads[wi]:
            assert off + kc * cw <= wb.shape[1], (off, kc, cw)
            Wv = wb[:, off:off + kc * cw].rearrange("p (k n) -> p k n", k=kc)
            kstep = max(1, min(kc, 2048 // cw))
            for k0 in range(0, kc, kstep):
                kk = min(kstep, kc - k0)
                src = wd[r0 + k0 * 128:r0 + (k0 + kk) * 128, c0:c0 + cw].rearrange("(k p) n -> p k n", p=128)
                if first:
                    kb.dma("pool", Wv[:, k0:k0 + kk, :], src, writes=[wr])
                    first = False
                else:
                    kb.dma("pool", Wv[:, k0:k0 + kk, :], src, pwrites=[wr])
            views.append(Wv)
            off += kc * cw
        W[wi] = (views, wr)

    nxt = 0
    while nxt < min(NWB, len(wloads)):
        issue_w(nxt)
        nxt += 1
    lastw = -1
    for u in units:
        wi = u["w"]
        if wi != lastw:
            if wi >= 1 and nxt < len(wloads) and nxt <= wi + NWB - 1:
                issue_w(nxt)
                nxt += 1
            lastw = wi
        views, wr = W[wi]
        if "pre" in u:
            u["pre"](u)
        for a in u["accs"]:
            Wv = views[a["piece"]]
            kc = wloads[wi][a["piece"]][2]
            X, xres = a["X"], a["xres"]
            wo, ww, t0, tw, bank = a["wo"], a["ww"], a["t0"], a["tw"], a["bank"]
            k0 = a.get("k0", 0)
            k1 = k0 + a.get("kn", kc - k0)
            for k in range(k0, k1):
                if a["kind"] == "fm":
                    kb.op("pe", lambda: nc.tensor.matmul(c.PS[0:ww, bank, 0:tw], Wv[:, k, wo:wo + ww], X[:, k, t0:t0 + tw],
                                                         start=(k == k0), stop=(k == k1 - 1)),
                          reads=[wr, xres], writes=[c.PS_res[bank]])
                else:
                    kb.op("pe", lambda: nc.tensor.matmul(c.PS[0:tw, bank, 0:ww], X[:, k, t0:t0 + tw], Wv[:, k, wo:wo + ww],
                                                         start=(k == k0), stop=(k == k1 - 1)),
                          reads=[wr, xres], writes=[c.PS_res[bank]])
        u["epi"](u)
        yield 1


def fm_acc(piece, X, xres, t0, tw, bank, wo=0, ww=128):
    return dict(kind="fm", piece=piece, X=X, xres=xres, t0=t0, tw=tw, bank=bank, wo=wo, ww=ww)


def residual_units(c, X, KC, wd, xsrc, xsrc_res, xdst, xdst_res, Tn, alpha, tok0):
    kb, nc = c.kb, c.nc
    units, wloads = [], []
    for nb in range(DC):
        pb = (nb % 4) * 2
        wloads.append([(wd, 0, KC, nb * 128, 128)])
        hold = {}

        def pre(u, nb=nb, hold=hold):
            xo, xo_r = stage_tile(c)
            kb.dma("sp", xo[:, :], xsrc[nb * 128:(nb + 1) * 128, tok0:tok0 + Tn],
                   reads=[xsrc_res[nb]] if xsrc_res is not None else [], writes=[xo_r])
            hold["xo"] = (xo, xo_r)

        def epi(u, nb=nb, pb=pb, hold=hold):
            xo, xo_r = hold["xo"]
            xn, xn_r = stage_tile(c)
            kb.op("dve", lambda: nc.vector.scalar_tensor_tensor(out=v3(xn[:, :]), in0=c.PS[:, pb:pb + 2, :], scalar=alpha,
                                                                in1=v3(xo[:, :]), op0=ALU.mult, op1=ALU.add),
                  reads=[xo_r, c.PS_res[pb], c.PS_res[pb + 1]], writes=[xn_r])
            kb.dma("sp", xdst[nb * 128:(nb + 1) * 128, tok0:tok0 + Tn], xn[:, :], reads=[xn_r],
                   pwrites=[xdst_res[nb]] if xdst_res is not None else [])

        accs = [fm_acc(0, X, c.XA_res, 0, 512, pb), fm_acc(0, X, c.XA_res, 512, 512, pb + 1)]
        units.append(dict(w=nb, accs=accs, epi=epi, pre=pre))
    return wloads, units


def ffn(c, xsrc, xdst, gcol, w_in, w_out, HT, HT_res):
    kb, nc = c.kb, c.nc
    xr = rlist("xr", DC)
    X = load_x_norm(c, xsrc, None, gcol, 0, T)
    units = []
    wloads = []
    for j in range(FF // 128):
        wloads.append([(w_in, 0, DC, j * 128, 128), (w_in, 0, DC, FF + j * 128, 128)])
        for th in range(2):
            pb = ((j * 2 + th) % 2) * 4

            def epi(u, j=j, th=th, pb=pb):
                sg, sg_r = stage_tile(c)
                kb.op("act", lambda: nc.scalar.activation(out=v3(sg[:, :]), in_=c.PS[:, pb:pb + 2, :], func=AF.Silu),
                      reads=[c.PS_res[pb], c.PS_res[pb + 1]], writes=[sg_r])
                hh, hh_r = stage_tile(c)
                hb = hh[:, 0:512].bitcast(BF16)
                kb.op("dve", lambda: nc.vector.tensor_tensor(out=v3(hb), in0=v3(sg[:, :]), in1=c.PS[:, pb + 2:pb + 4, :], op=ALU.mult),
                      reads=[sg_r, c.PS_res[pb + 2], c.PS_res[pb + 3]], writes=[hh_r])
                kb.dma("sp", HT[j * 128:(j + 1) * 128, th * 1024:(th + 1) * 1024], hb, reads=[hh_r], pwrites=[HT_res[j]])

            accs = [fm_acc(0, X, c.XA_res, th * 1024, 512, pb), fm_acc(0, X, c.XA_res, th * 1024 + 512, 512, pb + 1),
                    fm_acc(1, X, c.XA_res, th * 1024, 512, pb + 2), fm_acc(1, X, c.XA_res, th * 1024 + 512, 512, pb + 3)]
            units.append(dict(w=j, accs=accs, epi=epi))
    gemm(c, wloads, units)
    for th in range(2):
        X2 = load_x_plain(c, HT, HT_res, FF // 128, th * 1024, 1024)
        wl, un = residual_units(c, X2, FF // 128, w_out, xsrc, xr, xdst, xr, 1024, 0.5, th * 1024)
        gemm(c, wl, un)


def stage_win(c, xsrc, gcol, w_in, CB, HN, DTW):
    kb, nc = c.kb, c.nc
    X = load_x_norm(c, xsrc, None, gcol, 0, T)
    for k in range(DC):
        kb.dma("sp", HN[k * 128:(k + 1) * 128, :], X[:, k, :], reads=[c.XA_res])
    segs = []
    two = [(0, 128), (128, 128)]
    for i in range(8):
        segs.append((i * 256, 256, two, "copy"))
    for i in range(4):
        segs.append((2048 + i * 256, 256, two, "gelu"))
    for i in range(4):
        segs.append((3072 + i * 256, 256, two, "gelu"))
    for i in range(4):
        segs.append((4096 + i * 256, 256, two, "silu"))
    for i in range(8):
        segs.append((5120 + i * 256, 256, two, "copy"))
    for i in range(4):
        segs.append((7184 + i * 256, 256, two, "copy"))
    segs.append((8208, 256, two, "copy"))
    segs.append((8464, 256, two, "copy"))
    for i in range(2):
        segs.append((8720 + i * 256, 256, [(0, 64), (64, 64), (128, 64), (192, 64)], "copy"))
    segs.append((9232, 64, [(0, 64)], "copy"))
    wloads, units = [], []
    un = 0
    for (c0, cw, subs, func) in segs:
        wloads.append([(w_in, 0, DC, c0, cw)])
        wi = len(wloads) - 1
        for (wo, ww) in subs:
            for th in range(2):
                pb = (un % 4) * 2
                un += 1

                def epi(u, c0=c0, wo=wo, ww=ww, th=th, pb=pb, func=func, un=un):
                    st, st_r = stage_tile(c)
                    ob = st[:, 0:512].bitcast(BF16)
                    if func == "copy" and un % 2 == 0:
                        kb.op("dve", lambda: nc.vector.tensor_copy(out=v3(ob[0:ww, :]), in_=c.PS[0:ww, pb:pb + 2, :]),
                              reads=[c.PS_res[pb], c.PS_res[pb + 1]], writes=[st_r])
                    else:
                        f = {"copy": AF.Copy, "gelu": AF.Gelu, "silu": AF.Silu}[func]
                        kb.op("act", lambda: nc.scalar.activation(out=v3(ob[0:ww, :]), in_=c.PS[0:ww, pb:pb + 2, :], func=f),
                              reads=[c.PS_res[pb], c.PS_res[pb + 1]], writes=[st_r])
                    kb.dma("sp", CB[c0 + wo:c0 + wo + ww, th * 1024:(th + 1) * 1024], ob[0:ww, :], reads=[st_r])

                accs = [fm_acc(0, X, c.XA_res, th * 1024, 512, pb, wo, ww), fm_acc(0, X, c.XA_res, th * 1024 + 512, 512, pb + 1, wo, ww)]
                units.append(dict(w=wi, accs=accs, epi=epi))
    wloads.append([(w_in, 0, DC, 7168, 16), (w_in, 0, DC, 9296, 8)])
    wi = len(wloads) - 1
    for tp in range(8):
        pb = (tp % 2) * 4

        def epi(u, tp=tp, pb=pb):
            for j in range(2):
                tb = tp * 2 + j
                st, st_r = stage_tile(c)
                kb.op("dve", lambda: nc.vector.tensor_copy(out=st[:, 0:16], in_=c.PS[:, pb + 2 * j, 0:16]),
                      reads=[c.PS_res[pb + 2 * j]], writes=[st_r])
                kb.op("dve", lambda: nc.vector.tensor_copy(out=st[:, 16:24], in_=c.PS[:, pb + 2 * j + 1, 0:8]),
                      reads=[c.PS_res[pb + 2 * j + 1]], pwrites=[st_r])
                kb.dma("sp", DTW[tb * 128:(tb + 1) * 128, :], st[:, 0:24], reads=[st_r])

        accs = []
        for j in range(2):
            tb = tp * 2 + j
            accs.append(dict(kind="tm", piece=0, X=X, xres=c.XA_res, t0=tb * 128, tw=128, bank=pb + 2 * j, wo=0, ww=16))
            accs.append(dict(kind="tm", piece=1, X=X, xres=c.XA_res, t0=tb * 128, tw=128, bank=pb + 2 * j + 1, wo=0, ww=8))
        units.append(dict(w=wi, accs=accs, epi=epi))
    gemm(c, wloads, units)


def stage_pool(c, CB, YT, pool_w, gates=None):
    kb, nc = c.kb, c.nc
    PW = 2048 + 16
    P = [xa(c, 0, [PW], F32), xa(c, PW * 4, [PW], F32)]
    P_r = [Res("P0"), Res("P1")]
    XP = xa(c, 2 * PW * 4, [4, T], BF16)
    XP_r = Res("XP")
    PWB = [(xa(c, 32896, [2048], BF16), Res("PWB0")), (xa(c, 36992, [2048], BF16), Res("PWB1"))]
    for i in range(2):
        kb.op("dve", lambda: nc.vector.memset(P[i][:, 0:16], 0.0), pwrites=[P_r[i]])
    for g in range(4):
        w = POOL_WINDOWS[g]
        nsteps = int(math.log2(w))
        for cc in range(4):
            ch = g * 4 + cc
            ab, ab_r = stage_tile(c)
            abf = ab[:, :].bitcast(BF16)
            kb.dma("sp", abf, CB[ch * 128:(ch + 1) * 128, :], writes=[ab_r])
            kb.op("act", lambda: nc.scalar.activation(out=P[0][:, 16:PW], in_=abf, func=AF.Copy),
                  reads=[ab_r], pwrites=[P_r[0]])
            cur = 0
            sh = 1
            for s in range(nsteps):
                nx = 1 - cur
                eng = "dve" if s % 2 == 0 else "pool"
                E = nc.vector if eng == "dve" else nc.gpsimd
                kb.op(eng, lambda: E.tensor_tensor(out=P[nx][:, 16:PW], in0=P[cur][:, 16:PW], in1=P[cur][:, 16 - sh:PW - sh], op=ALU.add),
                      reads=[P_r[cur]], pwrites=[P_r[nx]])
                cur = nx
                sh *= 2
            kb.op("dve", lambda: nc.vector.scalar_tensor_tensor(out=XP[:, cc, :], in0=P[cur][:, 16:PW], scalar=1.0 / w, in1=abf,
                                                                op0=ALU.mult, op1=ALU.subtract),
                  reads=[P_r[cur], ab_r], pwrites=[XP_r])
            t16, t16_r = stage_tile(c)
            kb.op("dve", lambda: nc.vector.tensor_tensor(out=t16[:, 0:16], in0=P[cur][:, 16:32], in1=c.cf[:, 16 + g * 16:32 + g * 16], op=ALU.mult),
                  reads=[P_r[cur], c.const_res], writes=[t16_r])
            kb.op("dve", lambda: nc.vector.tensor_tensor(out=XP[:, cc, 0:16], in0=t16[:, 0:16], in1=abf[:, 0:16], op=ALU.subtract),
                  reads=[t16_r, ab_r], pwrites=[XP_r])
        if gates is not None:
            gates.step(6)
        wloads = [[(pool_w[g], 0, 4, 0, 512)]]
        units = []
        for nb in range(4):
            for th in range(2):
                pb = ((nb * 2 + th) % 3) * 2

                def epi(u, nb=nb, th=th, pb=pb, g=g):
                    st, st_r = stage_tile(c)
                    ob = st[:, 0:512].bitcast(BF16)
                    col = SMO["pool_scale"] + g * 4 + nb
                    kb.op("act", lambda: nc.scalar.activation(out=v3(ob), in_=c.PS[:, pb:pb + 2, :], func=AF.Copy, scale=c.sm[:, col:col + 1]),
                          reads=[c.PS_res[pb], c.PS_res[pb + 1], c.sm_res], writes=[st_r])
                    kb.dma("sp", YT[g * 512 + nb * 128:g * 512 + (nb + 1) * 128, th * 1024:(th + 1) * 1024], ob, reads=[st_r])

                accs = [fm_acc(0, XP, XP_r, th * 1024, 512, pb, nb * 128, 128), fm_acc(0, XP, XP_r, th * 1024 + 512, 512, pb + 1, nb * 128, 128)]
                units.append(dict(w=0, accs=accs, epi=epi))
        gemm(c, wloads, units, wbufs=PWB)


def stage_sg(c, CB, YT, sg_w):
    kb, nc = c.kb, c.nc
    V = xa(c, 0, [8, T], BF16)
    V_r = rlist("V", 8)
    mean = xa(c, 32768, [T], F32)
    rstd = xa(c, 40960, [T], F32)
    nb_ = xa(c, 49152, [T], F32)
    st_r = Res("stats")
    VT = xa(c, 57344, [16, 1024], BF16)
    VT_r = rlist("VT", 16)
    WmT = xa(c, 90112, [4, 128], BF16)
    WmT_r = Res("WmT")
    tA = xa(c, 91136, [T], F32)
    tA_r = Res("tA")
    tB = xa(c, 99328, [T], F32)
    tB_r = Res("tB")
    sgb = xa(c, 107520, [512], BF16)
    sgb_r = Res("sgb")
    OUTB = xa(c, 108544, [T], BF16)
    OUTB_r = Res("OUTB")
    UB = xa(c, 112640, [T], BF16)
    UB_r = Res("UB")
    kb.op("act", lambda: nc.scalar.activation(out=sgb, in_=c.rw[:, RWO["sg_b"]:RWO["sg_b"] + 512], func=AF.Copy),
          reads=[c.rw_res], writes=[sgb_r])
    for cch in range(8):
        kb.dma("sp", V[:, cch, :], CB[3072 + cch * 128:3072 + (cch + 1) * 128, :], writes=[V_r[cch]])
        sq, sq_r = stage_tile(c)
        sqb = sq[:, :].bitcast(BF16)
        kb.op("act", lambda: nc.scalar.activation(out=sqb, in_=V[:, cch, :], func=AF.Square), reads=[V_r[cch]], writes=[sq_r])
        for j in range(4):
            kb.op("pe", lambda: nc.tensor.matmul(c.PS[:, j, :], c.ones, V[:, cch, j * 512:(j + 1) * 512], start=(cch == 0), stop=(cch == 7)),
                  reads=[V_r[cch], c.const_res], writes=[c.PS_res[j]])
            kb.op("pe", lambda: nc.tensor.matmul(c.PS[:, 4 + j, :], c.ones, sqb[:, j * 512:(j + 1) * 512], start=(cch == 0), stop=(cch == 7)),
                  reads=[sq_r, c.const_res], writes=[c.PS_res[4 + j]])
    kb.op("act", lambda: nc.scalar.activation(out=v3(mean, 4), in_=c.PS[:, 0:4, :], func=AF.Copy, scale=1.0 / 1024),
          reads=c.PS_res[0:4], writes=[st_r])
    kb.op("dve", lambda: nc.vector.tensor_tensor(out=tA, in0=mean, in1=mean, op=ALU.mult), reads=[st_r], writes=[tA_r])
    kb.op("dve", lambda: nc.vector.scalar_tensor_tensor(out=v3(tB, 4), in0=c.PS[:, 4:8, :], scalar=1.0 / 1024, in1=v3(tA, 4),
                                                        op0=ALU.mult, op1=ALU.subtract),
          reads=c.PS_res[4:8] + [tA_r], writes=[tB_r])
    kb.op("act", lambda: nc.scalar.activation(out=rstd, in_=tB, func=AF.Sqrt, bias=c.cf[:, 0:1], scale=1.0),
          reads=[tB_r, c.const_res, st_r], writes=[st_r])
    kb.op("dve", lambda: nc.vector.reciprocal(out=rstd, in_=rstd), reads=[st_r], writes=[st_r])
    kb.op("dve", lambda: nc.vector.scalar_tensor_tensor(out=nb_, in0=mean, scalar=-1.0, in1=rstd, op0=ALU.mult, op1=ALU.mult),
          reads=[st_r], writes=[st_r])
    for cch in range(8):
        kb.op("dve", lambda: nc.vector.tensor_tensor(out=tA, in0=V[:, cch, :], in1=rstd, op=ALU.mult),
              reads=[V_r[cch], st_r], writes=[tA_r])
        kb.op("pool", lambda: nc.gpsimd.tensor_tensor(out=tB, in0=tA, in1=nb_, op=ALU.add), reads=[tA_r, st_r], writes=[tB_r])
        cg = SMO["sg_ln_g"] + cch
        cb = SMO["sg_ln_b"] + cch
        kb.op("act", lambda: nc.scalar.activation(out=V[:, cch, :], in_=tB, func=AF.Identity, scale=c.sm[:, cg:cg + 1], bias=c.sm[:, cb:cb + 1]),
              reads=[tB_r, c.sm_res], writes=[V_r[cch]])
    for tb in range(16):
        bank = tb % 2
        psb = c.PS[:, bank, :].bitcast(BF16)
        for cch in range(8):
            kb.op("pe", lambda: nc.tensor.transpose(psb[:, cch * 128:(cch + 1) * 128], V[:, cch, tb * 128:(tb + 1) * 128], c.ident),
                  reads=[V_r[cch], c.const_res], writes=[c.PS_res[bank]])
        if tb % 2 == 0:
            kb.op("act", lambda: nc.scalar.activation(out=VT[:, tb, :], in_=psb, func=AF.Copy), reads=[c.PS_res[bank]], writes=[VT_r[tb]])
        else:
            kb.op("dve", lambda: nc.vector.tensor_copy(out=VT[:, tb, :], in_=psb), reads=[c.PS_res[bank]], writes=[VT_r[tb]])
    for g in range(4):
        wst, wst_r = stage_tile(c)
        kb.dma("sp", wst[:, 0:128], sg_w[g], writes=[wst_r])
        wm, wm_r = stage_tile(c)
        wmb = wm[:, 0:64].bitcast(BF16)
        kb.op("dve", lambda: nc.vector.tensor_tensor(out=wmb, in0=wst[:, 0:128], in1=c.mf[:, 0, :], op=ALU.mult),
              reads=[wst_r, c.const_res], writes=[wm_r])
        psb = c.PS[:, 2, :].bitcast(BF16)
        kb.op("pe", lambda: nc.tensor.transpose(psb[:, 0:128], wmb, c.ident), reads=[wm_r, c.const_res], writes=[c.PS_res[2]])
        kb.op("act", lambda: nc.scalar.activation(out=WmT[:, g, :], in_=psb[:, 0:128], func=AF.Copy), reads=[c.PS_res[2]], pwrites=[WmT_r])
    for cch in range(8):
        g = cch // 2
        kb.dma("sp", UB, CB[2048 + cch * 128:2048 + (cch + 1) * 128, :], writes=[UB_r])
        for quad in range(4):
            bank = 4 + (cch * 4 + quad) % 4
            for j in range(4):
                tb = quad * 4 + j
                kb.op("pe", lambda: nc.tensor.matmul(c.PS[:, bank, j * 128:(j + 1) * 128], VT[:, tb, cch * 128:(cch + 1) * 128], WmT[:, g, :],
                                                     start=True, stop=False),
                      reads=[VT_r[tb], WmT_r], writes=[c.PS_res[bank]])
                kb.op("pe", lambda: nc.tensor.matmul(c.PS[:, bank, j * 128:(j + 1) * 128], c.mb[0:1, 0, :], sgb[0:1, g * 128:(g + 1) * 128],
                                                     start=False, stop=True),
                      reads=[sgb_r, c.const_res], writes=[c.PS_res[bank]])
            kb.op("dve", lambda: nc.vector.tensor_tensor(out=OUTB[:, quad * 512:(quad + 1) * 512], in0=c.PS[:, bank, :], in1=UB[:, quad * 512:(quad + 1) * 512], op=ALU.mult),
                  reads=[c.PS_res[bank], UB_r], pwrites=[OUTB_r])
        kb.dma("sp", YT[2048 + cch * 128:2048 + (cch + 1) * 128, :], OUTB, reads=[OUTB_r])


def stage_ssd(c, CB, DTW, YT, gates=None):
    kb, nc = c.kb, c.nc
    SEG = 512
    XSf = xa(c, 0, [8, SEG], BF16)
    XS_r = rlist("XSf", 8)
    BTf = xa(c, 8192, [4, SEG], BF16)
    BT_r = rlist("BTf", 4)
    CTf = xa(c, 12288, [4, SEG], BF16)
    CT_r = rlist("CTf", 4)
    ZTf = xa(c, 16384, [8, SEG], BF16)
    ZT_r = Res("ZTf")
    RP = [xa(c, 24576, [SEG + 4], BF16), xa(c, 25616, [SEG + 4], BF16)]
    RP_r = [Res("RP0"), Res("RP1")]
    AC = [xa(c, 26656, [SEG], F32), xa(c, 28704, [SEG], F32)]
    AC_r = [Res("AC0"), Res("AC1")]
    B0 = 30752
    dt_all = xa(c, B0, [32, 16], F32, parts=64)
    dtA_all = xa(c, B0 + 2048, [32, 16], F32, parts=64)
    dt_r = Res("dt")
    Ab = xa(c, B0 + 4096, [16], F32, parts=64)
    Dd = xa(c, B0 + 4160, [16, 64], BF16, parts=64)
    Dd_r = Res("Dd")
    MT = xa(c, B0 + 6208, [16, 64], BF16, parts=64)
    MT_r = Res("MT")
    cbm = xa(c, B0 + 8256, [4, 64], F32, parts=64)
    cbm_r = Res("cbm")
    xs_tok = xa(c, B0 + 9280, [16, 64], BF16, parts=64)
    xst_r = Res("xs_tok")
    xd = xa(c, B0 + 11328, [16, 64], BF16, parts=64)
    xd_r = Res("xd")
    xdw = xa(c, B0 + 13376, [1024], BF16, parts=64)
    xdw_r = Res("xdw")
    Btok = xa(c, B0 + 15424, [512], BF16, parts=64)
    Btok_r = Res("Btok")
    S = xa(c, B0 + 16448, [1024], F32)
    S_r = Res("S")
    Sbf = xa(c, B0 + 20544, [1024], BF16)
    Sbf_r = Res("Sbf")
    ea = xa(c, B0 + 22592, [16], F32, parts=64)
    ea_r = Res("ea")
    cdb = xa(c, B0 + 22656, [16], F32)
    cdb_r = Res("cdb")
    ssq = xa(c, B0 + 22720, [4], F32, parts=64)
    ssq_r = Res("ssq")
    yn = xa(c, B0 + 22784, [1024], BF16, parts=64)
    yn_r = Res("yn")
    YC = xa(c, B0 + 24832, [8, 256], BF16)
    YC_r = Res("YC")
    assert B0 + 24832 + 4096 <= 63488
    UT = c.mf[0:64, 1, 0:64]
    SL = c.mf[0:64, 2, 0:64]
    SEL = c.mf[0:64, 3, :]

    def gstep(k):
        if gates is not None:
            gates.step(k)

    def conv_segment(seg):
        s0 = seg * SEG
        for ch in range(16):
            i = ch % 2
            row = 5120 + ch * 128
            if seg == 0:
                kb.op("dve", lambda: nc.vector.memset(RP[i][:, 0:4], 0.0), writes=[RP_r[i]])
                kb.dma("sp", RP[i][:, 4:SEG + 4], CB[row:row + 128, 0:SEG], reads=[RP_r[i]], writes=[RP_r[i]])
            else:
                kb.dma("sp", RP[i][:, 0:SEG + 4], CB[row:row + 128, s0 - 4:s0 + SEG], writes=[RP_r[i]])
            w = [SMO["conv_w%d" % j] + ch for j in range(4)]
            bcol = SMO["conv_b"] + ch
            kb.op("dve", lambda: nc.vector.tensor_scalar(out=AC[i], in0=RP[i][:, 4:SEG + 4], scalar1=c.sm[:, w[3]:w[3] + 1], scalar2=c.sm[:, bcol:bcol + 1],
                                                         op0=ALU.mult, op1=ALU.add),
                  reads=[RP_r[i], c.sm_res], writes=[AC_r[i]])
            for j in (2, 1, 0):
                kb.op("dve", lambda: nc.vector.scalar_tensor_tensor(out=AC[i], in0=RP[i][:, 1 + j:SEG + 1 + j], scalar=c.sm[:, w[j]:w[j] + 1], in1=AC[i],
                                                                    op0=ALU.mult, op1=ALU.add),
                      reads=[RP_r[i], AC_r[i], c.sm_res], writes=[AC_r[i]])
            if ch < 8:
                dst, dr = XSf[:, ch, :], XS_r[ch]
            elif ch < 12:
                dst, dr = BTf[:, ch - 8, :], BT_r[ch - 8]
            else:
                dst, dr = CTf[:, ch - 12, :], CT_r[ch - 12]
            kb.op("act", lambda: nc.scalar.activation(out=dst, in_=AC[i], func=AF.Silu), reads=[AC_r[i]], writes=[dr])
        for j in range(8):
            kb.dma("sp", ZTf[:, j, :], CB[4096 + j * 128:4096 + (j + 1) * 128, s0:s0 + SEG],
                   writes=[ZT_r] if j == 0 else (), pwrites=[ZT_r] if j > 0 else ())

    kb.dma("sp", dt_all, DTW[:, 0:16].rearrange("(c l) h -> l c h", l=64), writes=[dt_r])
    o = RWO["dt_bias"]
    kb.op("dve", lambda: nc.vector.tensor_tensor(out=dt_all, in0=dt_all, in1=c.rw[0:64, o:o + 16].unsqueeze(1).broadcast_to([64, 32, 16]), op=ALU.add),
          reads=[dt_r, c.rw_res], writes=[dt_r])
    kb.op("act", lambda: nc.scalar.activation(out=dt_all, in_=dt_all, func=AF.Exp), reads=[dt_r], writes=[dt_r])
    kb.op("act", lambda: nc.scalar.activation(out=dt_all, in_=dt_all, func=AF.Ln, bias=1.0), reads=[dt_r], writes=[dt_r])
    o = RWO["a_log"]
    kb.op("act", lambda: nc.scalar.activation(out=Ab, in_=c.rw[0:64, o:o + 16], func=AF.Exp), reads=[c.rw_res, dt_r], writes=[dt_r])
    kb.op("dve", lambda: nc.vector.scalar_tensor_tensor(out=dtA_all, in0=dt_all, scalar=-1.0, in1=Ab.unsqueeze(1).broadcast_to([64, 32, 16]),
                                                        op0=ALU.mult, op1=ALU.mult),
          reads=[dt_r], writes=[dt_r])
    o = RWO["ssm_d"]
    kb.op("dve", lambda: nc.vector.tensor_tensor(out=Dd, in0=c.mb[0:64, 1, 0:64].unsqueeze(1).broadcast_to([64, 16, 64]),
                                                 in1=c.rw[0:64, o:o + 16].unsqueeze(2).broadcast_to([64, 16, 64]), op=ALU.mult),
          reads=[c.const_res, c.rw_res], writes=[Dd_r])
    kb.op("dve", lambda: nc.vector.memset(S, 0.0), writes=[S_r])
    kb.op("dve", lambda: nc.vector.memset(Sbf, 0.0), writes=[Sbf_r])
    psb0 = c.PS[:, 0, :].bitcast(BF16)
    psb1 = c.PS[:, 1, :].bitcast(BF16)
    P = c.PS_res
    gcol = SMO["ssm_norm_g"]

    def v16(ap):
        return ap.rearrange("p a (h l) -> p (a h) l", l=64)

    for ch in range(32):
        if ch % 8 == 0:
            conv_segment(ch // 8)
        t0 = (ch % 8) * 64
        for j in range(8):
            kb.op("pe", lambda: nc.tensor.transpose(psb0[0:64, j * 128:(j + 1) * 128], XSf[:, j, t0:t0 + 64], c.ident),
                  reads=[XS_r[j], c.const_res], writes=[P[0]])
        for g in range(4):
            kb.op("pe", lambda: nc.tensor.transpose(psb1[0:64, g * 128:(g + 1) * 128], BTf[:, g, t0:t0 + 64], c.ident),
                  reads=[BT_r[g], c.const_res], writes=[P[1]])
        kb.op("act", lambda: nc.scalar.activation(out=xs_tok.rearrange("p h l -> p (h l)"), in_=psb0[0:64, :], func=AF.Copy), reads=[P[0]], writes=[xst_r])
        kb.op("act", lambda: nc.scalar.activation(out=Btok, in_=psb1[0:64, 0:512], func=AF.Copy), reads=[P[1]], writes=[Btok_r])
        for j in range(8):
            kb.op("pe", lambda: nc.tensor.transpose(psb0[0:64, j * 128:(j + 1) * 128], ZTf[:, j, t0:t0 + 64], c.ident),
                  reads=[ZT_r, c.const_res], writes=[P[0]])
        zt, zt_r = stage_tile(c)
        Zt = zt[0:64, :].rearrange("p (h l) -> p h l", l=64)
        kb.op("dve", lambda: nc.vector.tensor_tensor(out=Zt, in0=UT.unsqueeze(1).broadcast_to([64, 16, 64]),
                                                     in1=dtA_all[:, ch, :].unsqueeze(2).broadcast_to([64, 16, 64]), op=ALU.mult),
              reads=[dt_r, c.const_res], writes=[zt_r])
        gstep(1)
        for hf in range(2):
            kb.op("pe", lambda: nc.tensor.matmul(c.PS[0:64, 2 + hf, :], SL, zt[0:64, hf * 512:(hf + 1) * 512], start=True, stop=True),
                  reads=[zt_r, c.const_res], writes=[P[2 + hf]])
        kb.op("pe", lambda: nc.tensor.matmul(c.PS[0:64, 4, 256:272], UT, dtA_all[:, ch, :], start=True, stop=True),
              reads=[dt_r, c.const_res], writes=[P[4]])
        for g in range(4):
            kb.op("pe", lambda: nc.tensor.matmul(c.PS[0:64, 4, g * 64:(g + 1) * 64], BTf[:, g, t0:t0 + 64], CTf[:, g, t0:t0 + 64], start=True, stop=True),
                  reads=[BT_r[g], CT_r[g]], writes=[P[4]])
        dc, dc_r = stage_tile(c)
        dec = dc[0:64, :].rearrange("p (h l) -> p h l", l=64)
        kb.op("act", lambda: nc.scalar.activation(out=v3(dc[0:64, :]), in_=c.PS[0:64, 2:4, :], func=AF.Exp), reads=[P[2], P[3]], writes=[dc_r])
        kb.op("act", lambda: nc.scalar.activation(out=ea, in_=c.PS[0:64, 4, 256:272], func=AF.Exp), reads=[P[4]], writes=[ea_r])
        kb.op("dve", lambda: nc.vector.tensor_tensor(out=cbm, in0=c.PS[0:64, 4, 0:256].rearrange("p (g l) -> p g l", l=64),
                                                     in1=UT.unsqueeze(1).broadcast_to([64, 4, 64]), op=ALU.mult),
              reads=[P[4], c.const_res], writes=[cbm_r])
        kb.op("dve", lambda: nc.vector.tensor_tensor(out=MT.rearrange("p (g a) l -> p g a l", a=4), in0=dec.rearrange("p (g a) l -> p g a l", a=4),
                                                     in1=cbm.unsqueeze(2).broadcast_to([64, 4, 4, 64]), op=ALU.mult),
              reads=[dc_r, cbm_r], writes=[MT_r])
        kb.op("dve", lambda: nc.vector.tensor_tensor(out=xd, in0=xs_tok, in1=dt_all[:, ch, :].unsqueeze(2).broadcast_to([64, 16, 64]), op=ALU.mult),
              reads=[xst_r, dt_r], writes=[xd_r])
        kb.op("dve", lambda: nc.vector.tensor_tensor(out=xdw.rearrange("p (h l) -> p h l", l=64), in0=xd, in1=dec[:, :, 63:64].broadcast_to([64, 16, 64]), op=ALU.mult),
              reads=[xd_r, dc_r], writes=[xdw_r])
        gstep(1)
        for h in range(16):
            o_ap = c.PS[0:64, 5 + h // 8, (h % 8) * 64:(h % 8 + 1) * 64]
            kb.op("pe", lambda: nc.tensor.matmul(o_ap, MT[:, h, :], xd[:, h, :], start=True, stop=False),
                  reads=[MT_r, xd_r], writes=[P[5 + h // 8]])
            kb.op("pe", lambda: nc.tensor.matmul(o_ap, Dd[:, h, :], xs_tok[:, h, :], start=False, stop=True),
                  reads=[Dd_r, xst_r], writes=[P[5 + h // 8]])
        for g in range(4):
            kb.op("pe", lambda: nc.tensor.matmul(c.PS[0:64, 2 + g // 2, (g % 2) * 256:(g % 2 + 1) * 256], CTf[:, g, t0:t0 + 64], Sbf[:, g * 256:(g + 1) * 256],
                                                 start=True, stop=True),
                  reads=[CT_r[g], Sbf_r], writes=[P[2 + g // 2]])
        t1, t1_r = stage_tile(c)
        t1v = t1[0:64, :].rearrange("p (h l) -> p h l", l=64)
        kb.op("dve", lambda: nc.vector.tensor_tensor(out=t1v, in0=v16(c.PS[0:64, 2:4, :]), in1=ea.unsqueeze(2).broadcast_to([64, 16, 64]), op=ALU.mult),
              reads=[P[2], P[3], ea_r], writes=[t1_r])
        kb.op("dve", lambda: nc.vector.tensor_tensor(out=t1v, in0=t1v, in1=v16(c.PS[0:64, 5:7, :]), op=ALU.add),
              reads=[t1_r, P[5], P[6]], writes=[t1_r])
        yz, yz_r = stage_tile(c)
        kb.op("dve", lambda: nc.vector.tensor_tensor(out=yz[0:64, :], in0=t1[0:64, :], in1=psb0[0:64, :], op=ALU.mult),
              reads=[t1_r, P[0]], writes=[yz_r])
        sq, sq_r = stage_tile(c)
        kb.op("pool", lambda: nc.gpsimd.tensor_tensor(out=sq[0:64, :], in0=yz[0:64, :], in1=yz[0:64, :], op=ALU.mult), reads=[yz_r], writes=[sq_r])
        kb.op("dve", lambda: nc.vector.tensor_reduce(out=ssq, in_=sq[0:64, :].rearrange("p (g q) -> p g q", g=4), axis=AX.X, op=ALU.add),
              reads=[sq_r], writes=[ssq_r])
        kb.op("act", lambda: nc.scalar.activation(out=ssq, in_=ssq, func=AF.Sqrt, bias=c.cf[0:64, 0:1], scale=1.0 / 256), reads=[ssq_r, c.const_res], writes=[ssq_r])
        kb.op("dve", lambda: nc.vector.reciprocal(out=ssq, in_=ssq), reads=[ssq_r], writes=[ssq_r])
        kb.op("dve", lambda: nc.vector.tensor_tensor(out=yn.rearrange("p (g q) -> p g q", g=4), in0=yz[0:64, :].rearrange("p (g q) -> p g q", g=4),
                                                     in1=ssq.unsqueeze(2).broadcast_to([64, 4, 256]), op=ALU.mult),
              reads=[yz_r, ssq_r], writes=[yn_r])
        gstep(1)
        for j in range(8):
            kb.op("pe", lambda: nc.tensor.transpose(psb1[:, j * 64:(j + 1) * 64], yn[:, j * 128:(j + 1) * 128], c.mb[0:64, 1, 0:64]),
                  reads=[yn_r, c.const_res], writes=[P[1]])
        slot = ch % 4
        kb.op("dve", lambda: nc.vector.tensor_tensor(out=YC[:, :, slot * 64:(slot + 1) * 64], in0=psb1[:, 0:512].rearrange("p (j l) -> p j l", l=64),
                                                     in1=c.sm[:, gcol:gcol + 8].unsqueeze(2).broadcast_to([128, 8, 64]), op=ALU.mult),
              reads=[P[1], c.sm_res], pwrites=[YC_r])
        if slot == 3:
            tq = ch // 4
            kb.dma("sp", YT[3072:4096, tq * 256:(tq + 1) * 256].rearrange("(j p) t -> p j t", p=128), YC, reads=[YC_r])
        if ch == 31:
            break
        for g in range(4):
            kb.op("pe", lambda: nc.tensor.matmul(c.PS[:, g // 2, (g % 2) * 256:(g % 2 + 1) * 256], Btok[:, g * 128:(g + 1) * 128], xdw[:, g * 256:(g + 1) * 256],
                                                 start=True, stop=True),
                  reads=[Btok_r, xdw_r], writes=[P[g // 2]])
        kb.op("pe", lambda: nc.tensor.matmul(c.PS[:, 4, 288:304], SEL, ea, start=True, stop=True), reads=[ea_r, c.const_res], writes=[P[4]])
        kb.op("act", lambda: nc.scalar.activation(out=cdb, in_=c.PS[:, 4, 288:304], func=AF.Copy), reads=[P[4]], writes=[cdb_r])
        kb.op("dve", lambda: nc.vector.tensor_tensor(out=S.rearrange("p (h l) -> p h l", l=64), in0=S.rearrange("p (h l) -> p h l", l=64),
                                                     in1=cdb.unsqueeze(2).broadcast_to([128, 16, 64]), op=ALU.mult),
              reads=[S_r, cdb_r], writes=[S_r])
        kb.op("dve", lambda: nc.vector.tensor_tensor(out=v3(S), in0=v3(S), in1=c.PS[:, 0:2, :], op=ALU.add), reads=[S_r, P[0], P[1]], writes=[S_r])
        kb.op("act", lambda: nc.scalar.activation(out=Sbf, in_=S, func=AF.Copy), reads=[S_r], writes=[Sbf_r])


def stage_rope(c, CB, rope_d, gates=None):
    kb, nc = c.kb, c.nc
    H = 1024
    tabs = [xa(c, i * 4096, [H], F32) for i in range(4)]
    xin = [xa(c, 16384, [H], BF16), xa(c, 18432, [H], BF16)]
    xin_r = [Res("xin0"), Res("xin1")]
    t1 = [xa(c, 20480, [H], F32), xa(c, 24576, [H], F32)]
    t1_r = [Res("t10"), Res("t11")]
    t2 = [xa(c, 28672, [H], F32), xa(c, 32768, [H], F32)]
    t2_r = [Res("t20"), Res("t21")]
    ob = [xa(c, 36864, [H], BF16), xa(c, 38912, [H], BF16)]
    ob_r = [Res("ob0"), Res("ob1")]
    blocks = [(7184 + i * 128, 128, 0) for i in range(8)] + [(8208 + i * 128, 128, 0) for i in range(2)]
    blocks += [(8720 + i * 128, 128, 1) for i in range(4)] + [(9232, 64, 1)]
    bi = 0
    for th in range(2):
        tab_r = Res("tabs%d" % th)
        for i in range(4):
            kb.dma("sp", tabs[i], rope_d[i][:, th * H:(th + 1) * H], writes=[tab_r, t1_r[0], t1_r[1], t2_r[0], t2_r[1]] if i == 0 else (),
                   pwrites=[tab_r] if i > 0 else ())
        for (r0, rows, kind) in blocks:
            i = bi % 2
            bi += 1
            pb = i * 2
            cosT, sinT = tabs[2 * kind], tabs[2 * kind + 1]
            Rm = c.mb[0:rows, 2 + kind, 0:rows]
            kb.dma("sp", xin[i][0:rows, :], CB[r0:r0 + rows, th * H:(th + 1) * H], writes=[xin_r[i]])
            for j in range(2):
                kb.op("pe", lambda: nc.tensor.matmul(c.PS[0:rows, pb + j, :], Rm, xin[i][0:rows, j * 512:(j + 1) * 512], start=True, stop=True),
                      reads=[xin_r[i], c.const_res], writes=[c.PS_res[pb + j]])
            kb.op("pool", lambda: nc.gpsimd.tensor_tensor(out=t1[i][0:rows, :], in0=xin[i][0:rows, :], in1=cosT[0:rows, :], op=ALU.mult),
                  reads=[xin_r[i], tab_r], writes=[t1_r[i]])
            kb.op("dve", lambda: nc.vector.tensor_tensor(out=v3(t2[i][0:rows, :], 2), in0=c.PS[0:rows, pb:pb + 2, :], in1=v3(sinT[0:rows, :], 2), op=ALU.mult),
                  reads=c.PS_res[pb:pb + 2] + [tab_r], writes=[t2_r[i]])
            kb.op("pool", lambda: nc.gpsimd.tensor_tensor(out=ob[i][0:rows, :], in0=t1[i][0:rows, :], in1=t2[i][0:rows, :], op=ALU.add),
                  reads=[t1_r[i], t2_r[i]], writes=[ob_r[i]])
            kb.dma("sp", CB[r0:r0 + rows, th * H:(th + 1) * H], ob[i][0:rows, :], reads=[ob_r[i]])
            if gates is not None:
                gates.step(1)


class GateGen:
    def __init__(self, c, HN, w_gate, GS):
        self.c, self.HN, self.w_gate, self.GS = c, HN, w_gate, GS
        self.it = self._gen()
        self.done = False
        self.nunits = 0
        self.banks = [6, 7]

    def step(self, k):
        for _ in range(k):
            if self.done:
                return
            try:
                next(self.it)
                self.nunits += 1
            except StopIteration:
                self.done = True

    def drain(self):
        while not self.done:
            self.step(64)

    def _gen(self):
        c = self.c
        kb, nc = c.kb, c.nc
        XH = xa(c, 65536, [DC, 1024], BF16)
        gst = [xa(c, 63488, [512], BF16), xa(c, 64512, [512], BF16)]
        gst_r = [Res("gst0"), Res("gst1")]
        prev = None
        for th in range(2):
            tok0 = th * 1024
            xh_r = Res("XH%d" % th)
            for k in range(DC):
                kb.dma("sp", XH[:, k, :], self.HN[k * 128:(k + 1) * 128, tok0:tok0 + 1024], pwrites=[xh_r],
                       writes=[prev] if (k == 0 and prev is not None) else ())
            prev = xh_r
            wloads = []
            for i in range(4):
                for nb in range(DC):
                    wloads.append([(self.w_gate[i], 0, DC, nb * 128, 128)])

            def unit_gen(th=th, tok0=tok0, xh_r=xh_r):
                un = 0
                for i in range(4):
                    for nb in range(DC):
                        for tq in range(2):
                            bank = self.banks[un % len(self.banks)]
                            un += 1

                            def epi(u, i=i, nb=nb, tq=tq, bank=bank):
                                j = bank % 2
                                kb.op("act", lambda: nc.scalar.activation(out=gst[j], in_=c.PS[:, bank, :], func=AF.Sigmoid),
                                      reads=[c.PS_res[bank]], writes=[gst_r[j]])
                                kb.dma("sp", self.GS[i, nb * 128:(nb + 1) * 128, tok0 + tq * 512:tok0 + (tq + 1) * 512], gst[j], reads=[gst_r[j]])

                            yield dict(w=i * DC + nb, accs=[fm_acc(0, XH, xh_r, tq * 512, 512, bank)], epi=epi)

            for _ in gemm_iter(c, wloads, unit_gen()):
                yield 1


def stage_dsa(c, CB, DTW, YT, gates):
    kb, nc = c.kb, c.nc
    P = c.PS_res
    KR = xa(c, 0, [2, T], BF16)
    KI = xa(c, 8192, [T], BF16, parts=64)
    ld_r = Res("loads")
    VTk = xa(c, 12288, [16, 256], BF16)
    VT_r = Res("VTk")
    QRq = [xa(c, 20480, [8, 128], BF16), xa(c, 22528, [8, 128], BF16)]
    QRq_r = [Res("QRq0"), Res("QRq1")]
    QIq = [xa(c, 24576, [8, 128], BF16, parts=64), xa(c, 26624, [8, 128], BF16, parts=64)]
    QIq_r = [Res("QIq0"), Res("QIq1")]
    acc = xa(c, 28672, [T], F32)
    acc_r = Res("acc")
    vtmp = xa(c, 28672, [2, T], BF16)
    work = xa(c, 36864, [T], F32)
    work_r = Res("work")
    maskb = xa(c, 45056, [T], BF16)
    maskb_r = Res("maskb")
    MTs = [xa(c, 49152, [16, 128], BF16), xa(c, 53248, [16, 128], BF16)]
    MTs_r = [Res("MTs0"), Res("MTs1")]
    wi_all = xa(c, 57344, [16, 8], F32)
    mx = xa(c, 57856, [8], F32)
    mx_r = Res("mx")
    Pt = [xa(c, 57888, [512], BF16), xa(c, 58912, [512], BF16)]
    Pt_r = [Res("Pt0"), Res("Pt1")]
    Pm = [xa(c, 59936, [512], BF16), xa(c, 60960, [512], BF16)]
    Pm_r = [Res("Pm0"), Res("Pm1")]
    ob = xa(c, 61984, [512], BF16)
    ob_r = Res("ob")
    for g in range(2):
        kb.dma("sp", KR[:, g, :], CB[8208 + g * 128:8208 + (g + 1) * 128, :], pwrites=[ld_r])
        kb.dma("sp", vtmp[:, g, :], CB[8464 + g * 128:8464 + (g + 1) * 128, :], pwrites=[acc_r])
    kb.dma("sp", KI, CB[9232:9296, :], pwrites=[ld_r])
    kb.dma("sp", wi_all, DTW[:, 16:24].rearrange("(b p) h -> p b h", p=128), pwrites=[ld_r])
    for tq in range(4):
        psb = c.PS[:, tq % 2, :].bitcast(BF16)
        for j in range(4):
            tb = tq * 4 + j
            for g in range(2):
                kb.op("pe", lambda: nc.tensor.transpose(psb[:, j * 256 + g * 128:j * 256 + (g + 1) * 128], vtmp[:, g, tb * 128:(tb + 1) * 128], c.ident),
                      reads=[acc_r, c.const_res], writes=[P[tq % 2]])
        kb.op("act", lambda: nc.scalar.activation(out=VTk[:, tq * 4:(tq + 1) * 4, :].rearrange("p a b -> p (a b)"), in_=psb, func=AF.Copy),
              reads=[P[tq % 2]], pwrites=[VT_r])
    scale = 128.0 ** -0.5

    def load_qr(qb):
        q0 = qb * 128
        kb.dma("sp", QRq[qb % 2], CB[7184:8208, q0:q0 + 128].rearrange("(h p) t -> p h t", p=128), writes=[QRq_r[qb % 2]])

    def load_qi(qb):
        q0 = qb * 128
        kb.dma("sp", QIq[qb % 2], CB[8720:9232, q0:q0 + 128].rearrange("(h p) t -> p h t", p=64), writes=[QIq_r[qb % 2]])

    def indexer(qb):
        n = (qb + 1) * 128
        ngr = (n + 511) // 512
        it = 0
        for h in range(8):
            for kg in range(ngr):
                kw = min(512, n - kg * 512)
                bank = it % 2
                it += 1
                kb.op("pe", lambda: nc.tensor.matmul(c.PS[:, bank, 0:kw], QIq[qb % 2][:, h, :], KI[:, kg * 512:kg * 512 + kw], start=True, stop=True),
                      reads=[ld_r, QIq_r[qb % 2]], writes=[P[bank]])
                rl, rl_r = stage_tile(c)
                kb.op("act", lambda: nc.scalar.activation(out=rl[:, 0:kw], in_=c.PS[:, bank, 0:kw], func=AF.Relu), reads=[P[bank]], writes=[rl_r])
                if h == 0:
                    kb.op("dve", lambda: nc.vector.tensor_scalar_mul(out=acc[:, kg * 512:kg * 512 + kw], in0=rl[:, 0:kw], scalar1=wi_all[:, qb, 0:1]),
                          reads=[rl_r, ld_r], writes=[acc_r] if kg == 0 else (), pwrites=[acc_r] if kg > 0 else ())
                else:
                    kb.op("dve", lambda: nc.vector.scalar_tensor_tensor(out=acc[:, kg * 512:kg * 512 + kw], in0=rl[:, 0:kw], scalar=wi_all[:, qb, h:h + 1],
                                                                        in1=acc[:, kg * 512:kg * 512 + kw], op0=ALU.mult, op1=ALU.add),
                          reads=[rl_r, ld_r, acc_r], writes=[acc_r])
        kb.op("dve", lambda: nc.vector.memset(acc[0:64, n - 64:n], NEG), reads=[acc_r], writes=[acc_r])

    def topk(qb):
        n = (qb + 1) * 128
        if qb >= 2:
            kb.op("pool", lambda: nc.gpsimd.tensor_copy(out=work[:, 0:n], in_=acc[:, 0:n]), reads=[acc_r], writes=[work_r])
            for r in range(32):
                kb.op("dve", lambda: nc.vector.max(out=mx, in_=work[:, 0:n]), reads=[work_r], writes=[mx_r])
                if r < 31:
                    kb.op("dve", lambda: nc.vector.match_replace(out=work[:, 0:n], in_to_replace=mx, in_values=work[:, 0:n], imm_value=NEG),
                          reads=[mx_r, work_r], writes=[work_r])
            thr, thr_reads = mx[:, 7:8], [mx_r]
        else:
            thr, thr_reads = c.cf[:, 1:2], [c.const_res]
        kb.op("dve", lambda: nc.vector.tensor_single_scalar(out=maskb[:, 0:n], in_=acc[:, 0:n], scalar=thr, op=ALU.is_ge),
              reads=[acc_r] + thr_reads, writes=[maskb_r])

    def mask_T(qb):
        nk = qb + 1
        M, M_r = MTs[qb % 2], MTs_r[qb % 2]
        for kb0 in range(0, nk, 8):
            cnt = min(8, nk - kb0)
            bank = kb0 // 8
            psb = c.PS[:, bank, :].bitcast(BF16)
            for j in range(cnt):
                kbi = kb0 + j
                kb.op("pe", lambda: nc.tensor.transpose(psb[:, j * 128:(j + 1) * 128], maskb[:, kbi * 128:(kbi + 1) * 128], c.ident),
                      reads=[maskb_r, c.const_res], writes=[P[bank]])
            kb.op("act", lambda: nc.scalar.activation(out=M[:, kb0:kb0 + cnt, :].rearrange("p a b -> p (a b)"), in_=psb[:, 0:cnt * 128], func=AF.Copy),
                  reads=[P[bank]], writes=[M_r] if kb0 == 0 else (), pwrites=[M_r] if kb0 > 0 else ())

    def attention(qb):
        nk = qb + 1
        q0 = qb * 128
        M, M_r = MTs[qb % 2], MTs_r[qb % 2]
        Q, Q_r = QRq[qb % 2], QRq_r[qb % 2]
        for g in range(2):
            for kbi in range(nk):
                sb = 2 + kbi % 2
                j = kbi % 2
                kb.op("pe", lambda: nc.tensor.matmul(c.PS[:, sb, :].rearrange("p (h q) -> p h q", h=4), KR[:, g, kbi * 128:(kbi + 1) * 128],
                                                     Q[:, 4 * g:4 * g + 4, :], start=True, stop=True),
                      reads=[ld_r, Q_r], writes=[P[sb]])
                kb.op("act", lambda: nc.scalar.activation(out=Pt[j], in_=c.PS[:, sb, :], func=AF.Exp, scale=scale), reads=[P[sb]], writes=[Pt_r[j]])
                kb.op("pool", lambda: nc.gpsimd.tensor_tensor(out=Pm[j].rearrange("p (h q) -> p h q", h=4), in0=Pt[j].rearrange("p (h q) -> p h q", h=4),
                                                              in1=M[:, kbi, :].unsqueeze(1).broadcast_to([128, 4, 128]), op=ALU.mult),
                      reads=[Pt_r[j], M_r], writes=[Pm_r[j]])
                kb.op("pe", lambda: nc.tensor.matmul(c.PS[:, 4, :], VTk[:, kbi, g * 128:(g + 1) * 128], Pm[j], start=(kbi == 0), stop=(kbi == nk - 1)),
                      reads=[VT_r, Pm_r[j]], writes=[P[4]])
                kb.op("pe", lambda: nc.tensor.matmul(c.PS[:, 5, :], c.ones, Pm[j], start=(kbi == 0), stop=(kbi == nk - 1)),
                      reads=[c.const_res, Pm_r[j]], writes=[P[5]])
            o_t, o_r = stage_tile(c)
            kb.op("act", lambda: nc.scalar.activation(out=o_t[:, 0:512], in_=c.PS[:, 4, :], func=AF.Copy), reads=[P[4]], writes=[o_r])
            kb.op("act", lambda: nc.scalar.activation(out=o_t[:, 512:1024], in_=c.PS[:, 5, :], func=AF.Ln), reads=[P[5]], pwrites=[o_r])
            kb.op("act", lambda: nc.scalar.activation(out=o_t[:, 512:1024], in_=o_t[:, 512:1024], func=AF.Exp, scale=-1.0), reads=[o_r], writes=[o_r])
            kb.op("pool", lambda: nc.gpsimd.tensor_tensor(out=ob, in0=o_t[:, 0:512], in1=o_t[:, 512:1024], op=ALU.mult), reads=[o_r], writes=[ob_r])
            kb.dma("sp", YT[4096 + 4 * g * 128:4096 + (4 * g + 4) * 128, q0:q0 + 128].rearrange("(h p) t -> p h t", p=128),
                   ob.rearrange("p (h q) -> p h q", h=4), reads=[ob_r])

    load_qr(0)
    load_qi(0)
    indexer(0)
    for qb in range(16):
        if qb < 15:
            load_qi(qb + 1)
        topk(qb)
        if qb > 0:
            attention(qb - 1)
        if qb < 15:
            load_qr(qb + 1)
        if gates is not None:
            gates.step(int(round(512.0 * (qb + 1) / 136.0)) + 1)
        if qb < 15:
            indexer(qb + 1)
        mask_T(qb)
    attention(15)
    if gates is not None:
        gates.drain()


BR_ROWS = ((0, 16), (2048, 8), (3072, 8), (4096, 8))


def stage_merge(c, YT, GS, w_branch, MTd):
    kb, nc = c.kb, c.nc
    for th in range(2):
        tok0 = th * 1024
        XY = load_x_plain(c, YT, None, 40, tok0, 1024, off=0)
        wloads, units = [], []
        for nb in range(DC):
            wloads.append([(w_branch, 0, 40, nb * 128, 128)])
            for tq in range(2):
                pb = ((nb * 2 + tq) % 2) * 4
                hold = {}

                def pre(u, nb=nb, tq=tq, hold=hold):
                    gt, gt_r = stage_tile(c)
                    kb.dma("sp", gt[:, :].bitcast(BF16).rearrange("p (i t) -> p i t", i=4),
                           GS[:, nb * 128:(nb + 1) * 128, tok0 + tq * 512:tok0 + (tq + 1) * 512].rearrange("i p t -> p i t"), writes=[gt_r])
                    hold["gt"] = (gt, gt_r)

                def epi(u, nb=nb, tq=tq, pb=pb, hold=hold):
                    gt, gt_r = hold["gt"]
                    gv = gt[:, :].bitcast(BF16).rearrange("p (i t) -> p i t", i=4)
                    ma, ma_r = stage_tile(c)
                    mb_, mb_r = stage_tile(c)
                    kb.op("dve", lambda: nc.vector.tensor_tensor(out=v3(ma[:, :]), in0=c.PS[:, pb:pb + 2, :], in1=gv[:, 0:2, :], op=ALU.mult),
                          reads=[gt_r, c.PS_res[pb], c.PS_res[pb + 1]], writes=[ma_r])
                    kb.op("dve", lambda: nc.vector.tensor_tensor(out=v3(mb_[:, :]), in0=c.PS[:, pb + 2:pb + 4, :], in1=gv[:, 2:4, :], op=ALU.mult),
                          reads=[gt_r, c.PS_res[pb + 2], c.PS_res[pb + 3]], writes=[mb_r])
                    kb.op("dve", lambda: nc.vector.tensor_tensor(out=ma[:, :], in0=ma[:, :], in1=mb_[:, :], op=ALU.add),
                          reads=[ma_r, mb_r], writes=[ma_r])
                    ob = mb_[:, 0:256].bitcast(BF16)
                    kb.op("dve", lambda: nc.vector.tensor_tensor(out=ob, in0=ma[:, 0:512], in1=ma[:, 512:1024], op=ALU.add),
                          reads=[ma_r, mb_r], writes=[mb_r])
                    kb.dma("sp", MTd[nb * 128:(nb + 1) * 128, tok0 + tq * 512:tok0 + (tq + 1) * 512], ob, reads=[mb_r])

                accs = []
                for i in range(4):
                    r0, kci = BR_ROWS[i]
                    a = fm_acc(0, XY, c.XA_res, tq * 512, 512, pb + i)
                    a["k0"] = r0 // 128
                    a["kn"] = kci
                    accs.append(a)
                units.append(dict(w=nb, accs=accs, epi=epi, pre=pre))
        gemm(c, wloads, units)


def stage_wout(c, MTd, w_out, XS):
    xr = rlist("xr", DC)
    for th in range(2):
        X = load_x_plain(c, MTd, None, DC, th * 1024, 1024)
        wl, un = residual_units(c, X, DC, w_out, XS, xr, XS, xr, 1024, 1.0, th * 1024)
        gemm(c, wl, un)


def stage_cross(c, XS, memT, w_q, w_kv, w_o, QMd):
    kb, nc = c.kb, c.nc
    P = c.PS_res
    KM = c.kvm[:, 0:1024].rearrange("p (h m) -> p h m", h=4)
    VM = c.kvm[:, 1024:2048].rearrange("p (a b) -> p a b", a=2)
    kv_r = Res("kv")
    X = load_x_norm(c, memT, None, SMO["g_mem"], 0, MEM)
    wloads, units = [], []
    for j in range(2):
        wloads.append([(w_kv, 0, DC, j * 256, 256)])
        for s in range(2):
            hh = j * 2 + s
            pb = hh * 2 % 8

            def epi(u, hh=hh, pb=pb):
                kb.op("act", lambda: nc.scalar.activation(out=KM[:, hh, :], in_=c.PS[:, pb, 0:256], func=AF.Copy), reads=[P[pb]], pwrites=[kv_r])

            units.append(dict(w=j, accs=[fm_acc(0, X, c.XA_res, 0, 256, pb, s * 128, 128)], epi=epi))
    for j in range(2):
        wloads.append([(w_kv, 0, DC, 512 + j * 256, 256)])
        for mb in range(2):
            pb = (j * 2 + mb) * 2 % 8 + 1

            def epi(u, j=j, mb=mb, pb=pb):
                kb.op("act", lambda: nc.scalar.activation(out=VM[:, mb, j * 256:(j + 1) * 256], in_=c.PS[:, pb, 0:256], func=AF.Copy), reads=[P[pb]], pwrites=[kv_r])

            units.append(dict(w=2 + j, accs=[dict(kind="tm", piece=0, X=X, xres=c.XA_res, t0=mb * 128, tw=128, bank=pb, wo=0, ww=256)], epi=epi))
    gemm(c, wloads, units)
    kb.barrier()
    X = load_x_norm(c, XS, None, SMO["g_cross"], 0, T)
    wloads, units = [], []
    for j in range(2):
        wloads.append([(w_q, 0, DC, j * 256, 256)])
        for s in range(2):
            for th in range(2):
                un = (j * 2 + s) * 2 + th
                pb = (un % 4) * 2

                def epi(u, j=j, s=s, th=th, pb=pb):
                    st, st_r = stage_tile(c)
                    ob = st[:, 0:512].bitcast(BF16)
                    kb.op("act", lambda: nc.scalar.activation(out=v3(ob), in_=c.PS[:, pb:pb + 2, :], func=AF.Copy), reads=[P[pb], P[pb + 1]], writes=[st_r])
                    kb.dma("sp", QMd[(j * 2 + s) * 128:(j * 2 + s + 1) * 128, th * 1024:(th + 1) * 1024], ob, reads=[st_r])

                accs = [fm_acc(0, X, c.XA_res, th * 1024, 512, pb, s * 128, 128), fm_acc(0, X, c.XA_res, th * 1024 + 512, 512, pb + 1, s * 128, 128)]
                units.append(dict(w=j, accs=accs, epi=epi))
    gemm(c, wloads, units)
    kb.barrier()
    QM = xa(c, 0, [4, T], BF16)
    qm_r = Res("QM")
    for h in range(4):
        kb.dma("sp", QM[:, h, :], QMd[h * 128:(h + 1) * 128, :], pwrites=[qm_r])
    OM = xa(c, 16384, [4, T], BF16)
    om_r = Res("OM")
    Pt = [xa(c, 32768, [512], BF16), xa(c, 33792, [512], BF16)]
    Pt_r = [Res("cPt0"), Res("cPt1")]
    rs = xa(c, 34816, [512], F32)
    rs_r = Res("crs")
    scale = 128.0 ** -0.5
    it = 0
    for h in range(4):
        for qg in range(4):
            Ob = 4 + (it % 2) * 2
            Sb = Ob + 1
            it += 1
            for mb in range(2):
                sb = mb
                kb.op("pe", lambda: nc.tensor.matmul(c.PS[:, sb, :], KM[:, h, mb * 128:(mb + 1) * 128], QM[:, h, qg * 512:(qg + 1) * 512], start=True, stop=True),
                      reads=[kv_r, qm_r], writes=[P[sb]])
                kb.op("act", lambda: nc.scalar.activation(out=Pt[sb], in_=c.PS[:, sb, :], func=AF.Exp, scale=scale), reads=[P[sb]], writes=[Pt_r[sb]])
                kb.op("pe", lambda: nc.tensor.matmul(c.PS[:, Ob, :], VM[:, mb, h * 128:(h + 1) * 128], Pt[sb], start=(mb == 0), stop=(mb == 1)),
                      reads=[kv_r, Pt_r[sb]], writes=[P[Ob]])
                kb.op("pe", lambda: nc.tensor.matmul(c.PS[:, Sb, :], c.ones, Pt[sb], start=(mb == 0), stop=(mb == 1)),
                      reads=[c.const_res, Pt_r[sb]], writes=[P[Sb]])
            kb.op("dve", lambda: nc.vector.reciprocal(out=rs, in_=c.PS[:, Sb, :]), reads=[P[Sb]], writes=[rs_r])
            kb.op("dve", lambda: nc.vector.tensor_tensor(out=OM[:, h, qg * 512:(qg + 1) * 512], in0=c.PS[:, Ob, :], in1=rs, op=ALU.mult),
                  reads=[P[Ob], rs_r], pwrites=[om_r])
    xr = rlist("xr", DC)
    for th in range(2):
        wloads, units = [], []
        tok0 = th * 1024
        for nb in range(DC):
            pb = (nb % 4) * 2
            if nb % 4 == 0:
                wloads.append([(w_o, 0, 4, nb * 128, 512)])
            hold = {}

            def pre(u, nb=nb, hold=hold):
                xo, xo_r = stage_tile(c)
                kb.dma("sp", xo[:, :], XS[nb * 128:(nb + 1) * 128, tok0:tok0 + 1024], reads=[xr[nb]], writes=[xo_r])
                hold["xo"] = (xo, xo_r)

            def epi(u, nb=nb, pb=pb, hold=hold):
                xo, xo_r = hold["xo"]
                xn, xn_r = stage_tile(c)
                kb.op("dve", lambda: nc.vector.tensor_tensor(out=v3(xn[:, :]), in0=c.PS[:, pb:pb + 2, :], in1=v3(xo[:, :]), op=ALU.add),
                      reads=[xo_r, P[pb], P[pb + 1]], writes=[xn_r])
                kb.dma("sp", XS[nb * 128:(nb + 1) * 128, tok0:tok0 + 1024], xn[:, :], reads=[xn_r], pwrites=[xr[nb]])

            accs = [fm_acc(0, OM, om_r, tok0, 512, pb, (nb % 4) * 128, 128), fm_acc(0, OM, om_r, tok0 + 512, 512, pb + 1, (nb % 4) * 128, 128)]
            units.append(dict(w=nb // 4, accs=accs, epi=epi, pre=pre))
        gemm(c, wloads, units)


STAGES = ("ffn1", "win", "pool", "sg", "ssd", "dsa", "merge", "cross", "ffn2")


def build(plan, dbg=()):
    nc = bass.Bass("TRN2", target_bir_lowering=False)
    kb = KB(nc)
    c = setup(nc, kb)

    def dram_in(name, shape, dt=F32):
        return nc.dram_tensor(name, list(shape), dt, kind="ExternalInput").ap()

    def dram_tmp(name, shape, dt):
        kind = "ExternalOutput" if name in dbg else "Internal"
        return nc.dram_tensor(name, list(shape), dt, kind=kind).ap()

    xT = dram_in("xT", [D, T])
    sm_d = dram_in("sm_in", [DEPTH, 128, NSM])
    rw_d = dram_in("rw_in", [DEPTH, 128, NRW])
    mats_d = dram_in("c_mats", [8, 128, 128])
    rope_d = dram_in("c_rope", [4, 128, T])
    cf_d = dram_in("c_f32", [128, 80])
    kb.dma("pool", c.mb[:, :, :], mats_d[0:4].rearrange("m p n -> p m n"), writes=[c.const_res])
    kb.dma("sp", c.mf[:, :, :], mats_d[4:8].rearrange("m p n -> p m n"), pwrites=[c.const_res])
    kb.dma("sp", c.cf[:, :], cf_d, pwrites=[c.const_res])

    XS = dram_tmp("XS", [D, T], F32)
    HT = dram_tmp("HT", [FF, T], BF16)
    HT_res = rlist("HT", FF // 128)
    CB = dram_tmp("CB", [NIN, T], BF16)
    HN = dram_tmp("HN", [D, T], BF16)
    DTW = dram_tmp("DTW", [T, 24], F32)
    YT = dram_tmp("YT", [5120, T], BF16)
    MTd = dram_tmp("MTd", [D, T], BF16)
    QMd = dram_tmp("QMd", [512, T], BF16)
    GS = dram_tmp("GS", [4, D, T], BF16)
    memT = None

    cur = xT
    for l in range(DEPTH):
        if not any(p[1] == l for p in plan):
            continue
        kb.barrier()
        kb.dma("sp", c.sm[:, :], sm_d[l], writes=[c.sm_res])
        kb.dma("sp", c.rw[:, :], rw_d[l], writes=[c.rw_res])
        kb.barrier()
        if ("ffn1", l) in plan:
            w1 = dram_in("w_ffn1_in_%d" % l, [D, 2 * FF])
            w2 = dram_in("w_ffn1_out_%d" % l, [FF, D])
            ffn(c, cur, XS, SMO["g_ffn1"], w1, w2, HT, HT_res)
            cur = XS
            kb.barrier()
        if ("win", l) in plan:
            w_in = dram_in("w_in_%d" % l, [D, NIN])
            stage_win(c, cur, SMO["g_mix"], w_in, CB, HN, DTW)
            kb.barrier()
        if ("sg", l) in plan:
            sw = dram_in("sg_w_%d" % l, [4, 128, 128])
            stage_sg(c, CB, YT, sw)
            kb.barrier()
        gates = None
        if ("merge", l) in plan:
            wg = dram_in("w_gate_%d" % l, [4, D, D])
            gates = GateGen(c, HN, wg, GS)
        if ("ssd", l) in plan:
            if gates is not None:
                gates.banks = [7]
            stage_ssd(c, CB, DTW, YT, gates)
            kb.barrier()
        if gates is not None:
            gates.banks = [6, 7]
        if ("pool", l) in plan:
            pw = dram_in("pool_w_%d" % l, [4, 512, 512])
            stage_pool(c, CB, YT, pw, gates)
            kb.barrier()
        if ("dsa", l) in plan:
            stage_rope(c, CB, rope_d, gates)
            kb.barrier()
            stage_dsa(c, CB, DTW, YT, gates)
            kb.barrier()
        elif gates is not None:
            gates.drain()
            kb.barrier()
        if ("merge", l) in plan:
            wbr = dram_in("w_branch_%d" % l, [5120, D])
            wo = dram_in("w_out_%d" % l, [D, D])
            if cur is xT:
                for k in range(DC):
                    kb.dma("sp", XS[k * 128:(k + 1) * 128, :], xT[k * 128:(k + 1) * 128, :])
                cur = XS
                kb.barrier()
            stage_merge(c, YT, GS, wbr, MTd)
            kb.barrier()
            stage_wout(c, MTd, wo, XS)
            kb.barrier()
        if ("cross", l) in plan:
            if memT is None:
                memT = dram_in("memT", [D, MEM])
            wq = dram_in("w_mem_q_%d" % l, [D, 512])
            wkv = dram_in("w_mem_kv_%d" % l, [D, 1024])
            wmo = dram_in("w_mem_o_%d" % l, [512, D])
            stage_cross(c, XS, memT, wq, wkv, wmo, QMd)
            kb.barrier()
        if ("ffn2", l) in plan:
            w1 = dram_in("w_ffn2_in_%d" % l, [D, 2 * FF])
            w2 = dram_in("w_ffn2_out_%d" % l, [FF, D])
            ffn(c, XS, XS, SMO["g_ffn2"], w1, w2, HT, HT_res)
            kb.barrier()
    if "final" in [p[0] for p in plan]:
        kb.barrier()
        yT = nc.dram_tensor("yT", [D, T], F32, kind="ExternalOutput").ap()
        load_x_norm(c, cur, None, SMO["g_final"], 0, T, out_dram=yT)
    kb.barrier()
    print("instructions", kb.ninst, "waits", kb.nwaits)
    return nc


_NC_CACHE = {}


def kernel(**inputs):
    inp = {k: np.asarray(v) for k, v in inputs.items()}
    plan = {(s, l) for s in STAGES for l in range(DEPTH)} | {("final", DEPTH)}
    if "nc" not in _NC_CACHE:
        _NC_CACHE["nc"] = build(plan)
    nc = _NC_CACHE["nc"]
    sm, rw = host_tables(inp)
    shared = dict(sm_in=sm, rw_in=rw, **host_consts())
    per_layer = ["w_ffn1_in", "w_ffn1_out", "w_in", "pool_w", "sg_w", "w_gate", "w_branch", "w_out",
                 "w_mem_q", "w_mem_kv", "w_mem_o", "w_ffn2_in", "w_ffn2_out"]
    for l in range(DEPTH):
        for n in per_layer:
            shared["%s_%d" % (n, l)] = np.ascontiguousarray(inp[n][l], dtype=np.float32)
    B = inp["x"].shape[0]
    in_maps = []
    for b in range(B):
        m = dict(shared)
        m["xT"] = np.ascontiguousarray(inp["x"][b].T, dtype=np.float32)
        m["memT"] = np.ascontiguousarray(inp["mem"][b].T, dtype=np.float32)
        in_maps.append(m)
    res = run_bass_kernel_spmd(nc, in_maps, core_ids=list(range(B)))
    out = np.stack([np.ascontiguousarray(res.results[b]["yT"].T) for b in range(B)], axis=0)
    return out.astype(np.float32)
```

```python
import math
import numpy as np
import concourse.bass as bass
import concourse.mybir as mybir
from concourse.bass_utils import run_bass_kernel_spmd

F32 = mybir.dt.float32
BF16 = mybir.dt.bfloat16
AF = mybir.ActivationFunctionType
ALU = mybir.AluOpType
AX = mybir.AxisListType

D = 4096
T = 2048
FF = 8192
DC = D // 128
EPS = 1e-6
DEPTH = 2
MEM = 256
NIN = 9304
POOL_WINDOWS = (2, 4, 8, 16)
NEG = -1.0e30


class Res:
    __slots__ = ("name", "writers", "readers")

    def __init__(self, name=""):
        self.name = name
        self.writers = {}
        self.readers = {}


class KB:
    def __init__(self, nc):
        self.nc = nc
        self.E = dict(pe=nc.tensor, act=nc.scalar, dve=nc.vector, pool=nc.gpsimd, sp=nc.sync)
        self.sems = {}
        self.cnt = {}
        for e in ("pe", "act", "dve", "pool"):
            k = "e_" + e
            self.sems[k] = nc.alloc_semaphore(k)
            self.cnt[k] = 0
        self.seen = {e: {} for e in self.E}
        self.dq = {}
        for q, n in (("sp", 10), ("pool", 8), ("act", 4)):
            keys = []
            for i in range(n):
                k = "d_%s%d" % (q, i)
                self.sems[k] = nc.alloc_semaphore(k)
                self.cnt[k] = 0
                keys.append(k)
            self.dq[q] = [keys, 0]
        self.nwaits = 0
        self.ninst = 0

    def wait(self, eng, key, val):
        if self.seen[eng].get(key, 0) >= val:
            return
        self.E[eng].wait_ge(self.sems[key], val)
        self.seen[eng][key] = val
        self.nwaits += 1

    def _deps(self, eng, reads, writes, pwrites, own):
        for r in reads:
            for k, v in r.writers.items():
                self.wait(eng, k, v)
        for w in writes:
            for k, v in w.writers.items():
                if k != own:
                    self.wait(eng, k, v)
            for k, v in w.readers.items():
                if k != own:
                    self.wait(eng, k, v)
        for w in pwrites:
            if w.readers:
                for k, v in w.readers.items():
                    if k != own:
                        self.wait(eng, k, v)
                w.readers = {}
                w.writers = {}

    def _commit(self, key, val, reads, writes, pwrites):
        for r in reads:
            if r.readers.get(key, 0) < val:
                r.readers[key] = val
        for w in writes:
            w.writers = {key: val}
            w.readers = {}
        for w in pwrites:
            if w.writers.get(key, 0) < val:
                w.writers[key] = val

    def op(self, eng, fn, reads=(), writes=(), pwrites=()):
        own = "e_" + eng
        self._deps(eng, reads, writes, pwrites, own)
        inst = fn()
        self.cnt[own] += 1
        inst.then_inc(self.sems[own], 1)
        self._commit(own, self.cnt[own], reads, writes, pwrites)
        self.ninst += 1
        return inst

    def dma(self, q, out, in_, reads=(), writes=(), pwrites=()):
        self._deps(q, reads, writes, pwrites, None)
        keys, rr = self.dq[q]
        key = keys[rr % len(keys)]
        self.dq[q][1] = rr + 1
        if self.cnt[key] > 0:
            self.wait(q, key, self.cnt[key])
        inst = self.E[q].dma_start(out=out, in_=in_)
        self.cnt[key] += 16
        inst.then_inc(self.sems[key], 16)
        self._commit(key, self.cnt[key], reads, writes, pwrites)
        self.ninst += 1
        return inst

    def barrier(self):
        for eng in self.E:
            for key, val in self.cnt.items():
                if val > 0:
                    self.wait(eng, key, val)


class Ctx:
    pass


def rlist(name, n):
    return [Res("%s%d" % (name, i)) for i in range(n)]


def v3(ap, a=2):
    return ap.rearrange("p (a b) -> p a b", a=a)


SMW = [("g_ffn1", 32), ("g_mix", 32), ("g_cross", 32), ("g_ffn2", 32), ("pool_scale", 16), ("sg_ln_g", 8),
       ("sg_ln_b", 8), ("conv_w0", 16), ("conv_w1", 16), ("conv_w2", 16), ("conv_w3", 16), ("conv_b", 16),
       ("ssm_norm_g", 8), ("g_final", 32), ("g_mem", 32)]
SMO = {}
_o = 0
for _n, _w in SMW:
    SMO[_n] = _o
    _o += _w
NSM = _o
RWW = [("dt_bias", 16), ("a_log", 16), ("ssm_d", 16), ("sg_b", 512)]
RWO = {}
_o = 0
for _n, _w in RWW:
    RWO[_n] = _o
    _o += _w
NRW = _o


def host_tables(inp):
    sm = np.zeros((DEPTH, 128, NSM), np.float32)
    rw = np.zeros((DEPTH, 128, NRW), np.float32)

    def pp(v):
        v = np.asarray(v, np.float32).reshape(-1)
        return v.reshape(-1, 128).T

    for l in range(DEPTH):
        for n in ("g_ffn1", "g_mix", "g_cross", "g_ffn2", "sg_ln_g", "sg_ln_b", "ssm_norm_g"):
            a = pp(inp[n][l])
            sm[l, :, SMO[n]:SMO[n] + a.shape[1]] = a
        sm[l, :, SMO["pool_scale"]:SMO["pool_scale"] + 16] = pp(inp["pool_scale"][l])
        for j in range(4):
            sm[l, :, SMO["conv_w%d" % j]:SMO["conv_w%d" % j] + 16] = pp(inp["ssm_conv_w"][l, j])
        sm[l, :, SMO["conv_b"]:SMO["conv_b"] + 16] = pp(inp["ssm_conv_b"][l])
        sm[l, :, SMO["g_final"]:SMO["g_final"] + 32] = pp(inp["g_final"])
        sm[l, :, SMO["g_mem"]:SMO["g_mem"] + 32] = pp(inp["g_mem"])
        rw[l, :, RWO["dt_bias"]:RWO["dt_bias"] + 16] = np.asarray(inp["ssm_dt_bias"][l])[None, :]
        rw[l, :, RWO["a_log"]:RWO["a_log"] + 16] = np.asarray(inp["ssm_a_log"][l])[None, :]
        rw[l, :, RWO["ssm_d"]:RWO["ssm_d"] + 16] = np.asarray(inp["ssm_d"][l])[None, :]
        rw[l, :, RWO["sg_b"]:RWO["sg_b"] + 512] = np.asarray(inp["sg_b"][l]).reshape(-1)[None, :]
    return sm, rw


M_ONES, M_ID, M_R128, M_R64, M_SG, M_UT, M_SL, M_SEL = range(8)


def host_consts():
    mats = np.zeros((8, 128, 128), np.float32)
    mats[M_ONES] = 1.0
    mats[M_ID] = np.eye(128)
    r = np.zeros((128, 128), np.float32)
    for d in range(64):
        r[d + 64, d] = -1.0
        r[d, d + 64] = 1.0
    mats[M_R128] = r
    r = np.zeros((128, 128), np.float32)
    for b in range(2):
        for d in range(32):
            r[b * 64 + d + 32, b * 64 + d] = -1.0
            r[b * 64 + d, b * 64 + d + 32] = 1.0
    mats[M_R64] = r
    i = np.arange(128)
    mats[M_SG] = ((i[:, None] // 64) >= (i[None, :] // 64)).astype(np.float32)
    j = np.arange(64)
    mats[M_UT, :64, :64] = (j[:, None] <= j[None, :]).astype(np.float32)
    mats[M_SL, :64, :64] = (j[:, None] > j[None, :]).astype(np.float32)
    mats[M_SEL, 63, :] = 1.0
    pos = np.arange(T, dtype=np.float32)
    rope = np.zeros((4, 128, T), np.float32)
    inv = (10000.0 ** (-np.arange(64, dtype=np.float32) / 64)).astype(np.float32)
    ang = pos[None, :] * inv[:, None]
    rope[0, :64] = np.cos(ang)
    rope[0, 64:] = np.cos(ang)
    rope[1, :64] = np.sin(ang)
    rope[1, 64:] = np.sin(ang)
    inv = (10000.0 ** (-np.arange(32, dtype=np.float32) / 32)).astype(np.float32)
    ang = pos[None, :] * inv[:, None]
    for b in range(4):
        rope[2, b * 32:(b + 1) * 32] = np.cos(ang)
        rope[3, b * 32:(b + 1) * 32] = np.sin(ang)
    cf = np.zeros((128, 80), np.float32)
    cf[:, 0] = EPS
    cf[:, 1] = -1.0e29
    for g, w in enumerate(POOL_WINDOWS):
        cf[:, 16 + g * 16:32 + g * 16] = 1.0 / np.minimum(w, np.arange(16) + 1.0)
    return dict(c_mats=mats, c_rope=rope, c_f32=cf)


def setup(nc, kb):
    c = Ctx()
    c.nc = nc
    c.kb = kb
    c.XA = nc.alloc_sbuf_tensor("XA", [128, 65536], BF16)
    c.XA_res = Res("XA")
    c.NW = 3
    c.WB = [nc.alloc_sbuf_tensor("WB%d" % i, [128, 8192], BF16) for i in range(c.NW)]
    c.WB_res = rlist("WB", c.NW)
    c.wrr = 0
    c.NS = 5
    c.ST = [nc.alloc_sbuf_tensor("ST%d" % i, [128, 1024], F32) for i in range(c.NS)]
    c.ST_res = rlist("ST", c.NS)
    c.srr = 0
    c.PS = nc.alloc_psum_tensor("PS", [128, 8, 512], F32)
    c.PS_res = rlist("PS", 8)
    c.mb = nc.alloc_sbuf_tensor("mats_bf", [128, 4, 128], BF16)
    c.mf = nc.alloc_sbuf_tensor("mats_f", [128, 4, 128], F32)
    c.cf = nc.alloc_sbuf_tensor("cf", [128, 80], F32)
    c.const_res = Res("const")
    c.sm = nc.alloc_sbuf_tensor("smt", [128, NSM], F32)
    c.sm_res = Res("sm")
    c.rw = nc.alloc_sbuf_tensor("rwt", [128, NRW], F32)
    c.rw_res = Res("rw")
    c.kvm = nc.alloc_sbuf_tensor("kvm", [128, 2048], BF16)
    c.ones = c.mb[:, 0, :]
    c.ident = c.mb[:, 1, :]
    c.rstd = c.WB[0][:, 0:4096].bitcast(F32)
    c.rstd_res = c.WB_res[0]
    return c


def stage_tile(c):
    i = c.srr % c.NS
    c.srr += 1
    return c.ST[i], c.ST_res[i]


def xa(c, off, shape, dt, parts=128, p0=0):
    n = 1
    for s in shape:
        n *= s
    esz = 2 if dt == BF16 else 4
    assert off % 4 == 0 and off + n * esz <= 131072, (off, shape)
    base = c.XA[p0:p0 + parts, off // 2: off // 2 + n * esz // 2]
    ap = base if dt == BF16 else base.bitcast(dt)
    if len(shape) == 2:
        ap = ap.rearrange("p (a b) -> p a b", a=shape[0])
    elif len(shape) == 3:
        ap = ap.rearrange("p (a b c) -> p a b c", a=shape[0], b=shape[1])
    return ap


def load_x_norm(c, src, src_res, gcol, t0, Tn, out_dram=None):
    kb, nc = c.kb, c.nc
    X = c.XA[:, 0:DC * Tn].rearrange("p (k t) -> p k t", k=DC)
    TS = min(Tn, 1024)
    nsub = Tn // TS
    nbk = TS // 512 if TS >= 512 else 1
    bw = min(512, TS)

    def rd(ch):
        return [src_res[ch]] if src_res is not None else []

    fused = out_dram is None
    it = 0
    for ch in range(DC):
        for hf in range(nsub):
            st, st_r = stage_tile(c)
            kb.dma("sp", st[:, 0:TS], src[ch * 128:(ch + 1) * 128, t0 + hf * TS:t0 + (hf + 1) * TS],
                   reads=rd(ch), writes=[st_r])
            sq, sq_r = stage_tile(c)
            sqb = sq[:, 0:512].bitcast(BF16)
            kb.op("act", lambda: nc.scalar.activation(out=sqb[:, 0:TS], in_=st[:, 0:TS], func=AF.Square),
                  reads=[st_r], writes=[sq_r])
            if fused:
                it += 1
                if it % 2 == 0:
                    kb.op("dve", lambda: nc.vector.tensor_scalar(out=X[:, ch, hf * TS:(hf + 1) * TS], in0=st[:, 0:TS], scalar1=c.sm[:, gcol + ch:gcol + ch + 1],
                                                                 scalar2=None, op0=ALU.mult),
                          reads=[st_r, c.sm_res], pwrites=[c.XA_res])
                else:
                    kb.op("act", lambda: nc.scalar.activation(out=X[:, ch, hf * TS:(hf + 1) * TS], in_=st[:, 0:TS], func=AF.Copy,
                                                              scale=c.sm[:, gcol + ch:gcol + ch + 1]),
                          reads=[st_r, c.sm_res], pwrites=[c.XA_res])
            for j in range(nbk):
                b = hf * nbk + j
                kb.op("pe", lambda: nc.tensor.matmul(c.PS[:, b, 0:bw], c.ones, sqb[:, j * bw:(j + 1) * bw],
                                                     start=(ch == 0), stop=(ch == DC - 1)),
                      reads=[sq_r, c.const_res], writes=[c.PS_res[b]])
    for b in range(nsub * nbk):
        kb.op("act", lambda: nc.scalar.activation(out=c.rstd[:, b * bw:(b + 1) * bw], in_=c.PS[:, b, 0:bw], func=AF.Sqrt,
                                                  bias=c.cf[:, 0:1], scale=1.0 / D),
              reads=[c.PS_res[b], c.const_res], writes=[c.rstd_res])
    kb.op("dve", lambda: nc.vector.reciprocal(out=c.rstd[:, 0:Tn], in_=c.rstd[:, 0:Tn]),
          reads=[c.rstd_res], writes=[c.rstd_res])
    if fused:
        xw = Res("xnorm")
        it = 0
        for ch in range(DC):
            kb.op("dve", lambda: nc.vector.tensor_tensor(out=X[:, ch, :], in0=X[:, ch, :], in1=c.rstd[:, 0:Tn], op=ALU.mult),
                  reads=[c.rstd_res, c.XA_res], pwrites=[xw])
        c.XA_res.writers = dict(xw.writers)
        c.XA_res.readers = {}
        return X
    for ch in range(DC):
        for hf in range(nsub):
            st, st_r = stage_tile(c)
            kb.dma("sp", st[:, 0:TS], src[ch * 128:(ch + 1) * 128, t0 + hf * TS:t0 + (hf + 1) * TS],
                   reads=rd(ch), writes=[st_r])
            if out_dram is None:
                kb.op("dve", lambda: nc.vector.scalar_tensor_tensor(out=X[:, ch, hf * TS:(hf + 1) * TS], in0=st[:, 0:TS],
                                                                    scalar=c.sm[:, gcol + ch:gcol + ch + 1],
                                                                    in1=c.rstd[:, hf * TS:(hf + 1) * TS],
                                                                    op0=ALU.mult, op1=ALU.mult),
                      reads=[st_r, c.rstd_res, c.sm_res], pwrites=[c.XA_res])
            else:
                so, so_r = stage_tile(c)
                kb.op("dve", lambda: nc.vector.scalar_tensor_tensor(out=so[:, 0:TS], in0=st[:, 0:TS],
                                                                    scalar=c.sm[:, gcol + ch:gcol + ch + 1],
                                                                    in1=c.rstd[:, hf * TS:(hf + 1) * TS],
                                                                    op0=ALU.mult, op1=ALU.mult),
                      reads=[st_r, c.rstd_res, c.sm_res], writes=[so_r])
                kb.dma("sp", out_dram[ch * 128:(ch + 1) * 128, t0 + hf * TS:t0 + (hf + 1) * TS], so[:, 0:TS],
                       reads=[so_r])
    return X


def load_x_plain(c, src, src_res, KC, t0, Tn, off=0):
    kb = c.kb
    X = xa(c, off, [KC, Tn], BF16)
    for k in range(KC):
        kb.dma("sp", X[:, k, :], src[k * 128:(k + 1) * 128, t0:t0 + Tn],
               reads=[src_res[k]] if src_res is not None else [], pwrites=[c.XA_res])
    return X


def gemm(c, wloads, units, wbufs=None):
    for _ in gemm_iter(c, wloads, units, wbufs):
        pass


def gemm_iter(c, wloads, units, wbufs=None):
    kb, nc = c.kb, c.nc
    W = {}
    priv = {"rr": 0}
    NWB = c.NW if wbufs is None else len(wbufs)

    def issue_w(wi):
        if wbufs is None:
            i = c.wrr % c.NW
            c.wrr += 1
            wb, wr = c.WB[i], c.WB_res[i]
        else:
            wb, wr = wbufs[priv["rr"] % len(wbufs)]
            priv["rr"] += 1
        off = 0
        views = []
        first = True
        for (wd, r0, kc, c0, cw) in wloads[wi]:
            assert off + kc * cw <= wb.shape[1], (off, kc, cw)
            Wv = wb[:, off:off + kc * cw].rearrange("p (k n) -> p k n", k=kc)
            kstep = max(1, min(kc, 2048 // cw))
            for k0 in range(0, kc, kstep):
                kk = min(kstep, kc - k0)
                src = wd[r0 + k0 * 128:r0 + (k0 + kk) * 128, c0:c0 + cw].rearrange("(k p) n -> p k n", p=128)
                if first:
                    kb.dma("pool", Wv[:, k0:k0 + kk, :], src, writes=[wr])
                    first = False
                else:
                    kb.dma("pool", Wv[:, k0:k0 + kk, :], src, pwrites=[wr])
            views.append(Wv)
            off += kc * cw
        W[wi] = (views, wr)

    nxt = 0
    while nxt < min(NWB, len(wloads)):
        issue_w(nxt)
        nxt += 1
    lastw = -1
    for u in units:
        wi = u["w"]
        if wi != lastw:
            if wi >= 1 and nxt < len(wloads) and nxt <= wi + NWB - 1:
                issue_w(nxt)
                nxt += 1
            lastw = wi
        views, wr = W[wi]
        if "pre" in u:
            u["pre"](u)
        for a in u["accs"]:
            Wv = views[a["piece"]]
            kc = wloads[wi][a["piece"]][2]
            X, xres = a["X"], a["xres"]
            wo, ww, t0, tw, bank = a["wo"], a["ww"], a["t0"], a["tw"], a["bank"]
            k0 = a.get("k0", 0)
            k1 = k0 + a.get("kn", kc - k0)
            for k in range(k0, k1):
                if a["kind"] == "fm":
                    kb.op("pe", lambda: nc.tensor.matmul(c.PS[0:ww, bank, 0:tw], Wv[:, k, wo:wo + ww], X[:, k, t0:t0 + tw],
                                                         start=(k == k0), stop=(k == k1 - 1)),
                          reads=[wr, xres], writes=[c.PS_res[bank]])
                else:
                    kb.op("pe", lambda: nc.tensor.matmul(c.PS[0:tw, bank, 0:ww], X[:, k, t0:t0 + tw], Wv[:, k, wo:wo + ww],
                                                         start=(k == k0), stop=(k == k1 - 1)),
                          reads=[wr, xres], writes=[c.PS_res[bank]])
        u["epi"](u)
        yield 1


def fm_acc(piece, X, xres, t0, tw, bank, wo=0, ww=128):
    return dict(kind="fm", piece=piece, X=X, xres=xres, t0=t0, tw=tw, bank=bank, wo=wo, ww=ww)


def residual_units(c, X, KC, wd, xsrc, xsrc_res, xdst, xdst_res, Tn, alpha, tok0):
    kb, nc = c.kb, c.nc
    units, wloads = [], []
    for nb in range(DC):
        pb = (nb % 4) * 2
        wloads.append([(wd, 0, KC, nb * 128, 128)])
        hold = {}

        def pre(u, nb=nb, hold=hold):
            xo, xo_r = stage_tile(c)
            kb.dma("sp", xo[:, :], xsrc[nb * 128:(nb + 1) * 128, tok0:tok0 + Tn],
                   reads=[xsrc_res[nb]] if xsrc_res is not None else [], writes=[xo_r])
            hold["xo"] = (xo, xo_r)

        def epi(u, nb=nb, pb=pb, hold=hold):
            xo, xo_r = hold["xo"]
            xn, xn_r = stage_tile(c)
            kb.op("dve", lambda: nc.vector.scalar_tensor_tensor(out=v3(xn[:, :]), in0=c.PS[:, pb:pb + 2, :], scalar=alpha,
                                                                in1=v3(xo[:, :]), op0=ALU.mult, op1=ALU.add),
                  reads=[xo_r, c.PS_res[pb], c.PS_res[pb + 1]], writes=[xn_r])
            kb.dma("sp", xdst[nb * 128:(nb + 1) * 128, tok0:tok0 + Tn], xn[:, :], reads=[xn_r],
                   pwrites=[xdst_res[nb]] if xdst_res is not None else [])

        accs = [fm_acc(0, X, c.XA_res, 0, 512, pb), fm_acc(0, X, c.XA_res, 512, 512, pb + 1)]
        units.append(dict(w=nb, accs=accs, epi=epi, pre=pre))
    return wloads, units


def ffn(c, xsrc, xdst, gcol, w_in, w_out, HT, HT_res):
    kb, nc = c.kb, c.nc
    xr = rlist("xr", DC)
    X = load_x_norm(c, xsrc, None, gcol, 0, T)
    units = []
    wloads = []
    for j in range(FF // 128):
        wloads.append([(w_in, 0, DC, j * 128, 128), (w_in, 0, DC, FF + j * 128, 128)])
        for th in range(2):
            pb = ((j * 2 + th) % 2) * 4

            def epi(u, j=j, th=th, pb=pb):
                sg, sg_r = stage_tile(c)
                kb.op("act", lambda: nc.scalar.activation(out=v3(sg[:, :]), in_=c.PS[:, pb:pb + 2, :], func=AF.Silu),
                      reads=[c.PS_res[pb], c.PS_res[pb + 1]], writes=[sg_r])
                hh, hh_r = stage_tile(c)
                hb = hh[:, 0:512].bitcast(BF16)
                kb.op("dve", lambda: nc.vector.tensor_tensor(out=v3(hb), in0=v3(sg[:, :]), in1=c.PS[:, pb + 2:pb + 4, :], op=ALU.mult),
                      reads=[sg_r, c.PS_res[pb + 2], c.PS_res[pb + 3]], writes=[hh_r])
                kb.dma("sp", HT[j * 128:(j + 1) * 128, th * 1024:(th + 1) * 1024], hb, reads=[hh_r], pwrites=[HT_res[j]])

            accs = [fm_acc(0, X, c.XA_res, th * 1024, 512, pb), fm_acc(0, X, c.XA_res, th * 1024 + 512, 512, pb + 1),
                    fm_acc(1, X, c.XA_res, th * 1024, 512, pb + 2), fm_acc(1, X, c.XA_res, th * 1024 + 512, 512, pb + 3)]
            units.append(dict(w=j, accs=accs, epi=epi))
    gemm(c, wloads, units)
    for th in range(2):
        X2 = load_x_plain(c, HT, HT_res, FF // 128, th * 1024, 1024)
        wl, un = residual_units(c, X2, FF // 128, w_out, xsrc, xr, xdst, xr, 1024, 0.5, th * 1024)
        gemm(c, wl, un)


def stage_win(c, xsrc, gcol, w_in, CB, HN, DTW):
    kb, nc = c.kb, c.nc
    X = load_x_norm(c, xsrc, None, gcol, 0, T)
    for k in range(DC):
        kb.dma("sp", HN[k * 128:(k + 1) * 128, :], X[:, k, :], reads=[c.XA_res])
    segs = []
    two = [(0, 128), (128, 128)]
    for i in range(8):
        segs.append((i * 256, 256, two, "copy"))
    for i in range(4):
        segs.append((2048 + i * 256, 256, two, "gelu"))
    for i in range(4):
        segs.append((3072 + i * 256, 256, two, "gelu"))
    for i in range(4):
        segs.append((4096 + i * 256, 256, two, "silu"))
    for i in range(8):
        segs.append((5120 + i * 256, 256, two, "copy"))
    for i in range(4):
        segs.append((7184 + i * 256, 256, two, "copy"))
    segs.append((8208, 256, two, "copy"))
    segs.append((8464, 256, two, "copy"))
    for i in range(2):
        segs.append((8720 + i * 256, 256, [(0, 64), (64, 64), (128, 64), (192, 64)], "copy"))
    segs.append((9232, 64, [(0, 64)], "copy"))
    wloads, units = [], []
    un = 0
    for (c0, cw, subs, func) in segs:
        wloads.append([(w_in, 0, DC, c0, cw)])
        wi = len(wloads) - 1
        for (wo, ww) in subs:
            for th in range(2):
                pb = (un % 4) * 2
                un += 1

                def epi(u, c0=c0, wo=wo, ww=ww, th=th, pb=pb, func=func, un=un):
                    st, st_r = stage_tile(c)
                    ob = st[:, 0:512].bitcast(BF16)
                    if func == "copy" and un % 2 == 0:
                        kb.op("dve", lambda: nc.vector.tensor_copy(out=v3(ob[0:ww, :]), in_=c.PS[0:ww, pb:pb + 2, :]),
                              reads=[c.PS_res[pb], c.PS_res[pb + 1]], writes=[st_r])
                    else:
                        f = {"copy": AF.Copy, "gelu": AF.Gelu, "silu": AF.Silu}[func]
                        kb.op("act", lambda: nc.scalar.activation(out=v3(ob[0:ww, :]), in_=c.PS[0:ww, pb:pb + 2, :], func=f),
                              reads=[c.PS_res[pb], c.PS_res[pb + 1]], writes=[st_r])
                    kb.dma("sp", CB[c0 + wo:c0 + wo + ww, th * 1024:(th + 1) * 1024], ob[0:ww, :], reads=[st_r])

                accs = [fm_acc(0, X, c.XA_res, th * 1024, 512, pb, wo, ww), fm_acc(0, X, c.XA_res, th * 1024 + 512, 512, pb + 1, wo, ww)]
                units.append(dict(w=wi, accs=accs, epi=epi))
    wloads.append([(w_in, 0, DC, 7168, 16), (w_in, 0, DC, 9296, 8)])
    wi = len(wloads) - 1
    for tp in range(8):
        pb = (tp % 2) * 4

        def epi(u, tp=tp, pb=pb):
            for j in range(2):
                tb = tp * 2 + j
                st, st_r = stage_tile(c)
                kb.op("dve", lambda: nc.vector.tensor_copy(out=st[:, 0:16], in_=c.PS[:, pb + 2 * j, 0:16]),
                      reads=[c.PS_res[pb + 2 * j]], writes=[st_r])
                kb.op("dve", lambda: nc.vector.tensor_copy(out=st[:, 16:24], in_=c.PS[:, pb + 2 * j + 1, 0:8]),
                      reads=[c.PS_res[pb + 2 * j + 1]], pwrites=[st_r])
                kb.dma("sp", DTW[tb * 128:(tb + 1) * 128, :], st[:, 0:24], reads=[st_r])

        accs = []
        for j in range(2):
            tb = tp * 2 + j
            accs.append(dict(kind="tm", piece=0, X=X, xres=c.XA_res, t0=tb * 128, tw=128, bank=pb + 2 * j, wo=0, ww=16))
            accs.append(dict(kind="tm", piece=1, X=X, xres=c.XA_res, t0=tb * 128, tw=128, bank=pb + 2 * j + 1, wo=0, ww=8))
        units.append(dict(w=wi, accs=accs, epi=epi))
    gemm(c, wloads, units)


def stage_pool(c, CB, YT, pool_w, gates=None):
    kb, nc = c.kb, c.nc
    PW = 2048 + 16
    P = [xa(c, 0, [PW], F32), xa(c, PW * 4, [PW], F32)]
    P_r = [Res("P0"), Res("P1")]
    XP = xa(c, 2 * PW * 4, [4, T], BF16)
    XP_r = Res("XP")
    PWB = [(xa(c, 32896, [2048], BF16), Res("PWB0")), (xa(c, 36992, [2048], BF16), Res("PWB1"))]
    for i in range(2):
        kb.op("dve", lambda: nc.vector.memset(P[i][:, 0:16], 0.0), pwrites=[P_r[i]])
    for g in range(4):
        w = POOL_WINDOWS[g]
        nsteps = int(math.log2(w))
        for cc in range(4):
            ch = g * 4 + cc
            ab, ab_r = stage_tile(c)
            abf = ab[:, :].bitcast(BF16)
            kb.dma("sp", abf, CB[ch * 128:(ch + 1) * 128, :], writes=[ab_r])
            kb.op("act", lambda: nc.scalar.activation(out=P[0][:, 16:PW], in_=abf, func=AF.Copy),
                  reads=[ab_r], pwrites=[P_r[0]])
            cur = 0
            sh = 1
            for s in range(nsteps):
                nx = 1 - cur
                eng = "dve" if s % 2 == 0 else "pool"
                E = nc.vector if eng == "dve" else nc.gpsimd
                kb.op(eng, lambda: E.tensor_tensor(out=P[nx][:, 16:PW], in0=P[cur][:, 16:PW], in1=P[cur][:, 16 - sh:PW - sh], op=ALU.add),
                      reads=[P_r[cur]], pwrites=[P_r[nx]])
                cur = nx
                sh *= 2
            kb.op("dve", lambda: nc.vector.scalar_tensor_tensor(out=XP[:, cc, :], in0=P[cur][:, 16:PW], scalar=1.0 / w, in1=abf,
                                                                op0=ALU.mult, op1=ALU.subtract),
                  reads=[P_r[cur], ab_r], pwrites=[XP_r])
            t16, t16_r = stage_tile(c)
            kb.op("dve", lambda: nc.vector.tensor_tensor(out=t16[:, 0:16], in0=P[cur][:, 16:32], in1=c.cf[:, 16 + g * 16:32 + g * 16], op=ALU.mult),
                  reads=[P_r[cur], c.const_res], writes=[t16_r])
            kb.op("dve", lambda: nc.vector.tensor_tensor(out=XP[:, cc, 0:16], in0=t16[:, 0:16], in1=abf[:, 0:16], op=ALU.subtract),
                  reads=[t16_r, ab_r], pwrites=[XP_r])
        if gates is not None:
            gates.step(6)
        wloads = [[(pool_w[g], 0, 4, 0, 512)]]
        units = []
        for nb in range(4):
            for th in range(2):
                pb = ((nb * 2 + th) % 3) * 2

                def epi(u, nb=nb, th=th, pb=pb, g=g):
                    st, st_r = stage_tile(c)
                    ob = st[:, 0:512].bitcast(BF16)
                    col = SMO["pool_scale"] + g * 4 + nb
                    kb.op("act", lambda: nc.scalar.activation(out=v3(ob), in_=c.PS[:, pb:pb + 2, :], func=AF.Copy, scale=c.sm[:, col:col + 1]),
                          reads=[c.PS_res[pb], c.PS_res[pb + 1], c.sm_res], writes=[st_r])
                    kb.dma("sp", YT[g * 512 + nb * 128:g * 512 + (nb + 1) * 128, th * 1024:(th + 1) * 1024], ob, reads=[st_r])

                accs = [fm_acc(0, XP, XP_r, th * 1024, 512, pb, nb * 128, 128), fm_acc(0, XP, XP_r, th * 1024 + 512, 512, pb + 1, nb * 128, 128)]
                units.append(dict(w=0, accs=accs, epi=epi))
        gemm(c, wloads, units, wbufs=PWB)


def stage_sg(c, CB, YT, sg_w):
    kb, nc = c.kb, c.nc
    V = xa(c, 0, [8, T], BF16)
    V_r = rlist("V", 8)
    mean = xa(c, 32768, [T], F32)
    rstd = xa(c, 40960, [T], F32)
    nb_ = xa(c, 49152, [T], F32)
    st_r = Res("stats")
    VT = xa(c, 57344, [16, 1024], BF16)
    VT_r = rlist("VT", 16)
    WmT = xa(c, 90112, [4, 128], BF16)
    WmT_r = Res("WmT")
    tA = xa(c, 91136, [T], F32)
    tA_r = Res("tA")
    tB = xa(c, 99328, [T], F32)
    tB_r = Res("tB")
    sgb = xa(c, 107520, [512], BF16)
    sgb_r = Res("sgb")
    OUTB = xa(c, 108544, [T], BF16)
    OUTB_r = Res("OUTB")
    UB = xa(c, 112640, [T], BF16)
    UB_r = Res("UB")
    kb.op("act", lambda: nc.scalar.activation(out=sgb, in_=c.rw[:, RWO["sg_b"]:RWO["sg_b"] + 512], func=AF.Copy),
          reads=[c.rw_res], writes=[sgb_r])
    for cch in range(8):
        kb.dma("sp", V[:, cch, :], CB[3072 + cch * 128:3072 + (cch + 1) * 128, :], writes=[V_r[cch]])
        sq, sq_r = stage_tile(c)
        sqb = sq[:, :].bitcast(BF16)
        kb.op("act", lambda: nc.scalar.activation(out=sqb, in_=V[:, cch, :], func=AF.Square), reads=[V_r[cch]], writes=[sq_r])
        for j in range(4):
            kb.op("pe", lambda: nc.tensor.matmul(c.PS[:, j, :], c.ones, V[:, cch, j * 512:(j + 1) * 512], start=(cch == 0), stop=(cch == 7)),
                  reads=[V_r[cch], c.const_res], writes=[c.PS_res[j]])
            kb.op("pe", lambda: nc.tensor.matmul(c.PS[:, 4 + j, :], c.ones, sqb[:, j * 512:(j + 1) * 512], start=(cch == 0), stop=(cch == 7)),
                  reads=[sq_r, c.const_res], writes=[c.PS_res[4 + j]])
    kb.op("act", lambda: nc.scalar.activation(out=v3(mean, 4), in_=c.PS[:, 0:4, :], func=AF.Copy, scale=1.0 / 1024),
          reads=c.PS_res[0:4], writes=[st_r])
    kb.op("dve", lambda: nc.vector.tensor_tensor(out=tA, in0=mean, in1=mean, op=ALU.mult), reads=[st_r], writes=[tA_r])
    kb.op("dve", lambda: nc.vector.scalar_tensor_tensor(out=v3(tB, 4), in0=c.PS[:, 4:8, :], scalar=1.0 / 1024, in1=v3(tA, 4),
                                                        op0=ALU.mult, op1=ALU.subtract),
          reads=c.PS_res[4:8] + [tA_r], writes=[tB_r])
    kb.op("act", lambda: nc.scalar.activation(out=rstd, in_=tB, func=AF.Sqrt, bias=c.cf[:, 0:1], scale=1.0),
          reads=[tB_r, c.const_res, st_r], writes=[st_r])
    kb.op("dve", lambda: nc.vector.reciprocal(out=rstd, in_=rstd), reads=[st_r], writes=[st_r])
    kb.op("dve", lambda: nc.vector.scalar_tensor_tensor(out=nb_, in0=mean, scalar=-1.0, in1=rstd, op0=ALU.mult, op1=ALU.mult),
          reads=[st_r], writes=[st_r])
    for cch in range(8):
        kb.op("dve", lambda: nc.vector.tensor_tensor(out=tA, in0=V[:, cch, :], in1=rstd, op=ALU.mult),
              reads=[V_r[cch], st_r], writes=[tA_r])
        kb.op("pool", lambda: nc.gpsimd.tensor_tensor(out=tB, in0=tA, in1=nb_, op=ALU.add), reads=[tA_r, st_r], writes=[tB_r])
        cg = SMO["sg_ln_g"] + cch
        cb = SMO["sg_ln_b"] + cch
        kb.op("act", lambda: nc.scalar.activation(out=V[:, cch, :], in_=tB, func=AF.Identity, scale=c.sm[:, cg:cg + 1], bias=c.sm[:, cb:cb + 1]),
              reads=[tB_r, c.sm_res], writes=[V_r[cch]])
    for tb in range(16):
        bank = tb % 2
        psb = c.PS[:, bank, :].bitcast(BF16)
        for cch in range(8):
            kb.op("pe", lambda: nc.tensor.transpose(psb[:, cch * 128:(cch + 1) * 128], V[:, cch, tb * 128:(tb + 1) * 128], c.ident),
                  reads=[V_r[cch], c.const_res], writes=[c.PS_res[bank]])
        if tb % 2 == 0:
            kb.op("act", lambda: nc.scalar.activation(out=VT[:, tb, :], in_=psb, func=AF.Copy), reads=[c.PS_res[bank]], writes=[VT_r[tb]])
        else:
            kb.op("dve", lambda: nc.vector.tensor_copy(out=VT[:, tb, :], in_=psb), reads=[c.PS_res[bank]], writes=[VT_r[tb]])
    for g in range(4):
        wst, wst_r = stage_tile(c)
        kb.dma("sp", wst[:, 0:128], sg_w[g], writes=[wst_r])
        wm, wm_r = stage_tile(c)
        wmb = wm[:, 0:64].bitcast(BF16)
        kb.op("dve", lambda: nc.vector.tensor_tensor(out=wmb, in0=wst[:, 0:128], in1=c.mf[:, 0, :], op=ALU.mult),
              reads=[wst_r, c.const_res], writes=[wm_r])
        psb = c.PS[:, 2, :].bitcast(BF16)
        kb.op("pe", lambda: nc.tensor.transpose(psb[:, 0:128], wmb, c.ident), reads=[wm_r, c.const_res], writes=[c.PS_res[2]])
        kb.op("act", lambda: nc.scalar.activation(out=WmT[:, g, :], in_=psb[:, 0:128], func=AF.Copy), reads=[c.PS_res[2]], pwrites=[WmT_r])
    for cch in range(8):
        g = cch // 2
        kb.dma("sp", UB, CB[2048 + cch * 128:2048 + (cch + 1) * 128, :], writes=[UB_r])
        for quad in range(4):
            bank = 4 + (cch * 4 + quad) % 4
            for j in range(4):
                tb = quad * 4 + j
                kb.op("pe", lambda: nc.tensor.matmul(c.PS[:, bank, j * 128:(j + 1) * 128], VT[:, tb, cch * 128:(cch + 1) * 128], WmT[:, g, :],
                                                     start=True, stop=False),
                      reads=[VT_r[tb], WmT_r], writes=[c.PS_res[bank]])
                kb.op("pe", lambda: nc.tensor.matmul(c.PS[:, bank, j * 128:(j + 1) * 128], c.mb[0:1, 0, :], sgb[0:1, g * 128:(g + 1) * 128],
                                                     start=False, stop=True),
                      reads=[sgb_r, c.const_res], writes=[c.PS_res[bank]])
            kb.op("dve", lambda: nc.vector.tensor_tensor(out=OUTB[:, quad * 512:(quad + 1) * 512], in0=c.PS[:, bank, :], in1=UB[:, quad * 512:(quad + 1) * 512], op=ALU.mult),
                  reads=[c.PS_res[bank], UB_r], pwrites=[OUTB_r])
        kb.dma("sp", YT[2048 + cch * 128:2048 + (cch + 1) * 128, :], OUTB, reads=[OUTB_r])


def stage_ssd(c, CB, DTW, YT, gates=None):
    kb, nc = c.kb, c.nc
    SEG = 512
    XSf = xa(c, 0, [8, SEG], BF16)
    XS_r = rlist("XSf", 8)
    BTf = xa(c, 8192, [4, SEG], BF16)
    BT_r = rlist("BTf", 4)
    CTf = xa(c, 12288, [4, SEG], BF16)
    CT_r = rlist("CTf", 4)
    ZTf = xa(c, 16384, [8, SEG], BF16)
    ZT_r = Res("ZTf")
    RP = [xa(c, 24576, [SEG + 4], BF16), xa(c, 25616, [SEG + 4], BF16)]
    RP_r = [Res("RP0"), Res("RP1")]
    AC = [xa(c, 26656, [SEG], F32), xa(c, 28704, [SEG], F32)]
    AC_r = [Res("AC0"), Res("AC1")]
    B0 = 30752
    dt_all = xa(c, B0, [32, 16], F32, parts=64)
    dtA_all = xa(c, B0 + 2048, [32, 16], F32, parts=64)
    dt_r = Res("dt")
    Ab = xa(c, B0 + 4096, [16], F32, parts=64)
    Dd = xa(c, B0 + 4160, [16, 64], BF16, parts=64)
    Dd_r = Res("Dd")
    MT = xa(c, B0 + 6208, [16, 64], BF16, parts=64)
    MT_r = Res("MT")
    cbm = xa(c, B0 + 8256, [4, 64], F32, parts=64)
    cbm_r = Res("cbm")
    xs_tok = xa(c, B0 + 9280, [16, 64], BF16, parts=64)
    xst_r = Res("xs_tok")
    xd = xa(c, B0 + 11328, [16, 64], BF16, parts=64)
    xd_r = Res("xd")
    xdw = xa(c, B0 + 13376, [1024], BF16, parts=64)
    xdw_r = Res("xdw")
    Btok = xa(c, B0 + 15424, [512], BF16, parts=64)
    Btok_r = Res("Btok")
    S = xa(c, B0 + 16448, [1024], F32)
    S_r = Res("S")
    Sbf = xa(c, B0 + 20544, [1024], BF16)
    Sbf_r = Res("Sbf")
    ea = xa(c, B0 + 22592, [16], F32, parts=64)
    ea_r = Res("ea")
    cdb = xa(c, B0 + 22656, [16], F32)
    cdb_r = Res("cdb")
    ssq = xa(c, B0 + 22720, [4], F32, parts=64)
    ssq_r = Res("ssq")
    yn = xa(c, B0 + 22784, [1024], BF16, parts=64)
    yn_r = Res("yn")
    YC = xa(c, B0 + 24832, [8, 256], BF16)
    YC_r = Res("YC")
    assert B0 + 24832 + 4096 <= 63488
    UT = c.mf[0:64, 1, 0:64]
    SL = c.mf[0:64, 2, 0:64]
    SEL = c.mf[0:64, 3, :]

    def gstep(k):
        if gates is not None:
            gates.step(k)

    def conv_segment(seg):
        s0 = seg * SEG
        for ch in range(16):
            i = ch % 2
            row = 5120 + ch * 128
            if seg == 0:
                kb.op("dve", lambda: nc.vector.memset(RP[i][:, 0:4], 0.0), writes=[RP_r[i]])
                kb.dma("sp", RP[i][:, 4:SEG + 4], CB[row:row + 128, 0:SEG], reads=[RP_r[i]], writes=[RP_r[i]])
            else:
                kb.dma("sp", RP[i][:, 0:SEG + 4], CB[row:row + 128, s0 - 4:s0 + SEG], writes=[RP_r[i]])
            w = [SMO["conv_w%d" % j] + ch for j in range(4)]
            bcol = SMO["conv_b"] + ch
            kb.op("dve", lambda: nc.vector.tensor_scalar(out=AC[i], in0=RP[i][:, 4:SEG + 4], scalar1=c.sm[:, w[3]:w[3] + 1], scalar2=c.sm[:, bcol:bcol + 1],
                                                         op0=ALU.mult, op1=ALU.add),
                  reads=[RP_r[i], c.sm_res], writes=[AC_r[i]])
            for j in (2, 1, 0):
                kb.op("dve", lambda: nc.vector.scalar_tensor_tensor(out=AC[i], in0=RP[i][:, 1 + j:SEG + 1 + j], scalar=c.sm[:, w[j]:w[j] + 1], in1=AC[i],
                                                                    op0=ALU.mult, op1=ALU.add),
                      reads=[RP_r[i], AC_r[i], c.sm_res], writes=[AC_r[i]])
            if ch < 8:
                dst, dr = XSf[:, ch, :], XS_r[ch]
            elif ch < 12:
                dst, dr = BTf[:, ch - 8, :], BT_r[ch - 8]
            else:
                dst, dr = CTf[:, ch - 12, :], CT_r[ch - 12]
            kb.op("act", lambda: nc.scalar.activation(out=dst, in_=AC[i], func=AF.Silu), reads=[AC_r[i]], writes=[dr])
        for j in range(8):
            kb.dma("sp", ZTf[:, j, :], CB[4096 + j * 128:4096 + (j + 1) * 128, s0:s0 + SEG],
                   writes=[ZT_r] if j == 0 else (), pwrites=[ZT_r] if j > 0 else ())

    kb.dma("sp", dt_all, DTW[:, 0:16].rearrange("(c l) h -> l c h", l=64), writes=[dt_r])
    o = RWO["dt_bias"]
    kb.op("dve", lambda: nc.vector.tensor_tensor(out=dt_all, in0=dt_all, in1=c.rw[0:64, o:o + 16].unsqueeze(1).broadcast_to([64, 32, 16]), op=ALU.add),
          reads=[dt_r, c.rw_res], writes=[dt_r])
    kb.op("act", lambda: nc.scalar.activation(out=dt_all, in_=dt_all, func=AF.Exp), reads=[dt_r], writes=[dt_r])
    kb.op("act", lambda: nc.scalar.activation(out=dt_all, in_=dt_all, func=AF.Ln, bias=1.0), reads=[dt_r], writes=[dt_r])
    o = RWO["a_log"]
    kb.op("act", lambda: nc.scalar.activation(out=Ab, in_=c.rw[0:64, o:o + 16], func=AF.Exp), reads=[c.rw_res, dt_r], writes=[dt_r])
    kb.op("dve", lambda: nc.vector.scalar_tensor_tensor(out=dtA_all, in0=dt_all, scalar=-1.0, in1=Ab.unsqueeze(1).broadcast_to([64, 32, 16]),
                                                        op0=ALU.mult, op1=ALU.mult),
          reads=[dt_r], writes=[dt_r])
    o = RWO["ssm_d"]
    kb.op("dve", lambda: nc.vector.tensor_tensor(out=Dd, in0=c.mb[0:64, 1, 0:64].unsqueeze(1).broadcast_to([64, 16, 64]),
                                                 in1=c.rw[0:64, o:o + 16].unsqueeze(2).broadcast_to([64, 16, 64]), op=ALU.mult),
          reads=[c.const_res, c.rw_res], writes=[Dd_r])
    kb.op("dve", lambda: nc.vector.memset(S, 0.0), writes=[S_r])
    kb.op("dve", lambda: nc.vector.memset(Sbf, 0.0), writes=[Sbf_r])
    psb0 = c.PS[:, 0, :].bitcast(BF16)
    psb1 = c.PS[:, 1, :].bitcast(BF16)
    P = c.PS_res
    gcol = SMO["ssm_norm_g"]

    def v16(ap):
        return ap.rearrange("p a (h l) -> p (a h) l", l=64)

    for ch in range(32):
        if ch % 8 == 0:
            conv_segment(ch // 8)
        t0 = (ch % 8) * 64
        for j in range(8):
            kb.op("pe", lambda: nc.tensor.transpose(psb0[0:64, j * 128:(j + 1) * 128], XSf[:, j, t0:t0 + 64], c.ident),
                  reads=[XS_r[j], c.const_res], writes=[P[0]])
        for g in range(4):
            kb.op("pe", lambda: nc.tensor.transpose(psb1[0:64, g * 128:(g + 1) * 128], BTf[:, g, t0:t0 + 64], c.ident),
                  reads=[BT_r[g], c.const_res], writes=[P[1]])
        kb.op("act", lambda: nc.scalar.activation(out=xs_tok.rearrange("p h l -> p (h l)"), in_=psb0[0:64, :], func=AF.Copy), reads=[P[0]], writes=[xst_r])
        kb.op("act", lambda: nc.scalar.activation(out=Btok, in_=psb1[0:64, 0:512], func=AF.Copy), reads=[P[1]], writes=[Btok_r])
        for j in range(8):
            kb.op("pe", lambda: nc.tensor.transpose(psb0[0:64, j * 128:(j + 1) * 128], ZTf[:, j, t0:t0 + 64], c.ident),
                  reads=[ZT_r, c.const_res], writes=[P[0]])
        zt, zt_r = stage_tile(c)
        Zt = zt[0:64, :].rearrange("p (h l) -> p h l", l=64)
        kb.op("dve", lambda: nc.vector.tensor_tensor(out=Zt, in0=UT.unsqueeze(1).broadcast_to([64, 16, 64]),
                                                     in1=dtA_all[:, ch, :].unsqueeze(2).broadcast_to([64, 16, 64]), op=ALU.mult),
              reads=[dt_r, c.const_res], writes=[zt_r])
        gstep(1)
        for hf in range(2):
            kb.op("pe", lambda: nc.tensor.matmul(c.PS[0:64, 2 + hf, :], SL, zt[0:64, hf * 512:(hf + 1) * 512], start=True, stop=True),
                  reads=[zt_r, c.const_res], writes=[P[2 + hf]])
        kb.op("pe", lambda: nc.tensor.matmul(c.PS[0:64, 4, 256:272], UT, dtA_all[:, ch, :], start=True, stop=True),
              reads=[dt_r, c.const_res], writes=[P[4]])
        for g in range(4):
            kb.op("pe", lambda: nc.tensor.matmul(c.PS[0:64, 4, g * 64:(g + 1) * 64], BTf[:, g, t0:t0 + 64], CTf[:, g, t0:t0 + 64], start=True, stop=True),
                  reads=[BT_r[g], CT_r[g]], writes=[P[4]])
        dc, dc_r = stage_tile(c)
        dec = dc[0:64, :].rearrange("p (h l) -> p h l", l=64)
        kb.op("act", lambda: nc.scalar.activation(out=v3(dc[0:64, :]), in_=c.PS[0:64, 2:4, :], func=AF.Exp), reads=[P[2], P[3]], writes=[dc_r])
        kb.op("act", lambda: nc.scalar.activation(out=ea, in_=c.PS[0:64, 4, 256:272], func=AF.Exp), reads=[P[4]], writes=[ea_r])
        kb.op("dve", lambda: nc.vector.tensor_tensor(out=cbm, in0=c.PS[0:64, 4, 0:256].rearrange("p (g l) -> p g l", l=64),
                                                     in1=UT.unsqueeze(1).broadcast_to([64, 4, 64]), op=ALU.mult),
              reads=[P[4], c.const_res], writes=[cbm_r])
        kb.op("dve", lambda: nc.vector.tensor_tensor(out=MT.rearrange("p (g a) l -> p g a l", a=4), in0=dec.rearrange("p (g a) l -> p g a l", a=4),
                                                     in1=cbm.unsqueeze(2).broadcast_to([64, 4, 4, 64]), op=ALU.mult),
              reads=[dc_r, cbm_r], writes=[MT_r])
        kb.op("dve", lambda: nc.vector.tensor_tensor(out=xd, in0=xs_tok, in1=dt_all[:, ch, :].unsqueeze(2).broadcast_to([64, 16, 64]), op=ALU.mult),
              reads=[xst_r, dt_r], writes=[xd_r])
        kb.op("dve", lambda: nc.vector.tensor_tensor(out=xdw.rearrange("p (h l) -> p h l", l=64), in0=xd, in1=dec[:, :, 63:64].broadcast_to([64, 16, 64]), op=ALU.mult),
              reads=[xd_r, dc_r], writes=[xdw_r])
        gstep(1)
        for h in range(16):
            o_ap = c.PS[0:64, 5 + h // 8, (h % 8) * 64:(h % 8 + 1) * 64]
            kb.op("pe", lambda: nc.tensor.matmul(o_ap, MT[:, h, :], xd[:, h, :], start=True, stop=False),
                  reads=[MT_r, xd_r], writes=[P[5 + h // 8]])
            kb.op("pe", lambda: nc.tensor.matmul(o_ap, Dd[:, h, :], xs_tok[:, h, :], start=False, stop=True),
                  reads=[Dd_r, xst_r], writes=[P[5 + h // 8]])
        for g in range(4):
            kb.op("pe", lambda: nc.tensor.matmul(c.PS[0:64, 2 + g // 2, (g % 2) * 256:(g % 2 + 1) * 256], CTf[:, g, t0:t0 + 64], Sbf[:, g * 256:(g + 1) * 256],
                                                 start=True, stop=True),
                  reads=[CT_r[g], Sbf_r], writes=[P[2 + g // 2]])
        t1, t1_r = stage_tile(c)
        t1v = t1[0:64, :].rearrange("p (h l) -> p h l", l=64)
        kb.op("dve", lambda: nc.vector.tensor_tensor(out=t1v, in0=v16(c.PS[0:64, 2:4, :]), in1=ea.unsqueeze(2).broadcast_to([64, 16, 64]), op=ALU.mult),
              reads=[P[2], P[3], ea_r], writes=[t1_r])
        kb.op("dve", lambda: nc.vector.tensor_tensor(out=t1v, in0=t1v, in1=v16(c.PS[0:64, 5:7, :]), op=ALU.add),
              reads=[t1_r, P[5], P[6]], writes=[t1_r])
        yz, yz_r = stage_tile(c)
        kb.op("dve", lambda: nc.vector.tensor_tensor(out=yz[0:64, :], in0=t1[0:64, :], in1=psb0[0:64, :], op=ALU.mult),
              reads=[t1_r, P[0]], writes=[yz_r])
        sq, sq_r = stage_tile(c)
        kb.op("pool", lambda: nc.gpsimd.tensor_tensor(out=sq[0:64, :], in0=yz[0:64, :], in1=yz[0:64, :], op=ALU.mult), reads=[yz_r], writes=[sq_r])
        kb.op("dve", lambda: nc.vector.tensor_reduce(out=ssq, in_=sq[0:64, :].rearrange("p (g q) -> p g q", g=4), axis=AX.X, op=ALU.add),
              reads=[sq_r], writes=[ssq_r])
        kb.op("act", lambda: nc.scalar.activation(out=ssq, in_=ssq, func=AF.Sqrt, bias=c.cf[0:64, 0:1], scale=1.0 / 256), reads=[ssq_r, c.const_res], writes=[ssq_r])
        kb.op("dve", lambda: nc.vector.reciprocal(out=ssq, in_=ssq), reads=[ssq_r], writes=[ssq_r])
        kb.op("dve", lambda: nc.vector.tensor_tensor(out=yn.rearrange("p (g q) -> p g q", g=4), in0=yz[0:64, :].rearrange("p (g q) -> p g q", g=4),
                                                     in1=ssq.unsqueeze(2).broadcast_to([64, 4, 256]), op=ALU.mult),
              reads=[yz_r, ssq_r], writes=[yn_r])
        gstep(1)
        for j in range(8):
            kb.op("pe", lambda: nc.tensor.transpose(psb1[:, j * 64:(j + 1) * 64], yn[:, j * 128:(j + 1) * 128], c.mb[0:64, 1, 0:64]),
                  reads=[yn_r, c.const_res], writes=[P[1]])
        slot = ch % 4
        kb.op("dve", lambda: nc.vector.tensor_tensor(out=YC[:, :, slot * 64:(slot + 1) * 64], in0=psb1[:, 0:512].rearrange("p (j l) -> p j l", l=64),
                                                     in1=c.sm[:, gcol:gcol + 8].unsqueeze(2).broadcast_to([128, 8, 64]), op=ALU.mult),
              reads=[P[1], c.sm_res], pwrites=[YC_r])
        if slot == 3:
            tq = ch // 4
            kb.dma("sp", YT[3072:4096, tq * 256:(tq + 1) * 256].rearrange("(j p) t -> p j t", p=128), YC, reads=[YC_r])
        if ch == 31:
            break
        for g in range(4):
            kb.op("pe", lambda: nc.tensor.matmul(c.PS[:, g // 2, (g % 2) * 256:(g % 2 + 1) * 256], Btok[:, g * 128:(g + 1) * 128], xdw[:, g * 256:(g + 1) * 256],
                                                 start=True, stop=True),
                  reads=[Btok_r, xdw_r], writes=[P[g // 2]])
        kb.op("pe", lambda: nc.tensor.matmul(c.PS[:, 4, 288:304], SEL, ea, start=True, stop=True), reads=[ea_r, c.const_res], writes=[P[4]])
        kb.op("act", lambda: nc.scalar.activation(out=cdb, in_=c.PS[:, 4, 288:304], func=AF.Copy), reads=[P[4]], writes=[cdb_r])
        kb.op("dve", lambda: nc.vector.tensor_tensor(out=S.rearrange("p (h l) -> p h l", l=64), in0=S.rearrange("p (h l) -> p h l", l=64),
                                                     in1=cdb.unsqueeze(2).broadcast_to([128, 16, 64]), op=ALU.mult),
              reads=[S_r, cdb_r], writes=[S_r])
        kb.op("dve", lambda: nc.vector.tensor_tensor(out=v3(S), in0=v3(S), in1=c.PS[:, 0:2, :], op=ALU.add), reads=[S_r, P[0], P[1]], writes=[S_r])
        kb.op("act", lambda: nc.scalar.activation(out=Sbf, in_=S, func=AF.Copy), reads=[S_r], writes=[Sbf_r])


def stage_rope(c, CB, rope_d, gates=None):
    kb, nc = c.kb, c.nc
    H = 1024
    tabs = [xa(c, i * 4096, [H], F32) for i in range(4)]
    xin = [xa(c, 16384, [H], BF16), xa(c, 18432, [H], BF16)]
    xin_r = [Res("xin0"), Res("xin1")]
    t1 = [xa(c, 20480, [H], F32), xa(c, 24576, [H], F32)]
    t1_r = [Res("t10"), Res("t11")]
    t2 = [xa(c, 28672, [H], F32), xa(c, 32768, [H], F32)]
    t2_r = [Res("t20"), Res("t21")]
    ob = [xa(c, 36864, [H], BF16), xa(c, 38912, [H], BF16)]
    ob_r = [Res("ob0"), Res("ob1")]
    blocks = [(7184 + i * 128, 128, 0) for i in range(8)] + [(8208 + i * 128, 128, 0) for i in range(2)]
    blocks += [(8720 + i * 128, 128, 1) for i in range(4)] + [(9232, 64, 1)]
    bi = 0
    for th in range(2):
        tab_r = Res("tabs%d" % th)
        for i in range(4):
            kb.dma("sp", tabs[i], rope_d[i][:, th * H:(th + 1) * H], writes=[tab_r, t1_r[0], t1_r[1], t2_r[0], t2_r[1]] if i == 0 else (),
                   pwrites=[tab_r] if i > 0 else ())
        for (r0, rows, kind) in blocks:
            i = bi % 2
            bi += 1
            pb = i * 2
            cosT, sinT = tabs[2 * kind], tabs[2 * kind + 1]
            Rm = c.mb[0:rows, 2 + kind, 0:rows]
            kb.dma("sp", xin[i][0:rows, :], CB[r0:r0 + rows, th * H:(th + 1) * H], writes=[xin_r[i]])
            for j in range(2):
                kb.op("pe", lambda: nc.tensor.matmul(c.PS[0:rows, pb + j, :], Rm, xin[i][0:rows, j * 512:(j + 1) * 512], start=True, stop=True),
                      reads=[xin_r[i], c.const_res], writes=[c.PS_res[pb + j]])
            kb.op("pool", lambda: nc.gpsimd.tensor_tensor(out=t1[i][0:rows, :], in0=xin[i][0:rows, :], in1=cosT[0:rows, :], op=ALU.mult),
                  reads=[xin_r[i], tab_r], writes=[t1_r[i]])
            kb.op("dve", lambda: nc.vector.tensor_tensor(out=v3(t2[i][0:rows, :], 2), in0=c.PS[0:rows, pb:pb + 2, :], in1=v3(sinT[0:rows, :], 2), op=ALU.mult),
                  reads=c.PS_res[pb:pb + 2] + [tab_r], writes=[t2_r[i]])
            kb.op("pool", lambda: nc.gpsimd.tensor_tensor(out=ob[i][0:rows, :], in0=t1[i][0:rows, :], in1=t2[i][0:rows, :], op=ALU.add),
                  reads=[t1_r[i], t2_r[i]], writes=[ob_r[i]])
            kb.dma("sp", CB[r0:r0 + rows, th * H:(th + 1) * H], ob[i][0:rows, :], reads=[ob_r[i]])
            if gates is not None:
                gates.step(1)


class GateGen:
    def __init__(self, c, HN, w_gate, GS):
        self.c, self.HN, self.w_gate, self.GS = c, HN, w_gate, GS
        self.it = self._gen()
        self.done = False
        self.nunits = 0
        self.banks = [6, 7]

    def step(self, k):
        for _ in range(k):
            if self.done:
                return
            try:
                next(self.it)
                self.nunits += 1
            except StopIteration:
                self.done = True

    def drain(self):
        while not self.done:
            self.step(64)

    def _gen(self):
        c = self.c
        kb, nc = c.kb, c.nc
        XH = xa(c, 65536, [DC, 1024], BF16)
        gst = [xa(c, 63488, [512], BF16), xa(c, 64512, [512], BF16)]
        gst_r = [Res("gst0"), Res("gst1")]
        prev = None
        for th in range(2):
            tok0 = th * 1024
            xh_r = Res("XH%d" % th)
            for k in range(DC):
                kb.dma("sp", XH[:, k, :], self.HN[k * 128:(k + 1) * 128, tok0:tok0 + 1024], pwrites=[xh_r],
                       writes=[prev] if (k == 0 and prev is not None) else ())
            prev = xh_r
            wloads = []
            for i in range(4):
                for nb in range(DC):
                    wloads.append([(self.w_gate[i], 0, DC, nb * 128, 128)])

            def unit_gen(th=th, tok0=tok0, xh_r=xh_r):
                un = 0
                for i in range(4):
                    for nb in range(DC):
                        for tq in range(2):
                            bank = self.banks[un % len(self.banks)]
                            un += 1

                            def epi(u, i=i, nb=nb, tq=tq, bank=bank):
                                j = bank % 2
                                kb.op("act", lambda: nc.scalar.activation(out=gst[j], in_=c.PS[:, bank, :], func=AF.Sigmoid),
                                      reads=[c.PS_res[bank]], writes=[gst_r[j]])
                                kb.dma("sp", self.GS[i, nb * 128:(nb + 1) * 128, tok0 + tq * 512:tok0 + (tq + 1) * 512], gst[j], reads=[gst_r[j]])

                            yield dict(w=i * DC + nb, accs=[fm_acc(0, XH, xh_r, tq * 512, 512, bank)], epi=epi)

            for _ in gemm_iter(c, wloads, unit_gen()):
                yield 1


def stage_dsa(c, CB, DTW, YT, gates):
    kb, nc = c.kb, c.nc
    P = c.PS_res
    KR = xa(c, 0, [2, T], BF16)
    KI = xa(c, 8192, [T], BF16, parts=64)
    ld_r = Res("loads")
    VTk = xa(c, 12288, [16, 256], BF16)
    VT_r = Res("VTk")
    QRq = [xa(c, 20480, [8, 128], BF16), xa(c, 22528, [8, 128], BF16)]
    QRq_r = [Res("QRq0"), Res("QRq1")]
    QIq = [xa(c, 24576, [8, 128], BF16, parts=64), xa(c, 26624, [8, 128], BF16, parts=64)]
    QIq_r = [Res("QIq0"), Res("QIq1")]
    acc = xa(c, 28672, [T], F32)
    acc_r = Res("acc")
    vtmp = xa(c, 28672, [2, T], BF16)
    work = xa(c, 36864, [T], F32)
    work_r = Res("work")
    maskb = xa(c, 45056, [T], BF16)
    maskb_r = Res("maskb")
    MTs = [xa(c, 49152, [16, 128], BF16), xa(c, 53248, [16, 128], BF16)]
    MTs_r = [Res("MTs0"), Res("MTs1")]
    wi_all = xa(c, 57344, [16, 8], F32)
    mx = xa(c, 57856, [8], F32)
    mx_r = Res("mx")
    Pt = [xa(c, 57888, [512], BF16), xa(c, 58912, [512], BF16)]
    Pt_r = [Res("Pt0"), Res("Pt1")]
    Pm = [xa(c, 59936, [512], BF16), xa(c, 60960, [512], BF16)]
    Pm_r = [Res("Pm0"), Res("Pm1")]
    ob = xa(c, 61984, [512], BF16)
    ob_r = Res("ob")
    for g in range(2):
        kb.dma("sp", KR[:, g, :], CB[8208 + g * 128:8208 + (g + 1) * 128, :], pwrites=[ld_r])
        kb.dma("sp", vtmp[:, g, :], CB[8464 + g * 128:8464 + (g + 1) * 128, :], pwrites=[acc_r])
    kb.dma("sp", KI, CB[9232:9296, :], pwrites=[ld_r])
    kb.dma("sp", wi_all, DTW[:, 16:24].rearrange("(b p) h -> p b h", p=128), pwrites=[ld_r])
    for tq in range(4):
        psb = c.PS[:, tq % 2, :].bitcast(BF16)
        for j in range(4):
            tb = tq * 4 + j
            for g in range(2):
                kb.op("pe", lambda: nc.tensor.transpose(psb[:, j * 256 + g * 128:j * 256 + (g + 1) * 128], vtmp[:, g, tb * 128:(tb + 1) * 128], c.ident),
                      reads=[acc_r, c.const_res], writes=[P[tq % 2]])
        kb.op("act", lambda: nc.scalar.activation(out=VTk[:, tq * 4:(tq + 1) * 4, :].rearrange("p a b -> p (a b)"), in_=psb, func=AF.Copy),
              reads=[P[tq % 2]], pwrites=[VT_r])
    scale = 128.0 ** -0.5

    def load_qr(qb):
        q0 = qb * 128
        kb.dma("sp", QRq[qb % 2], CB[7184:8208, q0:q0 + 128].rearrange("(h p) t -> p h t", p=128), writes=[QRq_r[qb % 2]])

    def load_qi(qb):
        q0 = qb * 128
        kb.dma("sp", QIq[qb % 2], CB[8720:9232, q0:q0 + 128].rearrange("(h p) t -> p h t", p=64), writes=[QIq_r[qb % 2]])

    def indexer(qb):
        n = (qb + 1) * 128
        ngr = (n + 511) // 512
        it = 0
        for h in range(8):
            for kg in range(ngr):
                kw = min(512, n - kg * 512)
                bank = it % 2
                it += 1
                kb.op("pe", lambda: nc.tensor.matmul(c.PS[:, bank, 0:kw], QIq[qb % 2][:, h, :], KI[:, kg * 512:kg * 512 + kw], start=True, stop=True),
                      reads=[ld_r, QIq_r[qb % 2]], writes=[P[bank]])
                rl, rl_r = stage_tile(c)
                kb.op("act", lambda: nc.scalar.activation(out=rl[:, 0:kw], in_=c.PS[:, bank, 0:kw], func=AF.Relu), reads=[P[bank]], writes=[rl_r])
                if h == 0:
                    kb.op("dve", lambda: nc.vector.tensor_scalar_mul(out=acc[:, kg * 512:kg * 512 + kw], in0=rl[:, 0:kw], scalar1=wi_all[:, qb, 0:1]),
                          reads=[rl_r, ld_r], writes=[acc_r] if kg == 0 else (), pwrites=[acc_r] if kg > 0 else ())
                else:
                    kb.op("dve", lambda: nc.vector.scalar_tensor_tensor(out=acc[:, kg * 512:kg * 512 + kw], in0=rl[:, 0:kw], scalar=wi_all[:, qb, h:h + 1],
                                                                        in1=acc[:, kg * 512:kg * 512 + kw], op0=ALU.mult, op1=ALU.add),
                          reads=[rl_r, ld_r, acc_r], writes=[acc_r])
        kb.op("dve", lambda: nc.vector.memset(acc[0:64, n - 64:n], NEG), reads=[acc_r], writes=[acc_r])

    def topk(qb):
        n = (qb + 1) * 128
        if qb >= 2:
            kb.op("pool", lambda: nc.gpsimd.tensor_copy(out=work[:, 0:n], in_=acc[:, 0:n]), reads=[acc_r], writes=[work_r])
            for r in range(32):
                kb.op("dve", lambda: nc.vector.max(out=mx, in_=work[:, 0:n]), reads=[work_r], writes=[mx_r])
                if r < 31:
                    kb.op("dve", lambda: nc.vector.match_replace(out=work[:, 0:n], in_to_replace=mx, in_values=work[:, 0:n], imm_value=NEG),
                          reads=[mx_r, work_r], writes=[work_r])
            thr, thr_reads = mx[:, 7:8], [mx_r]
        else:
            thr, thr_reads = c.cf[:, 1:2], [c.const_res]
        kb.op("dve", lambda: nc.vector.tensor_single_scalar(out=maskb[:, 0:n], in_=acc[:, 0:n], scalar=thr, op=ALU.is_ge),
              reads=[acc_r] + thr_reads, writes=[maskb_r])

    def mask_T(qb):
        nk = qb + 1
        M, M_r = MTs[qb % 2], MTs_r[qb % 2]
        for kb0 in range(0, nk, 8):
            cnt = min(8, nk - kb0)
            bank = kb0 // 8
            psb = c.PS[:, bank, :].bitcast(BF16)
            for j in range(cnt):
                kbi = kb0 + j
                kb.op("pe", lambda: nc.tensor.transpose(psb[:, j * 128:(j + 1) * 128], maskb[:, kbi * 128:(kbi + 1) * 128], c.ident),
                      reads=[maskb_r, c.const_res], writes=[P[bank]])
            kb.op("act", lambda: nc.scalar.activation(out=M[:, kb0:kb0 + cnt, :].rearrange("p a b -> p (a b)"), in_=psb[:, 0:cnt * 128], func=AF.Copy),
                  reads=[P[bank]], writes=[M_r] if kb0 == 0 else (), pwrites=[M_r] if kb0 > 0 else ())

    def attention(qb):
        nk = qb + 1
        q0 = qb * 128
        M, M_r = MTs[qb % 2], MTs_r[qb % 2]
        Q, Q_r = QRq[qb % 2], QRq_r[qb % 2]
        for g in range(2):
            for kbi in range(nk):
                sb = 2 + kbi % 2
                j = kbi % 2
                kb.op("pe", lambda: nc.tensor.matmul(c.PS[:, sb, :].rearrange("p (h q) -> p h q", h=4), KR[:, g, kbi * 128:(kbi + 1) * 128],
                                                     Q[:, 4 * g:4 * g + 4, :], start=True, stop=True),
                      reads=[ld_r, Q_r], writes=[P[sb]])
                kb.op("act", lambda: nc.scalar.activation(out=Pt[j], in_=c.PS[:, sb, :], func=AF.Exp, scale=scale), reads=[P[sb]], writes=[Pt_r[j]])
                kb.op("pool", lambda: nc.gpsimd.tensor_tensor(out=Pm[j].rearrange("p (h q) -> p h q", h=4), in0=Pt[j].rearrange("p (h q) -> p h q", h=4),
                                                              in1=M[:, kbi, :].unsqueeze(1).broadcast_to([128, 4, 128]), op=ALU.mult),
                      reads=[Pt_r[j], M_r], writes=[Pm_r[j]])
                kb.op("pe", lambda: nc.tensor.matmul(c.PS[:, 4, :], VTk[:, kbi, g * 128:(g + 1) * 128], Pm[j], start=(kbi == 0), stop=(kbi == nk - 1)),
                      reads=[VT_r, Pm_r[j]], writes=[P[4]])
                kb.op("pe", lambda: nc.tensor.matmul(c.PS[:, 5, :], c.ones, Pm[j], start=(kbi == 0), stop=(kbi == nk - 1)),
                      reads=[c.const_res, Pm_r[j]], writes=[P[5]])
            o_t, o_r = stage_tile(c)
            kb.op("act", lambda: nc.scalar.activation(out=o_t[:, 0:512], in_=c.PS[:, 4, :], func=AF.Copy), reads=[P[4]], writes=[o_r])
            kb.op("act", lambda: nc.scalar.activation(out=o_t[:, 512:1024], in_=c.PS[:, 5, :], func=AF.Ln), reads=[P[5]], pwrites=[o_r])
            kb.op("act", lambda: nc.scalar.activation(out=o_t[:, 512:1024], in_=o_t[:, 512:1024], func=AF.Exp, scale=-1.0), reads=[o_r], writes=[o_r])
            kb.op("pool", lambda: nc.gpsimd.tensor_tensor(out=ob, in0=o_t[:, 0:512], in1=o_t[:, 512:1024], op=ALU.mult), reads=[o_r], writes=[ob_r])
            kb.dma("sp", YT[4096 + 4 * g * 128:4096 + (4 * g + 4) * 128, q0:q0 + 128].rearrange("(h p) t -> p h t", p=128),
                   ob.rearrange("p (h q) -> p h q", h=4), reads=[ob_r])

    load_qr(0)
    load_qi(0)
    indexer(0)
    for qb in range(16):
        if qb < 15:
            load_qi(qb + 1)
        topk(qb)
        if qb > 0:
            attention(qb - 1)
        if qb < 15:
            load_qr(qb + 1)
        if gates is not None:
            gates.step(int(round(512.0 * (qb + 1) / 136.0)) + 1)
        if qb < 15:
            indexer(qb + 1)
        mask_T(qb)
    attention(15)
    if gates is not None:
        gates.drain()


BR_ROWS = ((0, 16), (2048, 8), (3072, 8), (4096, 8))


def stage_merge(c, YT, GS, w_branch, MTd):
    kb, nc = c.kb, c.nc
    for th in range(2):
        tok0 = th * 1024
        XY = load_x_plain(c, YT, None, 40, tok0, 1024, off=0)
        wloads, units = [], []
        for nb in range(DC):
            wloads.append([(w_branch, 0, 40, nb * 128, 128)])
            for tq in range(2):
                pb = ((nb * 2 + tq) % 2) * 4
                hold = {}

                def pre(u, nb=nb, tq=tq, hold=hold):
                    gt, gt_r = stage_tile(c)
                    kb.dma("sp", gt[:, :].bitcast(BF16).rearrange("p (i t) -> p i t", i=4),
                           GS[:, nb * 128:(nb + 1) * 128, tok0 + tq * 512:tok0 + (tq + 1) * 512].rearrange("i p t -> p i t"), writes=[gt_r])
                    hold["gt"] = (gt, gt_r)

                def epi(u, nb=nb, tq=tq, pb=pb, hold=hold):
                    gt, gt_r = hold["gt"]
                    gv = gt[:, :].bitcast(BF16).rearrange("p (i t) -> p i t", i=4)
                    ma, ma_r = stage_tile(c)
                    mb_, mb_r = stage_tile(c)
                    kb.op("dve", lambda: nc.vector.tensor_tensor(out=v3(ma[:, :]), in0=c.PS[:, pb:pb + 2, :], in1=gv[:, 0:2, :], op=ALU.mult),
                          reads=[gt_r, c.PS_res[pb], c.PS_res[pb + 1]], writes=[ma_r])
                    kb.op("dve", lambda: nc.vector.tensor_tensor(out=v3(mb_[:, :]), in0=c.PS[:, pb + 2:pb + 4, :], in1=gv[:, 2:4, :], op=ALU.mult),
                          reads=[gt_r, c.PS_res[pb + 2], c.PS_res[pb + 3]], writes=[mb_r])
                    kb.op("dve", lambda: nc.vector.tensor_tensor(out=ma[:, :], in0=ma[:, :], in1=mb_[:, :], op=ALU.add),
                          reads=[ma_r, mb_r], writes=[ma_r])
                    ob = mb_[:, 0:256].bitcast(BF16)
                    kb.op("dve", lambda: nc.vector.tensor_tensor(out=ob, in0=ma[:, 0:512], in1=ma[:, 512:1024], op=ALU.add),
                          reads=[ma_r, mb_r], writes=[mb_r])
                    kb.dma("sp", MTd[nb * 128:(nb + 1) * 128, tok0 + tq * 512:tok0 + (tq + 1) * 512], ob, reads=[mb_r])

                accs = []
                for i in range(4):
                    r0, kci = BR_ROWS[i]
                    a = fm_acc(0, XY, c.XA_res, tq * 512, 512, pb + i)
                    a["k0"] = r0 // 128
                    a["kn"] = kci
                    accs.append(a)
                units.append(dict(w=nb, accs=accs, epi=epi, pre=pre))
        gemm(c, wloads, units)


def stage_wout(c, MTd, w_out, XS):
    xr = rlist("xr", DC)
    for th in range(2):
        X = load_x_plain(c, MTd, None, DC, th * 1024, 1024)
        wl, un = residual_units(c, X, DC, w_out, XS, xr, XS, xr, 1024, 1.0, th * 1024)
        gemm(c, wl, un)


def stage_cross(c, XS, memT, w_q, w_kv, w_o, QMd):
    kb, nc = c.kb, c.nc
    P = c.PS_res
    KM = c.kvm[:, 0:1024].rearrange("p (h m) -> p h m", h=4)
    VM = c.kvm[:, 1024:2048].rearrange("p (a b) -> p a b", a=2)
    kv_r = Res("kv")
    X = load_x_norm(c, memT, None, SMO["g_mem"], 0, MEM)
    wloads, units = [], []
    for j in range(2):
        wloads.append([(w_kv, 0, DC, j * 256, 256)])
        for s in range(2):
            hh = j * 2 + s
            pb = hh * 2 % 8

            def epi(u, hh=hh, pb=pb):
                kb.op("act", lambda: nc.scalar.activation(out=KM[:, hh, :], in_=c.PS[:, pb, 0:256], func=AF.Copy), reads=[P[pb]], pwrites=[kv_r])

            units.append(dict(w=j, accs=[fm_acc(0, X, c.XA_res, 0, 256, pb, s * 128, 128)], epi=epi))
    for j in range(2):
        wloads.append([(w_kv, 0, DC, 512 + j * 256, 256)])
        for mb in range(2):
            pb = (j * 2 + mb) * 2 % 8 + 1

            def epi(u, j=j, mb=mb, pb=pb):
                kb.op("act", lambda: nc.scalar.activation(out=VM[:, mb, j * 256:(j + 1) * 256], in_=c.PS[:, pb, 0:256], func=AF.Copy), reads=[P[pb]], pwrites=[kv_r])

            units.append(dict(w=2 + j, accs=[dict(kind="tm", piece=0, X=X, xres=c.XA_res, t0=mb * 128, tw=128, bank=pb, wo=0, ww=256)], epi=epi))
    gemm(c, wloads, units)
    kb.barrier()
    X = load_x_norm(c, XS, None, SMO["g_cross"], 0, T)
    wloads, units = [], []
    for j in range(2):
        wloads.append([(w_q, 0, DC, j * 256, 256)])
        for s in range(2):
            for th in range(2):
                un = (j * 2 + s) * 2 + th
                pb = (un % 4) * 2

                def epi(u, j=j, s=s, th=th, pb=pb):
                    st, st_r = stage_tile(c)
                    ob = st[:, 0:512].bitcast(BF16)
                    kb.op("act", lambda: nc.scalar.activation(out=v3(ob), in_=c.PS[:, pb:pb + 2, :], func=AF.Copy), reads=[P[pb], P[pb + 1]], writes=[st_r])
                    kb.dma("sp", QMd[(j * 2 + s) * 128:(j * 2 + s + 1) * 128, th * 1024:(th + 1) * 1024], ob, reads=[st_r])

                accs = [fm_acc(0, X, c.XA_res, th * 1024, 512, pb, s * 128, 128), fm_acc(0, X, c.XA_res, th * 1024 + 512, 512, pb + 1, s * 128, 128)]
                units.append(dict(w=j, accs=accs, epi=epi))
    gemm(c, wloads, units)
    kb.barrier()
    QM = xa(c, 0, [4, T], BF16)
    qm_r = Res("QM")
    for h in range(4):
        kb.dma("sp", QM[:, h, :], QMd[h * 128:(h + 1) * 128, :], pwrites=[qm_r])
    OM = xa(c, 16384, [4, T], BF16)
    om_r = Res("OM")
    Pt = [xa(c, 32768, [512], BF16), xa(c, 33792, [512], BF16)]
    Pt_r = [Res("cPt0"), Res("cPt1")]
    rs = xa(c, 34816, [512], F32)
    rs_r = Res("crs")
    scale = 128.0 ** -0.5
    it = 0
    for h in range(4):
        for qg in range(4):
            Ob = 4 + (it % 2) * 2
            Sb = Ob + 1
            it += 1
            for mb in range(2):
                sb = mb
                kb.op("pe", lambda: nc.tensor.matmul(c.PS[:, sb, :], KM[:, h, mb * 128:(mb + 1) * 128], QM[:, h, qg * 512:(qg + 1) * 512], start=True, stop=True),
                      reads=[kv_r, qm_r], writes=[P[sb]])
                kb.op("act", lambda: nc.scalar.activation(out=Pt[sb], in_=c.PS[:, sb, :], func=AF.Exp, scale=scale), reads=[P[sb]], writes=[Pt_r[sb]])
                kb.op("pe", lambda: nc.tensor.matmul(c.PS[:, Ob, :], VM[:, mb, h * 128:(h + 1) * 128], Pt[sb], start=(mb == 0), stop=(mb == 1)),
                      reads=[kv_r, Pt_r[sb]], writes=[P[Ob]])
                kb.op("pe", lambda: nc.tensor.matmul(c.PS[:, Sb, :], c.ones, Pt[sb], start=(mb == 0), stop=(mb == 1)),
                      reads=[c.const_res, Pt_r[sb]], writes=[P[Sb]])
            kb.op("dve", lambda: nc.vector.reciprocal(out=rs, in_=c.PS[:, Sb, :]), reads=[P[Sb]], writes=[rs_r])
            kb.op("dve", lambda: nc.vector.tensor_tensor(out=OM[:, h, qg * 512:(qg + 1) * 512], in0=c.PS[:, Ob, :], in1=rs, op=ALU.mult),
                  reads=[P[Ob], rs_r], pwrites=[om_r])
    xr = rlist("xr", DC)
    for th in range(2):
        wloads, units = [], []
        tok0 = th * 1024
        for nb in range(DC):
            pb = (nb % 4) * 2
            if nb % 4 == 0:
                wloads.append([(w_o, 0, 4, nb * 128, 512)])
            hold = {}

            def pre(u, nb=nb, hold=hold):
                xo, xo_r = stage_tile(c)
                kb.dma("sp", xo[:, :], XS[nb * 128:(nb + 1) * 128, tok0:tok0 + 1024], reads=[xr[nb]], writes=[xo_r])
                hold["xo"] = (xo, xo_r)

            def epi(u, nb=nb, pb=pb, hold=hold):
                xo, xo_r = hold["xo"]
                xn, xn_r = stage_tile(c)
                kb.op("dve", lambda: nc.vector.tensor_tensor(out=v3(xn[:, :]), in0=c.PS[:, pb:pb + 2, :], in1=v3(xo[:, :]), op=ALU.add),
                      reads=[xo_r, P[pb], P[pb + 1]], writes=[xn_r])
                kb.dma("sp", XS[nb * 128:(nb + 1) * 128, tok0:tok0 + 1024], xn[:, :], reads=[xn_r], pwrites=[xr[nb]])

            accs = [fm_acc(0, OM, om_r, tok0, 512, pb, (nb % 4) * 128, 128), fm_acc(0, OM, om_r, tok0 + 512, 512, pb + 1, (nb % 4) * 128, 128)]
            units.append(dict(w=nb // 4, accs=accs, epi=epi, pre=pre))
        gemm(c, wloads, units)


STAGES = ("ffn1", "win", "pool", "sg", "ssd", "dsa", "merge", "cross", "ffn2")


def build(plan, dbg=()):
    nc = bass.Bass("TRN2", target_bir_lowering=False)
    kb = KB(nc)
    c = setup(nc, kb)

    def dram_in(name, shape, dt=F32):
        return nc.dram_tensor(name, list(shape), dt, kind="ExternalInput").ap()

    def dram_tmp(name, shape, dt):
        kind = "ExternalOutput" if name in dbg else "Internal"
        return nc.dram_tensor(name, list(shape), dt, kind=kind).ap()

    xT = dram_in("xT", [D, T])
    sm_d = dram_in("sm_in", [DEPTH, 128, NSM])
    rw_d = dram_in("rw_in", [DEPTH, 128, NRW])
    mats_d = dram_in("c_mats", [8, 128, 128])
    rope_d = dram_in("c_rope", [4, 128, T])
    cf_d = dram_in("c_f32", [128, 80])
    kb.dma("pool", c.mb[:, :, :], mats_d[0:4].rearrange("m p n -> p m n"), writes=[c.const_res])
    kb.dma("sp", c.mf[:, :, :], mats_d[4:8].rearrange("m p n -> p m n"), pwrites=[c.const_res])
    kb.dma("sp", c.cf[:, :], cf_d, pwrites=[c.const_res])

    XS = dram_tmp("XS", [D, T], F32)
    HT = dram_tmp("HT", [FF, T], BF16)
    HT_res = rlist("HT", FF // 128)
    CB = dram_tmp("CB", [NIN, T], BF16)
    HN = dram_tmp("HN", [D, T], BF16)
    DTW = dram_tmp("DTW", [T, 24], F32)
    YT = dram_tmp("YT", [5120, T], BF16)
    MTd = dram_tmp("MTd", [D, T], BF16)
    QMd = dram_tmp("QMd", [512, T], BF16)
    GS = dram_tmp("GS", [4, D, T], BF16)
    memT = None

    cur = xT
    for l in range(DEPTH):
        if not any(p[1] == l for p in plan):
            continue
        kb.barrier()
        kb.dma("sp", c.sm[:, :], sm_d[l], writes=[c.sm_res])
        kb.dma("sp", c.rw[:, :], rw_d[l], writes=[c.rw_res])
        kb.barrier()
        if ("ffn1", l) in plan:
            w1 = dram_in("w_ffn1_in_%d" % l, [D, 2 * FF])
            w2 = dram_in("w_ffn1_out_%d" % l, [FF, D])
            ffn(c, cur, XS, SMO["g_ffn1"], w1, w2, HT, HT_res)
            cur = XS
            kb.barrier()
        if ("win", l) in plan:
            w_in = dram_in("w_in_%d" % l, [D, NIN])
            stage_win(c, cur, SMO["g_mix"], w_in, CB, HN, DTW)
            kb.barrier()
        if ("sg", l) in plan:
            sw = dram_in("sg_w_%d" % l, [4, 128, 128])
            stage_sg(c, CB, YT, sw)
            kb.barrier()
        gates = None
        if ("merge", l) in plan:
            wg = dram_in("w_gate_%d" % l, [4, D, D])
            gates = GateGen(c, HN, wg, GS)
        if ("ssd", l) in plan:
            if gates is not None:
                gates.banks = [7]
            stage_ssd(c, CB, DTW, YT, gates)
            kb.barrier()
        if gates is not None:
            gates.banks = [6, 7]
        if ("pool", l) in plan:
            pw = dram_in("pool_w_%d" % l, [4, 512, 512])
            stage_pool(c, CB, YT, pw, gates)
            kb.barrier()
        if ("dsa", l) in plan:
            stage_rope(c, CB, rope_d, gates)
            kb.barrier()
            stage_dsa(c, CB, DTW, YT, gates)
            kb.barrier()
        elif gates is not None:
            gates.drain()
            kb.barrier()
        if ("merge", l) in plan:
            wbr = dram_in("w_branch_%d" % l, [5120, D])
            wo = dram_in("w_out_%d" % l, [D, D])
            if cur is xT:
                for k in range(DC):
                    kb.dma("sp", XS[k * 128:(k + 1) * 128, :], xT[k * 128:(k + 1) * 128, :])
                cur = XS
                kb.barrier()
            stage_merge(c, YT, GS, wbr, MTd)
            kb.barrier()
            stage_wout(c, MTd, wo, XS)
            kb.barrier()
        if ("cross", l) in plan:
            if memT is None:
                memT = dram_in("memT", [D, MEM])
            wq = dram_in("w_mem_q_%d" % l, [D, 512])
            wkv = dram_in("w_mem_kv_%d" % l, [D, 1024])
            wmo = dram_in("w_mem_o_%d" % l, [512, D])
            stage_cross(c, XS, memT, wq, wkv, wmo, QMd)
            kb.barrier()
        if ("ffn2", l) in plan:
            w1 = dram_in("w_ffn2_in_%d" % l, [D, 2 * FF])
            w2 = dram_in("w_ffn2_out_%d" % l, [FF, D])
            ffn(c, XS, XS, SMO["g_ffn2"], w1, w2, HT, HT_res)
            kb.barrier()
    if "final" in [p[0] for p in plan]:
        kb.barrier()
        yT = nc.dram_tensor("yT", [D, T], F32, kind="ExternalOutput").ap()
        load_x_norm(c, cur, None, SMO["g_final"], 0, T, out_dram=yT)
    kb.barrier()
    print("instructions", kb.ninst, "waits", kb.nwaits)
    return nc


_NC_CACHE = {}


def kernel(**inputs):
    inp = {k: np.asarray(v) for k, v in inputs.items()}
    plan = {(s, l) for s in STAGES for l in range(DEPTH)} | {("final", DEPTH)}
    if "nc" not in _NC_CACHE:
        _NC_CACHE["nc"] = build(plan)
    nc = _NC_CACHE["nc"]
    sm, rw = host_tables(inp)
    shared = dict(sm_in=sm, rw_in=rw, **host_consts())
    per_layer = ["w_ffn1_in", "w_ffn1_out", "w_in", "pool_w", "sg_w", "w_gate", "w_branch", "w_out",
                 "w_mem_q", "w_mem_kv", "w_mem_o", "w_ffn2_in", "w_ffn2_out"]
    for l in range(DEPTH):
        for n in per_layer:
            shared["%s_%d" % (n, l)] = np.ascontiguousarray(inp[n][l], dtype=np.float32)
    B = inp["x"].shape[0]
    in_maps = []
    for b in range(B):
        m = dict(shared)
        m["xT"] = np.ascontiguousarray(inp["x"][b].T, dtype=np.float32)
        m["memT"] = np.ascontiguousarray(inp["mem"][b].T, dtype=np.float32)
        in_maps.append(m)
    res = run_bass_kernel_spmd(nc, in_maps, core_ids=list(range(B)))
    out = np.stack([np.ascontiguousarray(res.results[b]["yT"].T) for b in range(B)], axis=0)
    return out.astype(np.float32)
```

```python
import math
import numpy as np
import concourse.bass as bass
import concourse.mybir as mybir
from concourse.bass_utils import run_bass_kernel_spmd

F32 = mybir.dt.float32
BF16 = mybir.dt.bfloat16
AF = mybir.ActivationFunctionType
ALU = mybir.AluOpType
AX = mybir.AxisListType

D = 4096
T = 2048
FF = 8192
DC = D // 128
EPS = 1e-6
DEPTH = 2
MEM = 256
NIN = 9304
POOL_WINDOWS = (2, 4, 8, 16)
NEG = -1.0e30


class Res:
    __slots__ = ("name", "writers", "readers")

    def __init__(self, name=""):
        self.name = name
        self.writers = {}
        self.readers = {}


class KB:
    def __init__(self, nc):
        self.nc = nc
        self.E = dict(pe=nc.tensor, act=nc.scalar, dve=nc.vector, pool=nc.gpsimd, sp=nc.sync)
        self.sems = {}
        self.cnt = {}
        for e in ("pe", "act", "dve", "pool"):
            k = "e_" + e
            self.sems[k] = nc.alloc_semaphore(k)
            self.cnt[k] = 0
        self.seen = {e: {} for e in self.E}
        self.dq = {}
        for q, n in (("sp", 10), ("pool", 8), ("act", 4)):
            keys = []
            for i in range(n):
                k = "d_%s%d" % (q, i)
                self.sems[k] = nc.alloc_semaphore(k)
                self.cnt[k] = 0
                keys.append(k)
            self.dq[q] = [keys, 0]
        self.nwaits = 0
        self.ninst = 0

    def wait(self, eng, key, val):
        if self.seen[eng].get(key, 0) >= val:
            return
        self.E[eng].wait_ge(self.sems[key], val)
        self.seen[eng][key] = val
        self.nwaits += 1

    def _deps(self, eng, reads, writes, pwrites, own):
        for r in reads:
            for k, v in r.writers.items():
                self.wait(eng, k, v)
        for w in writes:
            for k, v in w.writers.items():
                if k != own:
                    self.wait(eng, k, v)
            for k, v in w.readers.items():
                if k != own:
                    self.wait(eng, k, v)
        for w in pwrites:
            if w.readers:
                for k, v in w.readers.items():
                    if k != own:
                        self.wait(eng, k, v)
                w.readers = {}
                w.writers = {}

    def _commit(self, key, val, reads, writes, pwrites):
        for r in reads:
            if r.readers.get(key, 0) < val:
                r.readers[key] = val
        for w in writes:
            w.writers = {key: val}
            w.readers = {}
        for w in pwrites:
            if w.writers.get(key, 0) < val:
                w.writers[key] = val

    def op(self, eng, fn, reads=(), writes=(), pwrites=()):
        own = "e_" + eng
        self._deps(eng, reads, writes, pwrites, own)
        inst = fn()
        self.cnt[own] += 1
        inst.then_inc(self.sems[own], 1)
        self._commit(own, self.cnt[own], reads, writes, pwrites)
        self.ninst += 1
        return inst

    def dma(self, q, out, in_, reads=(), writes=(), pwrites=()):
        self._deps(q, reads, writes, pwrites, None)
        keys, rr = self.dq[q]
        key = keys[rr % len(keys)]
        self.dq[q][1] = rr + 1
        if self.cnt[key] > 0:
            self.wait(q, key, self.cnt[key])
        inst = self.E[q].dma_start(out=out, in_=in_)
        self.cnt[key] += 16
        inst.then_inc(self.sems[key], 16)
        self._commit(key, self.cnt[key], reads, writes, pwrites)
        self.ninst += 1
        return inst

    def barrier(self):
        for eng in self.E:
            for key, val in self.cnt.items():
                if val > 0:
                    self.wait(eng, key, val)


class Ctx:
    pass


def rlist(name, n):
    return [Res("%s%d" % (name, i)) for i in range(n)]


def v3(ap, a=2):
    return ap.rearrange("p (a b) -> p a b", a=a)


SMW = [("g_ffn1", 32), ("g_mix", 32), ("g_cross", 32), ("g_ffn2", 32), ("pool_scale", 16), ("sg_ln_g", 8),
       ("sg_ln_b", 8), ("conv_w0", 16), ("conv_w1", 16), ("conv_w2", 16), ("conv_w3", 16), ("conv_b", 16),
       ("ssm_norm_g", 8), ("g_final", 32), ("g_mem", 32)]
SMO = {}
_o = 0
for _n, _w in SMW:
    SMO[_n] = _o
    _o += _w
NSM = _o
RWW = [("dt_bias", 16), ("a_log", 16), ("ssm_d", 16), ("sg_b", 512)]
RWO = {}
_o = 0
for _n, _w in RWW:
    RWO[_n] = _o
    _o += _w
NRW = _o


def host_tables(inp):
    sm = np.zeros((DEPTH, 128, NSM), np.float32)
    rw = np.zeros((DEPTH, 128, NRW), np.float32)

    def pp(v):
        v = np.asarray(v, np.float32).reshape(-1)
        return v.reshape(-1, 128).T

    for l in range(DEPTH):
        for n in ("g_ffn1", "g_mix", "g_cross", "g_ffn2", "sg_ln_g", "sg_ln_b", "ssm_norm_g"):
            a = pp(inp[n][l])
            sm[l, :, SMO[n]:SMO[n] + a.shape[1]] = a
        sm[l, :, SMO["pool_scale"]:SMO["pool_scale"] + 16] = pp(inp["pool_scale"][l])
        for j in range(4):
            sm[l, :, SMO["conv_w%d" % j]:SMO["conv_w%d" % j] + 16] = pp(inp["ssm_conv_w"][l, j])
        sm[l, :, SMO["conv_b"]:SMO["conv_b"] + 16] = pp(inp["ssm_conv_b"][l])
        sm[l, :, SMO["g_final"]:SMO["g_final"] + 32] = pp(inp["g_final"])
        sm[l, :, SMO["g_mem"]:SMO["g_mem"] + 32] = pp(inp["g_mem"])
        rw[l, :, RWO["dt_bias"]:RWO["dt_bias"] + 16] = np.asarray(inp["ssm_dt_bias"][l])[None, :]
        rw[l, :, RWO["a_log"]:RWO["a_log"] + 16] = np.asarray(inp["ssm_a_log"][l])[None, :]
        rw[l, :, RWO["ssm_d"]:RWO["ssm_d"] + 16] = np.asarray(inp["ssm_d"][l])[None, :]
        rw[l, :, RWO["sg_b"]:RWO["sg_b"] + 512] = np.asarray(inp["sg_b"][l]).reshape(-1)[None, :]
    return sm, rw


M_ONES, M_ID, M_R128, M_R64, M_SG, M_UT, M_SL, M_SEL = range(8)


def host_consts():
    mats = np.zeros((8, 128, 128), np.float32)
    mats[M_ONES] = 1.0
    mats[M_ID] = np.eye(128)
    r = np.zeros((128, 128), np.float32)
    for d in range(64):
        r[d + 64, d] = -1.0
        r[d, d + 64] = 1.0
    mats[M_R128] = r
    r = np.zeros((128, 128), np.float32)
    for b in range(2):
        for d in range(32):
            r[b * 64 + d + 32, b * 64 + d] = -1.0
            r[b * 64 + d, b * 64 + d + 32] = 1.0
    mats[M_R64] = r
    i = np.arange(128)
    mats[M_SG] = ((i[:, None] // 64) >= (i[None, :] // 64)).astype(np.float32)
    j = np.arange(64)
    mats[M_UT, :64, :64] = (j[:, None] <= j[None, :]).astype(np.float32)
    mats[M_SL, :64, :64] = (j[:, None] > j[None, :]).astype(np.float32)
    mats[M_SEL, 63, :] = 1.0
    pos = np.arange(T, dtype=np.float32)
    rope = np.zeros((4, 128, T), np.float32)
    inv = (10000.0 ** (-np.arange(64, dtype=np.float32) / 64)).astype(np.float32)
    ang = pos[None, :] * inv[:, None]
    rope[0, :64] = np.cos(ang)
    rope[0, 64:] = np.cos(ang)
    rope[1, :64] = np.sin(ang)
    rope[1, 64:] = np.sin(ang)
    inv = (10000.0 ** (-np.arange(32, dtype=np.float32) / 32)).astype(np.float32)
    ang = pos[None, :] * inv[:, None]
    for b in range(4):
        rope[2, b * 32:(b + 1) * 32] = np.cos(ang)
        rope[3, b * 32:(b + 1) * 32] = np.sin(ang)
    cf = np.zeros((128, 80), np.float32)
    cf[:, 0] = EPS
    cf[:, 1] = -1.0e29
    for g, w in enumerate(POOL_WINDOWS):
        cf[:, 16 + g * 16:32 + g * 16] = 1.0 / np.minimum(w, np.arange(16) + 1.0)
    return dict(c_mats=mats, c_rope=rope, c_f32=cf)


def setup(nc, kb):
    c = Ctx()
    c.nc = nc
    c.kb = kb
    c.XA = nc.alloc_sbuf_tensor("XA", [128, 65536], BF16)
    c.XA_res = Res("XA")
    c.NW = 3
    c.WB = [nc.alloc_sbuf_tensor("WB%d" % i, [128, 8192], BF16) for i in range(c.NW)]
    c.WB_res = rlist("WB", c.NW)
    c.wrr = 0
    c.NS = 5
    c.ST = [nc.alloc_sbuf_tensor("ST%d" % i, [128, 1024], F32) for i in range(c.NS)]
    c.ST_res = rlist("ST", c.NS)
    c.srr = 0
    c.PS = nc.alloc_psum_tensor("PS", [128, 8, 512], F32)
    c.PS_res = rlist("PS", 8)
    c.mb = nc.alloc_sbuf_tensor("mats_bf", [128, 4, 128], BF16)
    c.mf = nc.alloc_sbuf_tensor("mats_f", [128, 4, 128], F32)
    c.cf = nc.alloc_sbuf_tensor("cf", [128, 80], F32)
    c.const_res = Res("const")
    c.sm = nc.alloc_sbuf_tensor("smt", [128, NSM], F32)
    c.sm_res = Res("sm")
    c.rw = nc.alloc_sbuf_tensor("rwt", [128, NRW], F32)
    c.rw_res = Res("rw")
    c.kvm = nc.alloc_sbuf_tensor("kvm", [128, 2048], BF16)
    c.ones = c.mb[:, 0, :]
    c.ident = c.mb[:, 1, :]
    c.rstd = c.WB[0][:, 0:4096].bitcast(F32)
    c.rstd_res = c.WB_res[0]
    return c


def stage_tile(c):
    i = c.srr % c.NS
    c.srr += 1
    return c.ST[i], c.ST_res[i]


def xa(c, off, shape, dt, parts=128, p0=0):
    n = 1
    for s in shape:
        n *= s
    esz = 2 if dt == BF16 else 4
    assert off % 4 == 0 and off + n * esz <= 131072, (off, shape)
    base = c.XA[p0:p0 + parts, off // 2: off // 2 + n * esz // 2]
    ap = base if dt == BF16 else base.bitcast(dt)
    if len(shape) == 2:
        ap = ap.rearrange("p (a b) -> p a b", a=shape[0])
    elif len(shape) == 3:
        ap = ap.rearrange("p (a b c) -> p a b c", a=shape[0], b=shape[1])
    return ap


def load_x_norm(c, src, src_res, gcol, t0, Tn, out_dram=None):
    kb, nc = c.kb, c.nc
    X = c.XA[:, 0:DC * Tn].rearrange("p (k t) -> p k t", k=DC)
    TS = min(Tn, 1024)
    nsub = Tn // TS
    nbk = TS // 512 if TS >= 512 else 1
    bw = min(512, TS)

    def rd(ch):
        return [src_res[ch]] if src_res is not None else []

    fused = out_dram is None
    it = 0
    for ch in range(DC):
        for hf in range(nsub):
            st, st_r = stage_tile(c)
            kb.dma("sp", st[:, 0:TS], src[ch * 128:(ch + 1) * 128, t0 + hf * TS:t0 + (hf + 1) * TS],
                   reads=rd(ch), writes=[st_r])
            sq, sq_r = stage_tile(c)
            sqb = sq[:, 0:512].bitcast(BF16)
            kb.op("act", lambda: nc.scalar.activation(out=sqb[:, 0:TS], in_=st[:, 0:TS], func=AF.Square),
                  reads=[st_r], writes=[sq_r])
            if fused:
                it += 1
                if it % 2 == 0:
                    kb.op("dve", lambda: nc.vector.tensor_scalar(out=X[:, ch, hf * TS:(hf + 1) * TS], in0=st[:, 0:TS], scalar1=c.sm[:, gcol + ch:gcol + ch + 1],
                                                                 scalar2=None, op0=ALU.mult),
                          reads=[st_r, c.sm_res], pwrites=[c.XA_res])
                else:
                    kb.op("act", lambda: nc.scalar.activation(out=X[:, ch, hf * TS:(hf + 1) * TS], in_=st[:, 0:TS], func=AF.Copy,
                                                              scale=c.sm[:, gcol + ch:gcol + ch + 1]),
                          reads=[st_r, c.sm_res], pwrites=[c.XA_res])
            for j in range(nbk):
                b = hf * nbk + j
                kb.op("pe", lambda: nc.tensor.matmul(c.PS[:, b, 0:bw], c.ones, sqb[:, j * bw:(j + 1) * bw],
                                                     start=(ch == 0), stop=(ch == DC - 1)),
                      reads=[sq_r, c.const_res], writes=[c.PS_res[b]])
    for b in range(nsub * nbk):
        kb.op("act", lambda: nc.scalar.activation(out=c.rstd[:, b * bw:(b + 1) * bw], in_=c.PS[:, b, 0:bw], func=AF.Sqrt,
                                                  bias=c.cf[:, 0:1], scale=1.0 / D),
              reads=[c.PS_res[b], c.const_res], writes=[c.rstd_res])
    kb.op("dve", lambda: nc.vector.reciprocal(out=c.rstd[:, 0:Tn], in_=c.rstd[:, 0:Tn]),
          reads=[c.rstd_res], writes=[c.rstd_res])
    if fused:
        xw = Res("xnorm")
        it = 0
        for ch in range(DC):
            kb.op("dve", lambda: nc.vector.tensor_tensor(out=X[:, ch, :], in0=X[:, ch, :], in1=c.rstd[:, 0:Tn], op=ALU.mult),
                  reads=[c.rstd_res, c.XA_res], pwrites=[xw])
        c.XA_res.writers = dict(xw.writers)
        c.XA_res.readers = {}
        return X
    for ch in range(DC):
        for hf in range(nsub):
            st, st_r = stage_tile(c)
            kb.dma("sp", st[:, 0:TS], src[ch * 128:(ch + 1) * 128, t0 + hf * TS:t0 + (hf + 1) * TS],
                   reads=rd(ch), writes=[st_r])
            if out_dram is None:
                kb.op("dve", lambda: nc.vector.scalar_tensor_tensor(out=X[:, ch, hf * TS:(hf + 1) * TS], in0=st[:, 0:TS],
                                                                    scalar=c.sm[:, gcol + ch:gcol + ch + 1],
                                                                    in1=c.rstd[:, hf * TS:(hf + 1) * TS],
                                                                    op0=ALU.mult, op1=ALU.mult),
                      reads=[st_r, c.rstd_res, c.sm_res], pwrites=[c.XA_res])
            else:
                so, so_r = stage_tile(c)
                kb.op("dve", lambda: nc.vector.scalar_tensor_tensor(out=so[:, 0:TS], in0=st[:, 0:TS],
                                                                    scalar=c.sm[:, gcol + ch:gcol + ch + 1],
                                                                    in1=c.rstd[:, hf * TS:(hf + 1) * TS],
                                                                    op0=ALU.mult, op1=ALU.mult),
                      reads=[st_r, c.rstd_res, c.sm_res], writes=[so_r])
                kb.dma("sp", out_dram[ch * 128:(ch + 1) * 128, t0 + hf * TS:t0 + (hf + 1) * TS], so[:, 0:TS],
                       reads=[so_r])
    return X


def load_x_plain(c, src, src_res, KC, t0, Tn, off=0):
    kb = c.kb
    X = xa(c, off, [KC, Tn], BF16)
    for k in range(KC):
        kb.dma("sp", X[:, k, :], src[k * 128:(k + 1) * 128, t0:t0 + Tn],
               reads=[src_res[k]] if src_res is not None else [], pwrites=[c.XA_res])
    return X


def gemm(c, wloads, units, wbufs=None):
    for _ in gemm_iter(c, wloads, units, wbufs):
        pass


def gemm_iter(c, wloads, units, wbufs=None):
    kb, nc = c.kb, c.nc
    W = {}
    priv = {"rr": 0}
    NWB = c.NW if wbufs is None else len(wbufs)

    def issue_w(wi):
        if wbufs is None:
            i = c.wrr % c.NW
            c.wrr += 1
            wb, wr = c.WB[i], c.WB_res[i]
        else:
            wb, wr = wbufs[priv["rr"] % len(wbufs)]
            priv["rr"] += 1
        off = 0
        views = []
        first = True
        for (wd, r0, kc, c0, cw) in wloads[wi]:
            assert off + kc * cw <= wb.shape[1], (off, kc, cw)
            Wv = wb[:, off:off + kc * cw].rearrange("p (k n) -> p k n", k=kc)
            kstep = max(1, min(kc, 2048 // cw))
            for k0 in range(0, kc, kstep):
                kk = min(kstep, kc - k0)
                src = wd[r0 + k0 * 128:r0 + (k0 + kk) * 128, c0:c0 + cw].rearrange("(k p) n -> p k n", p=128)
                if first:
                    kb.dma("pool", Wv[:, k0:k0 + kk, :], src, writes=[wr])
                    first = False
                else:
                    kb.dma("pool", Wv[:, k0:k0 + kk, :], src, pwrites=[wr])
            views.append(Wv)
            off += kc * cw
        W[wi] = (views, wr)

    nxt = 0
    while nxt < min(NWB, len(wloads)):
        issue_w(nxt)
        nxt += 1
    lastw = -1
    for u in units:
        wi = u["w"]
        if wi != lastw:
            if wi >= 1 and nxt < len(wloads) and nxt <= wi + NWB - 1:
                issue_w(nxt)
                nxt += 1
            lastw = wi
        views, wr = W[wi]
        if "pre" in u:
            u["pre"](u)
        for a in u["accs"]:
            Wv = views[a["piece"]]
            kc = wloads[wi][a["piece"]][2]
            X, xres = a["X"], a["xres"]
            wo, ww, t0, tw, bank = a["wo"], a["ww"], a["t0"], a["tw"], a["bank"]
            k0 = a.get("k0", 0)
            k1 = k0 + a.get("kn", kc - k0)
            for k in range(k0, k1):
                if a["kind"] == "fm":
                    kb.op("pe", lambda: nc.tensor.matmul(c.PS[0:ww, bank, 0:tw], Wv[:, k, wo:wo + ww], X[:, k, t0:t0 + tw],
                                                         start=(k == k0), stop=(k == k1 - 1)),
                          reads=[wr, xres], writes=[c.PS_res[bank]])
                else:
                    kb.op("pe", lambda: nc.tensor.matmul(c.PS[0:tw, bank, 0:ww], X[:, k, t0:t0 + tw], Wv[:, k, wo:wo + ww],
                                                         start=(k == k0), stop=(k == k1 - 1)),
                          reads=[wr, xres], writes=[c.PS_res[bank]])
        u["epi"](u)
        yield 1


def fm_acc(piece, X, xres, t0, tw, bank, wo=0, ww=128):
    return dict(kind="fm", piece=piece, X=X, xres=xres, t0=t0, tw=tw, bank=bank, wo=wo, ww=ww)


def residual_units(c, X, KC, wd, xsrc, xsrc_res, xdst, xdst_res, Tn, alpha, tok0):
    kb, nc = c.kb, c.nc
    units, wloads = [], []
    for nb in range(DC):
        pb = (nb % 4) * 2
        wloads.append([(wd, 0, KC, nb * 128, 128)])
        hold = {}

        def pre(u, nb=nb, hold=hold):
            xo, xo_r = stage_tile(c)
            kb.dma("sp", xo[:, :], xsrc[nb * 128:(nb + 1) * 128, tok0:tok0 + Tn],
                   reads=[xsrc_res[nb]] if xsrc_res is not None else [], writes=[xo_r])
            hold["xo"] = (xo, xo_r)

        def epi(u, nb=nb, pb=pb, hold=hold):
            xo, xo_r = hold["xo"]
            xn, xn_r = stage_tile(c)
            kb.op("dve", lambda: nc.vector.scalar_tensor_tensor(out=v3(xn[:, :]), in0=c.PS[:, pb:pb + 2, :], scalar=alpha,
                                                                in1=v3(xo[:, :]), op0=ALU.mult, op1=ALU.add),
                  reads=[xo_r, c.PS_res[pb], c.PS_res[pb + 1]], writes=[xn_r])
            kb.dma("sp", xdst[nb * 128:(nb + 1) * 128, tok0:tok0 + Tn], xn[:, :], reads=[xn_r],
                   pwrites=[xdst_res[nb]] if xdst_res is not None else [])

        accs = [fm_acc(0, X, c.XA_res, 0, 512, pb), fm_acc(0, X, c.XA_res, 512, 512, pb + 1)]
        units.append(dict(w=nb, accs=accs, epi=epi, pre=pre))
    return wloads, units


def ffn(c, xsrc, xdst, gcol, w_in, w_out, HT, HT_res):
    kb, nc = c.kb, c.nc
    xr = rlist("xr", DC)
    X = load_x_norm(c, xsrc, None, gcol, 0, T)
    units = []
    wloads = []
    for j in range(FF // 128):
        wloads.append([(w_in, 0, DC, j * 128, 128), (w_in, 0, DC, FF + j * 128, 128)])
        for th in range(2):
            pb = ((j * 2 + th) % 2) * 4

            def epi(u, j=j, th=th, pb=pb):
                sg, sg_r = stage_tile(c)
                kb.op("act", lambda: nc.scalar.activation(out=v3(sg[:, :]), in_=c.PS[:, pb:pb + 2, :], func=AF.Silu),
                      reads=[c.PS_res[pb], c.PS_res[pb + 1]], writes=[sg_r])
                hh, hh_r = stage_tile(c)
                hb = hh[:, 0:512].bitcast(BF16)
                kb.op("dve", lambda: nc.vector.tensor_tensor(out=v3(hb), in0=v3(sg[:, :]), in1=c.PS[:, pb + 2:pb + 4, :], op=ALU.mult),
                      reads=[sg_r, c.PS_res[pb + 2], c.PS_res[pb + 3]], writes=[hh_r])
                kb.dma("sp", HT[j * 128:(j + 1) * 128, th * 1024:(th + 1) * 1024], hb, reads=[hh_r], pwrites=[HT_res[j]])

            accs = [fm_acc(0, X, c.XA_res, th * 1024, 512, pb), fm_acc(0, X, c.XA_res, th * 1024 + 512, 512, pb + 1),
                    fm_acc(1, X, c.XA_res, th * 1024, 512, pb + 2), fm_acc(1, X, c.XA_res, th * 1024 + 512, 512, pb + 3)]
            units.append(dict(w=j, accs=accs, epi=epi))
    gemm(c, wloads, units)
    for th in range(2):
        X2 = load_x_plain(c, HT, HT_res, FF // 128, th * 1024, 1024)
        wl, un = residual_units(c, X2, FF // 128, w_out, xsrc, xr, xdst, xr, 1024, 0.5, th * 1024)
        gemm(c, wl, un)


def stage_win(c, xsrc, gcol, w_in, CB, HN, DTW):
    kb, nc = c.kb, c.nc
    X = load_x_norm(c, xsrc, None, gcol, 0, T)
    for k in range(DC):
        kb.dma("sp", HN[k * 128:(k + 1) * 128, :], X[:, k, :], reads=[c.XA_res])
    segs = []
    two = [(0, 128), (128, 128)]
    for i in range(8):
        segs.append((i * 256, 256, two, "copy"))
    for i in range(4):
        segs.append((2048 + i * 256, 256, two, "gelu"))
    for i in range(4):
        segs.append((3072 + i * 256, 256, two, "gelu"))
    for i in range(4):
        segs.append((4096 + i * 256, 256, two, "silu"))
    for i in range(8):
        segs.append((5120 + i * 256, 256, two, "copy"))
    for i in range(4):
        segs.append((7184 + i * 256, 256, two, "copy"))
    segs.append((8208, 256, two, "copy"))
    segs.append((8464, 256, two, "copy"))
    for i in range(2):
        segs.append((8720 + i * 256, 256, [(0, 64), (64, 64), (128, 64), (192, 64)], "copy"))
    segs.append((9232, 64, [(0, 64)], "copy"))
    wloads, units = [], []
    un = 0
    for (c0, cw, subs, func) in segs:
        wloads.append([(w_in, 0, DC, c0, cw)])
        wi = len(wloads) - 1
        for (wo, ww) in subs:
            for th in range(2):
                pb = (un % 4) * 2
                un += 1

                def epi(u, c0=c0, wo=wo, ww=ww, th=th, pb=pb, func=func, un=un):
                    st, st_r = stage_tile(c)
                    ob = st[:, 0:512].bitcast(BF16)
                    if func == "copy" and un % 2 == 0:
                        kb.op("dve", lambda: nc.vector.tensor_copy(out=v3(ob[0:ww, :]), in_=c.PS[0:ww, pb:pb + 2, :]),
                              reads=[c.PS_res[pb], c.PS_res[pb + 1]], writes=[st_r])
                    else:
                        f = {"copy": AF.Copy, "gelu": AF.Gelu, "silu": AF.Silu}[func]
                        kb.op("act", lambda: nc.scalar.activation(out=v3(ob[0:ww, :]), in_=c.PS[0:ww, pb:pb + 2, :], func=f),
                              reads=[c.PS_res[pb], c.PS_res[pb + 1]], writes=[st_r])
                    kb.dma("sp", CB[c0 + wo:c0 + wo + ww, th * 1024:(th + 1) * 1024], ob[0:ww, :], reads=[st_r])

                accs = [fm_acc(0, X, c.XA_res, th * 1024, 512, pb, wo, ww), fm_acc(0, X, c.XA_res, th * 1024 + 512, 512, pb + 1, wo, ww)]
                units.append(dict(w=wi, accs=accs, epi=epi))
    wloads.append([(w_in, 0, DC, 7168, 16), (w_in, 0, DC, 9296, 8)])
    wi = len(wloads) - 1
    for tp in range(8):
        pb = (tp % 2) * 4

        def epi(u, tp=tp, pb=pb):
            for j in range(2):
                tb = tp * 2 + j
                st, st_r = stage_tile(c)
                kb.op("dve", lambda: nc.vector.tensor_copy(out=st[:, 0:16], in_=c.PS[:, pb + 2 * j, 0:16]),
                      reads=[c.PS_res[pb + 2 * j]], writes=[st_r])
                kb.op("dve", lambda: nc.vector.tensor_copy(out=st[:, 16:24], in_=c.PS[:, pb + 2 * j + 1, 0:8]),
                      reads=[c.PS_res[pb + 2 * j + 1]], pwrites=[st_r])
                kb.dma("sp", DTW[tb * 128:(tb + 1) * 128, :], st[:, 0:24], reads=[st_r])

        accs = []
        for j in range(2):
            tb = tp * 2 + j
            accs.append(dict(kind="tm", piece=0, X=X, xres=c.XA_res, t0=tb * 128, tw=128, bank=pb + 2 * j, wo=0, ww=16))
            accs.append(dict(kind="tm", piece=1, X=X, xres=c.XA_res, t0=tb * 128, tw=128, bank=pb + 2 * j + 1, wo=0, ww=8))
        units.append(dict(w=wi, accs=accs, epi=epi))
    gemm(c, wloads, units)


def stage_pool(c, CB, YT, pool_w, gates=None):
    kb, nc = c.kb, c.nc
    PW = 2048 + 16
    P = [xa(c, 0, [PW], F32), xa(c, PW * 4, [PW], F32)]
    P_r = [Res("P0"), Res("P1")]
    XP = xa(c, 2 * PW * 4, [4, T], BF16)
    XP_r = Res("XP")
    PWB = [(xa(c, 32896, [2048], BF16), Res("PWB0")), (xa(c, 36992, [2048], BF16), Res("PWB1"))]
    for i in range(2):
        kb.op("dve", lambda: nc.vector.memset(P[i][:, 0:16], 0.0), pwrites=[P_r[i]])
    for g in range(4):
        w = POOL_WINDOWS[g]
        nsteps = int(math.log2(w))
        for cc in range(4):
            ch = g * 4 + cc
            ab, ab_r = stage_tile(c)
            abf = ab[:, :].bitcast(BF16)
            kb.dma("sp", abf, CB[ch * 128:(ch + 1) * 128, :], writes=[ab_r])
            kb.op("act", lambda: nc.scalar.activation(out=P[0][:, 16:PW], in_=abf, func=AF.Copy),
                  reads=[ab_r], pwrites=[P_r[0]])
            cur = 0
            sh = 1
            for s in range(nsteps):
                nx = 1 - cur
                eng = "dve"
                E = nc.vector
                kb.op(eng, lambda: E.tensor_tensor(out=P[nx][:, 16:PW], in0=P[cur][:, 16:PW], in1=P[cur][:, 16 - sh:PW - sh], op=ALU.add),
                      reads=[P_r[cur]], pwrites=[P_r[nx]])
                cur = nx
                sh *= 2
            kb.op("dve", lambda: nc.vector.scalar_tensor_tensor(out=XP[:, cc, :], in0=P[cur][:, 16:PW], scalar=1.0 / w, in1=abf,
                                                                op0=ALU.mult, op1=ALU.subtract),
                  reads=[P_r[cur], ab_r], pwrites=[XP_r])
            t16, t16_r = stage_tile(c)
            kb.op("dve", lambda: nc.vector.tensor_tensor(out=t16[:, 0:16], in0=P[cur][:, 16:32], in1=c.cf[:, 16 + g * 16:32 + g * 16], op=ALU.mult),
                  reads=[P_r[cur], c.const_res], writes=[t16_r])
            kb.op("dve", lambda: nc.vector.tensor_tensor(out=XP[:, cc, 0:16], in0=t16[:, 0:16], in1=abf[:, 0:16], op=ALU.subtract),
                  reads=[t16_r, ab_r], pwrites=[XP_r])
        if gates is not None:
            gates.step(6)
        wloads = [[(pool_w[g], 0, 4, 0, 512)]]
        units = []
        for nb in range(4):
            for th in range(2):
                pb = ((nb * 2 + th) % 3) * 2

                def epi(u, nb=nb, th=th, pb=pb, g=g):
                    st, st_r = stage_tile(c)
                    ob = st[:, 0:512].bitcast(BF16)
                    col = SMO["pool_scale"] + g * 4 + nb
                    kb.op("act", lambda: nc.scalar.activation(out=v3(ob), in_=c.PS[:, pb:pb + 2, :], func=AF.Copy, scale=c.sm[:, col:col + 1]),
                          reads=[c.PS_res[pb], c.PS_res[pb + 1], c.sm_res], writes=[st_r])
                    kb.dma("sp", YT[g * 512 + nb * 128:g * 512 + (nb + 1) * 128, th * 1024:(th + 1) * 1024], ob, reads=[st_r])

                accs = [fm_acc(0, XP, XP_r, th * 1024, 512, pb, nb * 128, 128), fm_acc(0, XP, XP_r, th * 1024 + 512, 512, pb + 1, nb * 128, 128)]
                units.append(dict(w=0, accs=accs, epi=epi))
        gemm(c, wloads, units, wbufs=PWB)


def stage_sg(c, CB, YT, sg_w):
    kb, nc = c.kb, c.nc
    V = xa(c, 0, [8, T], BF16)
    V_r = rlist("V", 8)
    mean = xa(c, 32768, [T], F32)
    rstd = xa(c, 40960, [T], F32)
    nb_ = xa(c, 49152, [T], F32)
    st_r = Res("stats")
    VT = xa(c, 57344, [16, 1024], BF16)
    VT_r = rlist("VT", 16)
    WmT = xa(c, 90112, [4, 128], BF16)
    WmT_r = Res("WmT")
    tA = xa(c, 91136, [T], F32)
    tA_r = Res("tA")
    tB = xa(c, 99328, [T], F32)
    tB_r = Res("tB")
    sgb = xa(c, 107520, [512], BF16)
    sgb_r = Res("sgb")
    OUTB = xa(c, 108544, [T], BF16)
    OUTB_r = Res("OUTB")
    UB = xa(c, 112640, [T], BF16)
    UB_r = Res("UB")
    kb.op("act", lambda: nc.scalar.activation(out=sgb, in_=c.rw[:, RWO["sg_b"]:RWO["sg_b"] + 512], func=AF.Copy),
          reads=[c.rw_res], writes=[sgb_r])
    for cch in range(8):
        kb.dma("sp", V[:, cch, :], CB[3072 + cch * 128:3072 + (cch + 1) * 128, :], writes=[V_r[cch]])
        sq, sq_r = stage_tile(c)
        sqb = sq[:, :].bitcast(BF16)
        kb.op("act", lambda: nc.scalar.activation(out=sqb, in_=V[:, cch, :], func=AF.Square), reads=[V_r[cch]], writes=[sq_r])
        for j in range(4):
            kb.op("pe", lambda: nc.tensor.matmul(c.PS[:, j, :], c.ones, V[:, cch, j * 512:(j + 1) * 512], start=(cch == 0), stop=(cch == 7)),
                  reads=[V_r[cch], c.const_res], writes=[c.PS_res[j]])
            kb.op("pe", lambda: nc.tensor.matmul(c.PS[:, 4 + j, :], c.ones, sqb[:, j * 512:(j + 1) * 512], start=(cch == 0), stop=(cch == 7)),
                  reads=[sq_r, c.const_res], writes=[c.PS_res[4 + j]])
    kb.op("act", lambda: nc.scalar.activation(out=v3(mean, 4), in_=c.PS[:, 0:4, :], func=AF.Copy, scale=1.0 / 1024),
          reads=c.PS_res[0:4], writes=[st_r])
    kb.op("dve", lambda: nc.vector.tensor_tensor(out=tA, in0=mean, in1=mean, op=ALU.mult), reads=[st_r], writes=[tA_r])
    kb.op("dve", lambda: nc.vector.scalar_tensor_tensor(out=v3(tB, 4), in0=c.PS[:, 4:8, :], scalar=1.0 / 1024, in1=v3(tA, 4),
                                                        op0=ALU.mult, op1=ALU.subtract),
          reads=c.PS_res[4:8] + [tA_r], writes=[tB_r])
    kb.op("act", lambda: nc.scalar.activation(out=rstd, in_=tB, func=AF.Sqrt, bias=c.cf[:, 0:1], scale=1.0),
          reads=[tB_r, c.const_res, st_r], writes=[st_r])
    kb.op("dve", lambda: nc.vector.reciprocal(out=rstd, in_=rstd), reads=[st_r], writes=[st_r])
    kb.op("dve", lambda: nc.vector.scalar_tensor_tensor(out=nb_, in0=mean, scalar=-1.0, in1=rstd, op0=ALU.mult, op1=ALU.mult),
          reads=[st_r], writes=[st_r])
    for cch in range(8):
        kb.op("dve", lambda: nc.vector.tensor_tensor(out=tA, in0=V[:, cch, :], in1=rstd, op=ALU.mult),
              reads=[V_r[cch], st_r], writes=[tA_r])
        kb.op("pool", lambda: nc.gpsimd.tensor_tensor(out=tB, in0=tA, in1=nb_, op=ALU.add), reads=[tA_r, st_r], writes=[tB_r])
        cg = SMO["sg_ln_g"] + cch
        cb = SMO["sg_ln_b"] + cch
        kb.op("act", lambda: nc.scalar.activation(out=V[:, cch, :], in_=tB, func=AF.Identity, scale=c.sm[:, cg:cg + 1], bias=c.sm[:, cb:cb + 1]),
              reads=[tB_r, c.sm_res], writes=[V_r[cch]])
    for tb in range(16):
        bank = tb % 2
        psb = c.PS[:, bank, :].bitcast(BF16)
        for cch in range(8):
            kb.op("pe", lambda: nc.tensor.transpose(psb[:, cch * 128:(cch + 1) * 128], V[:, cch, tb * 128:(tb + 1) * 128], c.ident),
                  reads=[V_r[cch], c.const_res], writes=[c.PS_res[bank]])
        if tb % 2 == 0:
            kb.op("act", lambda: nc.scalar.activation(out=VT[:, tb, :], in_=psb, func=AF.Copy), reads=[c.PS_res[bank]], writes=[VT_r[tb]])
        else:
            kb.op("dve", lambda: nc.vector.tensor_copy(out=VT[:, tb, :], in_=psb), reads=[c.PS_res[bank]], writes=[VT_r[tb]])
    for g in range(4):
        wst, wst_r = stage_tile(c)
        kb.dma("sp", wst[:, 0:128], sg_w[g], writes=[wst_r])
        wm, wm_r = stage_tile(c)
        wmb = wm[:, 0:64].bitcast(BF16)
        kb.op("dve", lambda: nc.vector.tensor_tensor(out=wmb, in0=wst[:, 0:128], in1=c.mf[:, 0, :], op=ALU.mult),
              reads=[wst_r, c.const_res], writes=[wm_r])
        psb = c.PS[:, 2, :].bitcast(BF16)
        kb.op("pe", lambda: nc.tensor.transpose(psb[:, 0:128], wmb, c.ident), reads=[wm_r, c.const_res], writes=[c.PS_res[2]])
        kb.op("act", lambda: nc.scalar.activation(out=WmT[:, g, :], in_=psb[:, 0:128], func=AF.Copy), reads=[c.PS_res[2]], pwrites=[WmT_r])
    for cch in range(8):
        g = cch // 2
        kb.dma("sp", UB, CB[2048 + cch * 128:2048 + (cch + 1) * 128, :], writes=[UB_r])
        for quad in range(4):
            bank = 4 + (cch * 4 + quad) % 4
            for j in range(4):
                tb = quad * 4 + j
                kb.op("pe", lambda: nc.tensor.matmul(c.PS[:, bank, j * 128:(j + 1) * 128], VT[:, tb, cch * 128:(cch + 1) * 128], WmT[:, g, :],
                                                     start=True, stop=False),
                      reads=[VT_r[tb], WmT_r], writes=[c.PS_res[bank]])
                kb.op("pe", lambda: nc.tensor.matmul(c.PS[:, bank, j * 128:(j + 1) * 128], c.mb[0:1, 0, :], sgb[0:1, g * 128:(g + 1) * 128],
                                                     start=False, stop=True),
                      reads=[sgb_r, c.const_res], writes=[c.PS_res[bank]])
            kb.op("dve", lambda: nc.vector.tensor_tensor(out=OUTB[:, quad * 512:(quad + 1) * 512], in0=c.PS[:, bank, :], in1=UB[:, quad * 512:(quad + 1) * 512], op=ALU.mult),
                  reads=[c.PS_res[bank], UB_r], pwrites=[OUTB_r])
        kb.dma("sp", YT[2048 + cch * 128:2048 + (cch + 1) * 128, :], OUTB, reads=[OUTB_r])


def stage_ssd(c, CB, DTW, YT, gates=None):
    kb, nc = c.kb, c.nc
    SEG = 512
    XSf = xa(c, 0, [8, SEG], BF16)
    XS_r = rlist("XSf", 8)
    BTf = xa(c, 8192, [4, SEG], BF16)
    BT_r = rlist("BTf", 4)
    CTf = xa(c, 12288, [4, SEG], BF16)
    CT_r = rlist("CTf", 4)
    ZTf = xa(c, 16384, [8, SEG], BF16)
    ZT_r = Res("ZTf")
    RP = [xa(c, 24576, [SEG + 4], BF16), xa(c, 25616, [SEG + 4], BF16)]
    RP_r = [Res("RP0"), Res("RP1")]
    AC = [xa(c, 26656, [SEG], F32), xa(c, 28704, [SEG], F32)]
    AC_r = [Res("AC0"), Res("AC1")]
    B0 = 30752
    dt_all = xa(c, B0, [32, 16], F32, parts=64)
    dtA_all = xa(c, B0 + 2048, [32, 16], F32, parts=64)
    dt_r = Res("dt")
    Ab = xa(c, B0 + 4096, [16], F32, parts=64)
    Dd = xa(c, B0 + 4160, [16, 64], BF16, parts=64)
    Dd_r = Res("Dd")
    MT = xa(c, B0 + 6208, [16, 64], BF16, parts=64)
    MT_r = Res("MT")
    cbm = xa(c, B0 + 8256, [4, 64], F32, parts=64)
    cbm_r = Res("cbm")
    xs_tok = xa(c, B0 + 9280, [16, 64], BF16, parts=64)
    xst_r = Res("xs_tok")
    xd = xa(c, B0 + 11328, [16, 64], BF16, parts=64)
    xd_r = Res("xd")
    xdw = xa(c, B0 + 13376, [1024], BF16, parts=64)
    xdw_r = Res("xdw")
    Btok = xa(c, B0 + 15424, [512], BF16, parts=64)
    Btok_r = Res("Btok")
    S = xa(c, B0 + 16448, [1024], F32)
    S_r = Res("S")
    Sbf = xa(c, B0 + 20544, [1024], BF16)
    Sbf_r = Res("Sbf")
    ea = xa(c, B0 + 22592, [16], F32, parts=64)
    ea_r = Res("ea")
    cdb = xa(c, B0 + 22656, [16], F32)
    cdb_r = Res("cdb")
    ssq = xa(c, B0 + 22720, [4], F32, parts=64)
    ssq_r = Res("ssq")
    yn = xa(c, B0 + 22784, [1024], BF16, parts=64)
    yn_r = Res("yn")
    YC = xa(c, B0 + 24832, [8, 256], BF16)
    YC_r = Res("YC")
    assert B0 + 24832 + 4096 <= 63488
    UT = c.mf[0:64, 1, 0:64]
    SL = c.mf[0:64, 2, 0:64]
    SEL = c.mf[0:64, 3, :]

    def gstep(k):
        if gates is not None:
            gates.step(k)

    def conv_segment(seg):
        s0 = seg * SEG
        for ch in range(16):
            i = ch % 2
            row = 5120 + ch * 128
            if seg == 0:
                kb.op("dve", lambda: nc.vector.memset(RP[i][:, 0:4], 0.0), writes=[RP_r[i]])
                kb.dma("sp", RP[i][:, 4:SEG + 4], CB[row:row + 128, 0:SEG], reads=[RP_r[i]], writes=[RP_r[i]])
            else:
                kb.dma("sp", RP[i][:, 0:SEG + 4], CB[row:row + 128, s0 - 4:s0 + SEG], writes=[RP_r[i]])
            w = [SMO["conv_w%d" % j] + ch for j in range(4)]
            bcol = SMO["conv_b"] + ch
            kb.op("dve", lambda: nc.vector.tensor_scalar(out=AC[i], in0=RP[i][:, 4:SEG + 4], scalar1=c.sm[:, w[3]:w[3] + 1], scalar2=c.sm[:, bcol:bcol + 1],
                                                         op0=ALU.mult, op1=ALU.add),
                  reads=[RP_r[i], c.sm_res], writes=[AC_r[i]])
            for j in (2, 1, 0):
                kb.op("dve", lambda: nc.vector.scalar_tensor_tensor(out=AC[i], in0=RP[i][:, 1 + j:SEG + 1 + j], scalar=c.sm[:, w[j]:w[j] + 1], in1=AC[i],
                                                                    op0=ALU.mult, op1=ALU.add),
                      reads=[RP_r[i], AC_r[i], c.sm_res], writes=[AC_r[i]])
            if ch < 8:
                dst, dr = XSf[:, ch, :], XS_r[ch]
            elif ch < 12:
                dst, dr = BTf[:, ch - 8, :], BT_r[ch - 8]
            else:
                dst, dr = CTf[:, ch - 12, :], CT_r[ch - 12]
            kb.op("act", lambda: nc.scalar.activation(out=dst, in_=AC[i], func=AF.Silu), reads=[AC_r[i]], writes=[dr])
        for j in range(8):
            kb.dma("sp", ZTf[:, j, :], CB[4096 + j * 128:4096 + (j + 1) * 128, s0:s0 + SEG],
                   writes=[ZT_r] if j == 0 else (), pwrites=[ZT_r] if j > 0 else ())

    kb.dma("sp", dt_all, DTW[:, 0:16].rearrange("(c l) h -> l c h", l=64), writes=[dt_r])
    o = RWO["dt_bias"]
    kb.op("dve", lambda: nc.vector.tensor_tensor(out=dt_all, in0=dt_all, in1=c.rw[0:64, o:o + 16].unsqueeze(1).broadcast_to([64, 32, 16]), op=ALU.add),
          reads=[dt_r, c.rw_res], writes=[dt_r])
    kb.op("act", lambda: nc.scalar.activation(out=dt_all, in_=dt_all, func=AF.Exp), reads=[dt_r], writes=[dt_r])
    kb.op("act", lambda: nc.scalar.activation(out=dt_all, in_=dt_all, func=AF.Ln, bias=1.0), reads=[dt_r], writes=[dt_r])
    o = RWO["a_log"]
    kb.op("act", lambda: nc.scalar.activation(out=Ab, in_=c.rw[0:64, o:o + 16], func=AF.Exp), reads=[c.rw_res, dt_r], writes=[dt_r])
    kb.op("dve", lambda: nc.vector.scalar_tensor_tensor(out=dtA_all, in0=dt_all, scalar=-1.0, in1=Ab.unsqueeze(1).broadcast_to([64, 32, 16]),
                                                        op0=ALU.mult, op1=ALU.mult),
          reads=[dt_r], writes=[dt_r])
    o = RWO["ssm_d"]
    kb.op("dve", lambda: nc.vector.tensor_tensor(out=Dd, in0=c.mb[0:64, 1, 0:64].unsqueeze(1).broadcast_to([64, 16, 64]),
                                                 in1=c.rw[0:64, o:o + 16].unsqueeze(2).broadcast_to([64, 16, 64]), op=ALU.mult),
          reads=[c.const_res, c.rw_res], writes=[Dd_r])
    kb.op("dve", lambda: nc.vector.memset(S, 0.0), writes=[S_r])
    kb.op("dve", lambda: nc.vector.memset(Sbf, 0.0), writes=[Sbf_r])
    psb0 = c.PS[:, 0, :].bitcast(BF16)
    psb1 = c.PS[:, 1, :].bitcast(BF16)
    P = c.PS_res
    gcol = SMO["ssm_norm_g"]

    def v16(ap):
        return ap.rearrange("p a (h l) -> p (a h) l", l=64)

    for ch in range(32):
        if ch % 8 == 0:
            conv_segment(ch // 8)
        t0 = (ch % 8) * 64
        for j in range(8):
            kb.op("pe", lambda: nc.tensor.transpose(psb0[0:64, j * 128:(j + 1) * 128], XSf[:, j, t0:t0 + 64], c.ident),
                  reads=[XS_r[j], c.const_res], writes=[P[0]])
        for g in range(4):
            kb.op("pe", lambda: nc.tensor.transpose(psb1[0:64, g * 128:(g + 1) * 128], BTf[:, g, t0:t0 + 64], c.ident),
                  reads=[BT_r[g], c.const_res], writes=[P[1]])
        kb.op("act", lambda: nc.scalar.activation(out=xs_tok.rearrange("p h l -> p (h l)"), in_=psb0[0:64, :], func=AF.Copy), reads=[P[0]], writes=[xst_r])
        kb.op("act", lambda: nc.scalar.activation(out=Btok, in_=psb1[0:64, 0:512], func=AF.Copy), reads=[P[1]], writes=[Btok_r])
        for j in range(8):
            kb.op("pe", lambda: nc.tensor.transpose(psb0[0:64, j * 128:(j + 1) * 128], ZTf[:, j, t0:t0 + 64], c.ident),
                  reads=[ZT_r, c.const_res], writes=[P[0]])
        zt, zt_r = stage_tile(c)
        Zt = zt[0:64, :].rearrange("p (h l) -> p h l", l=64)
        kb.op("dve", lambda: nc.vector.tensor_tensor(out=Zt, in0=UT.unsqueeze(1).broadcast_to([64, 16, 64]),
                                                     in1=dtA_all[:, ch, :].unsqueeze(2).broadcast_to([64, 16, 64]), op=ALU.mult),
              reads=[dt_r, c.const_res], writes=[zt_r])
        gstep(1)
        for hf in range(2):
            kb.op("pe", lambda: nc.tensor.matmul(c.PS[0:64, 2 + hf, :], SL, zt[0:64, hf * 512:(hf + 1) * 512], start=True, stop=True),
                  reads=[zt_r, c.const_res], writes=[P[2 + hf]])
        kb.op("pe", lambda: nc.tensor.matmul(c.PS[0:64, 4, 256:272], UT, dtA_all[:, ch, :], start=True, stop=True),
              reads=[dt_r, c.const_res], writes=[P[4]])
        for g in range(4):
            kb.op("pe", lambda: nc.tensor.matmul(c.PS[0:64, 4, g * 64:(g + 1) * 64], BTf[:, g, t0:t0 + 64], CTf[:, g, t0:t0 + 64], start=True, stop=True),
                  reads=[BT_r[g], CT_r[g]], writes=[P[4]])
        dc, dc_r = stage_tile(c)
        dec = dc[0:64, :].rearrange("p (h l) -> p h l", l=64)
        kb.op("act", lambda: nc.scalar.activation(out=v3(dc[0:64, :]), in_=c.PS[0:64, 2:4, :], func=AF.Exp), reads=[P[2], P[3]], writes=[dc_r])
        kb.op("act", lambda: nc.scalar.activation(out=ea, in_=c.PS[0:64, 4, 256:272], func=AF.Exp), reads=[P[4]], writes=[ea_r])
        kb.op("dve", lambda: nc.vector.tensor_tensor(out=cbm, in0=c.PS[0:64, 4, 0:256].rearrange("p (g l) -> p g l", l=64),
                                                     in1=UT.unsqueeze(1).broadcast_to([64, 4, 64]), op=ALU.mult),
              reads=[P[4], c.const_res], writes=[cbm_r])
        kb.op("dve", lambda: nc.vector.tensor_tensor(out=MT.rearrange("p (g a) l -> p g a l", a=4), in0=dec.rearrange("p (g a) l -> p g a l", a=4),
                                                     in1=cbm.unsqueeze(2).broadcast_to([64, 4, 4, 64]), op=ALU.mult),
              reads=[dc_r, cbm_r], writes=[MT_r])
        kb.op("dve", lambda: nc.vector.tensor_tensor(out=xd, in0=xs_tok, in1=dt_all[:, ch, :].unsqueeze(2).broadcast_to([64, 16, 64]), op=ALU.mult),
              reads=[xst_r, dt_r], writes=[xd_r])
        kb.op("dve", lambda: nc.vector.tensor_tensor(out=xdw.rearrange("p (h l) -> p h l", l=64), in0=xd, in1=dec[:, :, 63:64].broadcast_to([64, 16, 64]), op=ALU.mult),
              reads=[xd_r, dc_r], writes=[xdw_r])
        gstep(1)
        for h in range(16):
            o_ap = c.PS[0:64, 5 + h // 8, (h % 8) * 64:(h % 8 + 1) * 64]
            kb.op("pe", lambda: nc.tensor.matmul(o_ap, MT[:, h, :], xd[:, h, :], start=True, stop=False),
                  reads=[MT_r, xd_r], writes=[P[5 + h // 8]])
            kb.op("pe", lambda: nc.tensor.matmul(o_ap, Dd[:, h, :], xs_tok[:, h, :], start=False, stop=True),
                  reads=[Dd_r, xst_r], writes=[P[5 + h // 8]])
        for g in range(4):
            kb.op("pe", lambda: nc.tensor.matmul(c.PS[0:64, 2 + g // 2, (g % 2) * 256:(g % 2 + 1) * 256], CTf[:, g, t0:t0 + 64], Sbf[:, g * 256:(g + 1) * 256],
                                                 start=True, stop=True),
                  reads=[CT_r[g], Sbf_r], writes=[P[2 + g // 2]])
        t1, t1_r = stage_tile(c)
        t1v = t1[0:64, :].rearrange("p (h l) -> p h l", l=64)
        kb.op("dve", lambda: nc.vector.tensor_tensor(out=t1v, in0=v16(c.PS[0:64, 2:4, :]), in1=ea.unsqueeze(2).broadcast_to([64, 16, 64]), op=ALU.mult),
              reads=[P[2], P[3], ea_r], writes=[t1_r])
        kb.op("dve", lambda: nc.vector.tensor_tensor(out=t1v, in0=t1v, in1=v16(c.PS[0:64, 5:7, :]), op=ALU.add),
              reads=[t1_r, P[5], P[6]], writes=[t1_r])
        yz, yz_r = stage_tile(c)
        kb.op("dve", lambda: nc.vector.tensor_tensor(out=yz[0:64, :], in0=t1[0:64, :], in1=psb0[0:64, :], op=ALU.mult),
              reads=[t1_r, P[0]], writes=[yz_r])
        sq, sq_r = stage_tile(c)
        kb.op("dve", lambda: nc.vector.tensor_tensor(out=sq[0:64, :], in0=yz[0:64, :], in1=yz[0:64, :], op=ALU.mult), reads=[yz_r], writes=[sq_r])
        kb.op("dve", lambda: nc.vector.tensor_reduce(out=ssq, in_=sq[0:64, :].rearrange("p (g q) -> p g q", g=4), axis=AX.X, op=ALU.add),
              reads=[sq_r], writes=[ssq_r])
        kb.op("act", lambda: nc.scalar.activation(out=ssq, in_=ssq, func=AF.Sqrt, bias=c.cf[0:64, 0:1], scale=1.0 / 256), reads=[ssq_r, c.const_res], writes=[ssq_r])
        kb.op("dve", lambda: nc.vector.reciprocal(out=ssq, in_=ssq), reads=[ssq_r], writes=[ssq_r])
        kb.op("dve", lambda: nc.vector.tensor_tensor(out=yn.rearrange("p (g q) -> p g q", g=4), in0=yz[0:64, :].rearrange("p (g q) -> p g q", g=4),
                                                     in1=ssq.unsqueeze(2).broadcast_to([64, 4, 256]), op=ALU.mult),
              reads=[yz_r, ssq_r], writes=[yn_r])
        gstep(1)
        for j in range(8):
            kb.op("pe", lambda: nc.tensor.transpose(psb1[:, j * 64:(j + 1) * 64], yn[:, j * 128:(j + 1) * 128], c.mb[0:64, 1, 0:64]),
                  reads=[yn_r, c.const_res], writes=[P[1]])
        slot = ch % 4
        kb.op("dve", lambda: nc.vector.tensor_tensor(out=YC[:, :, slot * 64:(slot + 1) * 64], in0=psb1[:, 0:512].rearrange("p (j l) -> p j l", l=64),
                                                     in1=c.sm[:, gcol:gcol + 8].unsqueeze(2).broadcast_to([128, 8, 64]), op=ALU.mult),
              reads=[P[1], c.sm_res], pwrites=[YC_r])
        if slot == 3:
            tq = ch // 4
            kb.dma("sp", YT[3072:4096, tq * 256:(tq + 1) * 256].rearrange("(j p) t -> p j t", p=128), YC, reads=[YC_r])
        if ch == 31:
            break
        for g in range(4):
            kb.op("pe", lambda: nc.tensor.matmul(c.PS[:, g // 2, (g % 2) * 256:(g % 2 + 1) * 256], Btok[:, g * 128:(g + 1) * 128], xdw[:, g * 256:(g + 1) * 256],
                                                 start=True, stop=True),
                  reads=[Btok_r, xdw_r], writes=[P[g // 2]])
        kb.op("pe", lambda: nc.tensor.matmul(c.PS[:, 4, 288:304], SEL, ea, start=True, stop=True), reads=[ea_r, c.const_res], writes=[P[4]])
        kb.op("act", lambda: nc.scalar.activation(out=cdb, in_=c.PS[:, 4, 288:304], func=AF.Copy), reads=[P[4]], writes=[cdb_r])
        kb.op("dve", lambda: nc.vector.tensor_tensor(out=S.rearrange("p (h l) -> p h l", l=64), in0=S.rearrange("p (h l) -> p h l", l=64),
                                                     in1=cdb.unsqueeze(2).broadcast_to([128, 16, 64]), op=ALU.mult),
              reads=[S_r, cdb_r], writes=[S_r])
        kb.op("dve", lambda: nc.vector.tensor_tensor(out=v3(S), in0=v3(S), in1=c.PS[:, 0:2, :], op=ALU.add), reads=[S_r, P[0], P[1]], writes=[S_r])
        kb.op("act", lambda: nc.scalar.activation(out=Sbf, in_=S, func=AF.Copy), reads=[S_r], writes=[Sbf_r])


def stage_rope(c, CB, rope_d, gates=None):
    kb, nc = c.kb, c.nc
    H = 1024
    tabs = [xa(c, i * 4096, [H], F32) for i in range(4)]
    xin = [xa(c, 16384, [H], BF16), xa(c, 18432, [H], BF16)]
    xin_r = [Res("xin0"), Res("xin1")]
    t1 = [xa(c, 20480, [H], F32), xa(c, 24576, [H], F32)]
    t1_r = [Res("t10"), Res("t11")]
    t2 = [xa(c, 28672, [H], F32), xa(c, 32768, [H], F32)]
    t2_r = [Res("t20"), Res("t21")]
    ob = [xa(c, 36864, [H], BF16), xa(c, 38912, [H], BF16)]
    ob_r = [Res("ob0"), Res("ob1")]
    blocks = [(7184 + i * 128, 128, 0) for i in range(8)] + [(8208 + i * 128, 128, 0) for i in range(2)]
    blocks += [(8720 + i * 128, 128, 1) for i in range(4)] + [(9232, 64, 1)]
    bi = 0
    for th in range(2):
        tab_r = Res("tabs%d" % th)
        for i in range(4):
            kb.dma("sp", tabs[i], rope_d[i][:, th * H:(th + 1) * H], writes=[tab_r, t1_r[0], t1_r[1], t2_r[0], t2_r[1]] if i == 0 else (),
                   pwrites=[tab_r] if i > 0 else ())
        for (r0, rows, kind) in blocks:
            i = bi % 2
            bi += 1
            pb = i * 2
            cosT, sinT = tabs[2 * kind], tabs[2 * kind + 1]
            Rm = c.mb[0:rows, 2 + kind, 0:rows]
            kb.dma("sp", xin[i][0:rows, :], CB[r0:r0 + rows, th * H:(th + 1) * H], writes=[xin_r[i]])
            for j in range(2):
                kb.op("pe", lambda: nc.tensor.matmul(c.PS[0:rows, pb + j, :], Rm, xin[i][0:rows, j * 512:(j + 1) * 512], start=True, stop=True),
                      reads=[xin_r[i], c.const_res], writes=[c.PS_res[pb + j]])
            kb.op("dve", lambda: nc.vector.tensor_tensor(out=t1[i][0:rows, :], in0=xin[i][0:rows, :], in1=cosT[0:rows, :], op=ALU.mult),
                  reads=[xin_r[i], tab_r], writes=[t1_r[i]])
            kb.op("dve", lambda: nc.vector.tensor_tensor(out=v3(t2[i][0:rows, :], 2), in0=c.PS[0:rows, pb:pb + 2, :], in1=v3(sinT[0:rows, :], 2), op=ALU.mult),
                  reads=c.PS_res[pb:pb + 2] + [tab_r], writes=[t2_r[i]])
            kb.op("dve", lambda: nc.vector.tensor_tensor(out=ob[i][0:rows, :], in0=t1[i][0:rows, :], in1=t2[i][0:rows, :], op=ALU.add),
                  reads=[t1_r[i], t2_r[i]], writes=[ob_r[i]])
            kb.dma("sp", CB[r0:r0 + rows, th * H:(th + 1) * H], ob[i][0:rows, :], reads=[ob_r[i]])
            if gates is not None:
                gates.step(1)


class GateGen:
    def __init__(self, c, HN, w_gate, GS):
        self.c, self.HN, self.w_gate, self.GS = c, HN, w_gate, GS
        self.it = self._gen()
        self.done = False
        self.nunits = 0
        self.banks = [6, 7]

    def step(self, k):
        for _ in range(k):
            if self.done:
                return
            try:
                next(self.it)
                self.nunits += 1
            except StopIteration:
                self.done = True

    def drain(self):
        while not self.done:
            self.step(64)

    def _gen(self):
        c = self.c
        kb, nc = c.kb, c.nc
        XH = xa(c, 65536, [DC, 1024], BF16)
        gst = [xa(c, 63488, [512], BF16), xa(c, 64512, [512], BF16)]
        gst_r = [Res("gst0"), Res("gst1")]
        prev = None
        for th in range(2):
            tok0 = th * 1024
            xh_r = Res("XH%d" % th)
            for k in range(DC):
                kb.dma("sp", XH[:, k, :], self.HN[k * 128:(k + 1) * 128, tok0:tok0 + 1024], pwrites=[xh_r],
                       writes=[prev] if (k == 0 and prev is not None) else ())
            prev = xh_r
            wloads = []
            for i in range(4):
                for nb in range(DC):
                    wloads.append([(self.w_gate[i], 0, DC, nb * 128, 128)])

            def unit_gen(th=th, tok0=tok0, xh_r=xh_r):
                un = 0
                for i in range(4):
                    for nb in range(DC):
                        for tq in range(2):
                            bank = self.banks[un % len(self.banks)]
                            un += 1

                            def epi(u, i=i, nb=nb, tq=tq, bank=bank):
                                j = bank % 2
                                kb.op("act", lambda: nc.scalar.activation(out=gst[j], in_=c.PS[:, bank, :], func=AF.Sigmoid),
                                      reads=[c.PS_res[bank]], writes=[gst_r[j]])
                                kb.dma("sp", self.GS[i, nb * 128:(nb + 1) * 128, tok0 + tq * 512:tok0 + (tq + 1) * 512], gst[j], reads=[gst_r[j]])

                            yield dict(w=i * DC + nb, accs=[fm_acc(0, XH, xh_r, tq * 512, 512, bank)], epi=epi)

            for _ in gemm_iter(c, wloads, unit_gen()):
                yield 1


def stage_dsa(c, CB, DTW, YT, gates):
    kb, nc = c.kb, c.nc
    P = c.PS_res
    KR = xa(c, 0, [2, T], BF16)
    KI = xa(c, 8192, [T], BF16, parts=64)
    ld_r = Res("loads")
    VTk = xa(c, 12288, [16, 256], BF16)
    VT_r = Res("VTk")
    QRq = [xa(c, 20480, [8, 128], BF16), xa(c, 22528, [8, 128], BF16)]
    QRq_r = [Res("QRq0"), Res("QRq1")]
    QIq = [xa(c, 24576, [8, 128], BF16, parts=64), xa(c, 26624, [8, 128], BF16, parts=64)]
    QIq_r = [Res("QIq0"), Res("QIq1")]
    acc = xa(c, 28672, [T], F32)
    acc_r = Res("acc")
    vtmp = xa(c, 28672, [2, T], BF16)
    work = xa(c, 36864, [T], F32)
    work_r = Res("work")
    maskb = xa(c, 45056, [T], BF16)
    maskb_r = Res("maskb")
    MTs = [xa(c, 49152, [16, 128], BF16), xa(c, 53248, [16, 128], BF16)]
    MTs_r = [Res("MTs0"), Res("MTs1")]
    wi_all = xa(c, 57344, [16, 8], F32)
    mx = xa(c, 57856, [8], F32)
    mx_r = Res("mx")
    Pt = [xa(c, 57888, [512], BF16), xa(c, 58912, [512], BF16)]
    Pt_r = [Res("Pt0"), Res("Pt1")]
    Pm = [xa(c, 59936, [512], BF16), xa(c, 60960, [512], BF16)]
    Pm_r = [Res("Pm0"), Res("Pm1")]
    ob = xa(c, 61984, [512], BF16)
    ob_r = Res("ob")
    for g in range(2):
        kb.dma("sp", KR[:, g, :], CB[8208 + g * 128:8208 + (g + 1) * 128, :], pwrites=[ld_r])
        kb.dma("sp", vtmp[:, g, :], CB[8464 + g * 128:8464 + (g + 1) * 128, :], pwrites=[acc_r])
    kb.dma("sp", KI, CB[9232:9296, :], pwrites=[ld_r])
    kb.dma("sp", wi_all, DTW[:, 16:24].rearrange("(b p) h -> p b h", p=128), pwrites=[ld_r])
    for tq in range(4):
        psb = c.PS[:, tq % 2, :].bitcast(BF16)
        for j in range(4):
            tb = tq * 4 + j
            for g in range(2):
                kb.op("pe", lambda: nc.tensor.transpose(psb[:, j * 256 + g * 128:j * 256 + (g + 1) * 128], vtmp[:, g, tb * 128:(tb + 1) * 128], c.ident),
                      reads=[acc_r, c.const_res], writes=[P[tq % 2]])
        kb.op("act", lambda: nc.scalar.activation(out=VTk[:, tq * 4:(tq + 1) * 4, :].rearrange("p a b -> p (a b)"), in_=psb, func=AF.Copy),
              reads=[P[tq % 2]], pwrites=[VT_r])
    scale = 128.0 ** -0.5

    def load_qr(qb):
        q0 = qb * 128
        kb.dma("sp", QRq[qb % 2], CB[7184:8208, q0:q0 + 128].rearrange("(h p) t -> p h t", p=128), writes=[QRq_r[qb % 2]])

    def load_qi(qb):
        q0 = qb * 128
        kb.dma("sp", QIq[qb % 2], CB[8720:9232, q0:q0 + 128].rearrange("(h p) t -> p h t", p=64), writes=[QIq_r[qb % 2]])

    def indexer(qb):
        n = (qb + 1) * 128
        ngr = (n + 511) // 512
        it = 0
        for h in range(8):
            for kg in range(ngr):
                kw = min(512, n - kg * 512)
                bank = it % 2
                it += 1
                kb.op("pe", lambda: nc.tensor.matmul(c.PS[:, bank, 0:kw], QIq[qb % 2][:, h, :], KI[:, kg * 512:kg * 512 + kw], start=True, stop=True),
                      reads=[ld_r, QIq_r[qb % 2]], writes=[P[bank]])
                rl, rl_r = stage_tile(c)
                kb.op("act", lambda: nc.scalar.activation(out=rl[:, 0:kw], in_=c.PS[:, bank, 0:kw], func=AF.Relu), reads=[P[bank]], writes=[rl_r])
                if h == 0:
                    kb.op("dve", lambda: nc.vector.tensor_scalar_mul(out=acc[:, kg * 512:kg * 512 + kw], in0=rl[:, 0:kw], scalar1=wi_all[:, qb, 0:1]),
                          reads=[rl_r, ld_r], writes=[acc_r] if kg == 0 else (), pwrites=[acc_r] if kg > 0 else ())
                else:
                    kb.op("dve", lambda: nc.vector.scalar_tensor_tensor(out=acc[:, kg * 512:kg * 512 + kw], in0=rl[:, 0:kw], scalar=wi_all[:, qb, h:h + 1],
                                                                        in1=acc[:, kg * 512:kg * 512 + kw], op0=ALU.mult, op1=ALU.add),
                          reads=[rl_r, ld_r, acc_r], writes=[acc_r])
        kb.op("dve", lambda: nc.vector.memset(acc[0:64, n - 64:n], NEG), reads=[acc_r], writes=[acc_r])

    def topk(qb):
        n = (qb + 1) * 128
        if qb >= 2:
            kb.op("act", lambda: nc.scalar.activation(out=work[:, 0:n], in_=acc[:, 0:n], func=AF.Copy), reads=[acc_r], writes=[work_r])
            for r in range(32):
                kb.op("dve", lambda: nc.vector.max(out=mx, in_=work[:, 0:n]), reads=[work_r], writes=[mx_r])
                if r < 31:
                    kb.op("dve", lambda: nc.vector.match_replace(out=work[:, 0:n], in_to_replace=mx, in_values=work[:, 0:n], imm_value=NEG),
                          reads=[mx_r, work_r], writes=[work_r])
            thr, thr_reads = mx[:, 7:8], [mx_r]
        else:
            thr, thr_reads = c.cf[:, 1:2], [c.const_res]
        kb.op("dve", lambda: nc.vector.tensor_single_scalar(out=maskb[:, 0:n], in_=acc[:, 0:n], scalar=thr, op=ALU.is_ge),
              reads=[acc_r] + thr_reads, writes=[maskb_r])

    def mask_T(qb):
        nk = qb + 1
        M, M_r = MTs[qb % 2], MTs_r[qb % 2]
        for kb0 in range(0, nk, 8):
            cnt = min(8, nk - kb0)
            bank = kb0 // 8
            psb = c.PS[:, bank, :].bitcast(BF16)
            for j in range(cnt):
                kbi = kb0 + j
                kb.op("pe", lambda: nc.tensor.transpose(psb[:, j * 128:(j + 1) * 128], maskb[:, kbi * 128:(kbi + 1) * 128], c.ident),
                      reads=[maskb_r, c.const_res], writes=[P[bank]])
            kb.op("act", lambda: nc.scalar.activation(out=M[:, kb0:kb0 + cnt, :].rearrange("p a b -> p (a b)"), in_=psb[:, 0:cnt * 128], func=AF.Copy),
                  reads=[P[bank]], writes=[M_r] if kb0 == 0 else (), pwrites=[M_r] if kb0 > 0 else ())

    def attention(qb):
        nk = qb + 1
        q0 = qb * 128
        M, M_r = MTs[qb % 2], MTs_r[qb % 2]
        Q, Q_r = QRq[qb % 2], QRq_r[qb % 2]
        for g in range(2):
            for kbi in range(nk):
                sb = 2 + kbi % 2
                j = kbi % 2
                kb.op("pe", lambda: nc.tensor.matmul(c.PS[:, sb, :].rearrange("p (h q) -> p h q", h=4), KR[:, g, kbi * 128:(kbi + 1) * 128],
                                                     Q[:, 4 * g:4 * g + 4, :], start=True, stop=True),
                      reads=[ld_r, Q_r], writes=[P[sb]])
                kb.op("act", lambda: nc.scalar.activation(out=Pt[j], in_=c.PS[:, sb, :], func=AF.Exp, scale=scale), reads=[P[sb]], writes=[Pt_r[j]])
                kb.op("pool", lambda: nc.gpsimd.tensor_tensor(out=Pm[j].rearrange("p (h q) -> p h q", h=4), in0=Pt[j].rearrange("p (h q) -> p h q", h=4),
                                                              in1=M[:, kbi, :].unsqueeze(1).broadcast_to([128, 4, 128]), op=ALU.mult),
                      reads=[Pt_r[j], M_r], writes=[Pm_r[j]])
                kb.op("pe", lambda: nc.tensor.matmul(c.PS[:, 4, :], VTk[:, kbi, g * 128:(g + 1) * 128], Pm[j], start=(kbi == 0), stop=(kbi == nk - 1)),
                      reads=[VT_r, Pm_r[j]], writes=[P[4]])
                kb.op("pe", lambda: nc.tensor.matmul(c.PS[:, 5, :], c.ones, Pm[j], start=(kbi == 0), stop=(kbi == nk - 1)),
                      reads=[c.const_res, Pm_r[j]], writes=[P[5]])
            o_t, o_r = stage_tile(c)
            kb.op("act", lambda: nc.scalar.activation(out=o_t[:, 0:512], in_=c.PS[:, 4, :], func=AF.Copy), reads=[P[4]], writes=[o_r])
            kb.op("act", lambda: nc.scalar.activation(out=o_t[:, 512:1024], in_=c.PS[:, 5, :], func=AF.Ln), reads=[P[5]], pwrites=[o_r])
            kb.op("act", lambda: nc.scalar.activation(out=o_t[:, 512:1024], in_=o_t[:, 512:1024], func=AF.Exp, scale=-1.0), reads=[o_r], writes=[o_r])
            kb.op("pool", lambda: nc.gpsimd.tensor_tensor(out=ob, in0=o_t[:, 0:512], in1=o_t[:, 512:1024], op=ALU.mult), reads=[o_r], writes=[ob_r])
            kb.dma("sp", YT[4096 + 4 * g * 128:4096 + (4 * g + 4) * 128, q0:q0 + 128].rearrange("(h p) t -> p h t", p=128),
                   ob.rearrange("p (h q) -> p h q", h=4), reads=[ob_r])

    load_qr(0)
    load_qi(0)
    indexer(0)
    for qb in range(16):
        if qb < 15:
            load_qi(qb + 1)
        topk(qb)
        if qb > 0:
            attention(qb - 1)
        if qb < 15:
            load_qr(qb + 1)
        if gates is not None:
            gates.step(int(round(512.0 * (qb + 1) / 136.0)) + 1)
        if qb < 15:
            indexer(qb + 1)
        mask_T(qb)
    attention(15)
    if gates is not None:
        gates.drain()


BR_ROWS = ((0, 16), (2048, 8), (3072, 8), (4096, 8))


def stage_merge(c, YT, GS, w_branch, MTd):
    kb, nc = c.kb, c.nc
    for th in range(2):
        tok0 = th * 1024
        XY = load_x_plain(c, YT, None, 40, tok0, 1024, off=0)
        wloads, units = [], []
        for nb in range(DC):
            wloads.append([(w_branch, 0, 40, nb * 128, 128)])
            for tq in range(2):
                pb = ((nb * 2 + tq) % 2) * 4
                hold = {}

                def pre(u, nb=nb, tq=tq, hold=hold):
                    gt, gt_r = stage_tile(c)
                    kb.dma("sp", gt[:, :].bitcast(BF16).rearrange("p (i t) -> p i t", i=4),
                           GS[:, nb * 128:(nb + 1) * 128, tok0 + tq * 512:tok0 + (tq + 1) * 512].rearrange("i p t -> p i t"), writes=[gt_r])
                    hold["gt"] = (gt, gt_r)

                def epi(u, nb=nb, tq=tq, pb=pb, hold=hold):
                    gt, gt_r = hold["gt"]
                    gv = gt[:, :].bitcast(BF16).rearrange("p (i t) -> p i t", i=4)
                    ma, ma_r = stage_tile(c)
                    mb_, mb_r = stage_tile(c)
                    kb.op("dve", lambda: nc.vector.tensor_tensor(out=v3(ma[:, :]), in0=c.PS[:, pb:pb + 2, :], in1=gv[:, 0:2, :], op=ALU.mult),
                          reads=[gt_r, c.PS_res[pb], c.PS_res[pb + 1]], writes=[ma_r])
                    kb.op("dve", lambda: nc.vector.tensor_tensor(out=v3(mb_[:, :]), in0=c.PS[:, pb + 2:pb + 4, :], in1=gv[:, 2:4, :], op=ALU.mult),
                          reads=[gt_r, c.PS_res[pb + 2], c.PS_res[pb + 3]], writes=[mb_r])
                    kb.op("dve", lambda: nc.vector.tensor_tensor(out=ma[:, :], in0=ma[:, :], in1=mb_[:, :], op=ALU.add),
                          reads=[ma_r, mb_r], writes=[ma_r])
                    ob = mb_[:, 0:256].bitcast(BF16)
                    kb.op("dve", lambda: nc.vector.tensor_tensor(out=ob, in0=ma[:, 0:512], in1=ma[:, 512:1024], op=ALU.add),
                          reads=[ma_r, mb_r], writes=[mb_r])
                    kb.dma("sp", MTd[nb * 128:(nb + 1) * 128, tok0 + tq * 512:tok0 + (tq + 1) * 512], ob, reads=[mb_r])

                accs = []
                for i in range(4):
                    r0, kci = BR_ROWS[i]
                    a = fm_acc(0, XY, c.XA_res, tq * 512, 512, pb + i)
                    a["k0"] = r0 // 128
                    a["kn"] = kci
                    accs.append(a)
                units.append(dict(w=nb, accs=accs, epi=epi, pre=pre))
        gemm(c, wloads, units)


def stage_wout(c, MTd, w_out, XS):
    xr = rlist("xr", DC)
    for th in range(2):
        X = load_x_plain(c, MTd, None, DC, th * 1024, 1024)
        wl, un = residual_units(c, X, DC, w_out, XS, xr, XS, xr, 1024, 1.0, th * 1024)
        gemm(c, wl, un)


def stage_cross(c, XS, memT, w_q, w_kv, w_o, QMd):
    kb, nc = c.kb, c.nc
    P = c.PS_res
    KM = c.kvm[:, 0:1024].rearrange("p (h m) -> p h m", h=4)
    VM = c.kvm[:, 1024:2048].rearrange("p (a b) -> p a b", a=2)
    kv_r = Res("kv")
    X = load_x_norm(c, memT, None, SMO["g_mem"], 0, MEM)
    wloads, units = [], []
    for j in range(2):
        wloads.append([(w_kv, 0, DC, j * 256, 256)])
        for s in range(2):
            hh = j * 2 + s
            pb = hh * 2 % 8

            def epi(u, hh=hh, pb=pb):
                kb.op("act", lambda: nc.scalar.activation(out=KM[:, hh, :], in_=c.PS[:, pb, 0:256], func=AF.Copy), reads=[P[pb]], pwrites=[kv_r])

            units.append(dict(w=j, accs=[fm_acc(0, X, c.XA_res, 0, 256, pb, s * 128, 128)], epi=epi))
    for j in range(2):
        wloads.append([(w_kv, 0, DC, 512 + j * 256, 256)])
        for mb in range(2):
            pb = (j * 2 + mb) * 2 % 8 + 1

            def epi(u, j=j, mb=mb, pb=pb):
                kb.op("act", lambda: nc.scalar.activation(out=VM[:, mb, j * 256:(j + 1) * 256], in_=c.PS[:, pb, 0:256], func=AF.Copy), reads=[P[pb]], pwrites=[kv_r])

            units.append(dict(w=2 + j, accs=[dict(kind="tm", piece=0, X=X, xres=c.XA_res, t0=mb * 128, tw=128, bank=pb, wo=0, ww=256)], epi=epi))
    gemm(c, wloads, units)
    kb.barrier()
    X = load_x_norm(c, XS, None, SMO["g_cross"], 0, T)
    wloads, units = [], []
    for j in range(2):
        wloads.append([(w_q, 0, DC, j * 256, 256)])
        for s in range(2):
            for th in range(2):
                un = (j * 2 + s) * 2 + th
                pb = (un % 4) * 2

                def epi(u, j=j, s=s, th=th, pb=pb):
                    st, st_r = stage_tile(c)
                    ob = st[:, 0:512].bitcast(BF16)
                    kb.op("act", lambda: nc.scalar.activation(out=v3(ob), in_=c.PS[:, pb:pb + 2, :], func=AF.Copy), reads=[P[pb], P[pb + 1]], writes=[st_r])
                    kb.dma("sp", QMd[(j * 2 + s) * 128:(j * 2 + s + 1) * 128, th * 1024:(th + 1) * 1024], ob, reads=[st_r])

                accs = [fm_acc(0, X, c.XA_res, th * 1024, 512, pb, s * 128, 128), fm_acc(0, X, c.XA_res, th * 1024 + 512, 512, pb + 1, s * 128, 128)]
                units.append(dict(w=j, accs=accs, epi=epi))
    gemm(c, wloads, units)
    kb.barrier()
    QM = xa(c, 0, [4, T], BF16)
    qm_r = Res("QM")
    for h in range(4):
        kb.dma("sp", QM[:, h, :], QMd[h * 128:(h + 1) * 128, :], pwrites=[qm_r])
    OM = xa(c, 16384, [4, T], BF16)
    om_r = Res("OM")
    Pt = [xa(c, 32768, [512], BF16), xa(c, 33792, [512], BF16)]
    Pt_r = [Res("cPt0"), Res("cPt1")]
    rs = xa(c, 34816, [512], F32)
    rs_r = Res("crs")
    scale = 128.0 ** -0.5
    it = 0
    for h in range(4):
        for qg in range(4):
            Ob = 4 + (it % 2) * 2
            Sb = Ob + 1
            it += 1
            for mb in range(2):
                sb = mb
                kb.op("pe", lambda: nc.tensor.matmul(c.PS[:, sb, :], KM[:, h, mb * 128:(mb + 1) * 128], QM[:, h, qg * 512:(qg + 1) * 512], start=True, stop=True),
                      reads=[kv_r, qm_r], writes=[P[sb]])
                kb.op("act", lambda: nc.scalar.activation(out=Pt[sb], in_=c.PS[:, sb, :], func=AF.Exp, scale=scale), reads=[P[sb]], writes=[Pt_r[sb]])
                kb.op("pe", lambda: nc.tensor.matmul(c.PS[:, Ob, :], VM[:, mb, h * 128:(h + 1) * 128], Pt[sb], start=(mb == 0), stop=(mb == 1)),
                      reads=[kv_r, Pt_r[sb]], writes=[P[Ob]])
                kb.op("pe", lambda: nc.tensor.matmul(c.PS[:, Sb, :], c.ones, Pt[sb], start=(mb == 0), stop=(mb == 1)),
                      reads=[c.const_res, Pt_r[sb]], writes=[P[Sb]])
            kb.op("dve", lambda: nc.vector.reciprocal(out=rs, in_=c.PS[:, Sb, :]), reads=[P[Sb]], writes=[rs_r])
            kb.op("dve", lambda: nc.vector.tensor_tensor(out=OM[:, h, qg * 512:(qg + 1) * 512], in0=c.PS[:, Ob, :], in1=rs, op=ALU.mult),
                  reads=[P[Ob], rs_r], pwrites=[om_r])
    xr = rlist("xr", DC)
    for th in range(2):
        wloads, units = [], []
        tok0 = th * 1024
        for nb in range(DC):
            pb = (nb % 4) * 2
            if nb % 4 == 0:
                wloads.append([(w_o, 0, 4, nb * 128, 512)])
            hold = {}

            def pre(u, nb=nb, hold=hold):
                xo, xo_r = stage_tile(c)
                kb.dma("sp", xo[:, :], XS[nb * 128:(nb + 1) * 128, tok0:tok0 + 1024], reads=[xr[nb]], writes=[xo_r])
                hold["xo"] = (xo, xo_r)

            def epi(u, nb=nb, pb=pb, hold=hold):
                xo, xo_r = hold["xo"]
                xn, xn_r = stage_tile(c)
                kb.op("dve", lambda: nc.vector.tensor_tensor(out=v3(xn[:, :]), in0=c.PS[:, pb:pb + 2, :], in1=v3(xo[:, :]), op=ALU.add),
                      reads=[xo_r, P[pb], P[pb + 1]], writes=[xn_r])
                kb.dma("sp", XS[nb * 128:(nb + 1) * 128, tok0:tok0 + 1024], xn[:, :], reads=[xn_r], pwrites=[xr[nb]])

            accs = [fm_acc(0, OM, om_r, tok0, 512, pb, (nb % 4) * 128, 128), fm_acc(0, OM, om_r, tok0 + 512, 512, pb + 1, (nb % 4) * 128, 128)]
            units.append(dict(w=nb // 4, accs=accs, epi=epi, pre=pre))
        gemm(c, wloads, units)


STAGES = ("ffn1", "win", "pool", "sg", "ssd", "dsa", "merge", "cross", "ffn2")


def build(plan, dbg=()):
    nc = bass.Bass("TRN2", target_bir_lowering=False)
    kb = KB(nc)
    c = setup(nc, kb)

    def dram_in(name, shape, dt=F32):
        return nc.dram_tensor(name, list(shape), dt, kind="ExternalInput").ap()

    def dram_tmp(name, shape, dt):
        kind = "ExternalOutput" if name in dbg else "Internal"
        return nc.dram_tensor(name, list(shape), dt, kind=kind).ap()

    xT = dram_in("xT", [D, T])
    sm_d = dram_in("sm_in", [DEPTH, 128, NSM])
    rw_d = dram_in("rw_in", [DEPTH, 128, NRW])
    mats_d = dram_in("c_mats", [8, 128, 128])
    rope_d = dram_in("c_rope", [4, 128, T])
    cf_d = dram_in("c_f32", [128, 80])
    kb.dma("pool", c.mb[:, :, :], mats_d[0:4].rearrange("m p n -> p m n"), writes=[c.const_res])
    kb.dma("sp", c.mf[:, :, :], mats_d[4:8].rearrange("m p n -> p m n"), pwrites=[c.const_res])
    kb.dma("sp", c.cf[:, :], cf_d, pwrites=[c.const_res])

    XS = dram_tmp("XS", [D, T], F32)
    HT = dram_tmp("HT", [FF, T], BF16)
    HT_res = rlist("HT", FF // 128)
    CB = dram_tmp("CB", [NIN, T], BF16)
    HN = dram_tmp("HN", [D, T], BF16)
    DTW = dram_tmp("DTW", [T, 24], F32)
    YT = dram_tmp("YT", [5120, T], BF16)
    MTd = dram_tmp("MTd", [D, T], BF16)
    QMd = dram_tmp("QMd", [512, T], BF16)
    GS = dram_tmp("GS", [4, D, T], BF16)
    memT = None

    cur = xT
    for l in range(DEPTH):
        if not any(p[1] == l for p in plan):
            continue
        kb.barrier()
        kb.dma("sp", c.sm[:, :], sm_d[l], writes=[c.sm_res])
        kb.dma("sp", c.rw[:, :], rw_d[l], writes=[c.rw_res])
        kb.barrier()
        if ("ffn1", l) in plan:
            w1 = dram_in("w_ffn1_in_%d" % l, [D, 2 * FF])
            w2 = dram_in("w_ffn1_out_%d" % l, [FF, D])
            ffn(c, cur, XS, SMO["g_ffn1"], w1, w2, HT, HT_res)
            cur = XS
            kb.barrier()
        if ("win", l) in plan:
            w_in = dram_in("w_in_%d" % l, [D, NIN])
            stage_win(c, cur, SMO["g_mix"], w_in, CB, HN, DTW)
            kb.barrier()
        if ("sg", l) in plan:
            sw = dram_in("sg_w_%d" % l, [4, 128, 128])
            stage_sg(c, CB, YT, sw)
            kb.barrier()
        gates = None
        if ("merge", l) in plan:
            wg = dram_in("w_gate_%d" % l, [4, D, D])
            gates = GateGen(c, HN, wg, GS)
        if ("ssd", l) in plan:
            if gates is not None:
                gates.banks = [7]
            stage_ssd(c, CB, DTW, YT, gates)
            kb.barrier()
        if gates is not None:
            gates.banks = [6, 7]
        if ("pool", l) in plan:
            pw = dram_in("pool_w_%d" % l, [4, 512, 512])
            stage_pool(c, CB, YT, pw, gates)
            kb.barrier()
        if ("dsa", l) in plan:
            stage_rope(c, CB, rope_d, gates)
            kb.barrier()
            stage_dsa(c, CB, DTW, YT, gates)
            kb.barrier()
        elif gates is not None:
            gates.drain()
            kb.barrier()
        if ("merge", l) in plan:
            wbr = dram_in("w_branch_%d" % l, [5120, D])
            wo = dram_in("w_out_%d" % l, [D, D])
            if cur is xT:
                for k in range(DC):
                    kb.dma("sp", XS[k * 128:(k + 1) * 128, :], xT[k * 128:(k + 1) * 128, :])
                cur = XS
                kb.barrier()
            stage_merge(c, YT, GS, wbr, MTd)
            kb.barrier()
            stage_wout(c, MTd, wo, XS)
            kb.barrier()
        if ("cross", l) in plan:
            if memT is None:
                memT = dram_in("memT", [D, MEM])
            wq = dram_in("w_mem_q_%d" % l, [D, 512])
            wkv = dram_in("w_mem_kv_%d" % l, [D, 1024])
            wmo = dram_in("w_mem_o_%d" % l, [512, D])
            stage_cross(c, XS, memT, wq, wkv, wmo, QMd)
            kb.barrier()
        if ("ffn2", l) in plan:
            w1 = dram_in("w_ffn2_in_%d" % l, [D, 2 * FF])
            w2 = dram_in("w_ffn2_out_%d" % l, [FF, D])
            ffn(c, XS, XS, SMO["g_ffn2"], w1, w2, HT, HT_res)
            kb.barrier()
    if "final" in [p[0] for p in plan]:
        kb.barrier()
        yT = nc.dram_tensor("yT", [D, T], F32, kind="ExternalOutput").ap()
        load_x_norm(c, cur, None, SMO["g_final"], 0, T, out_dram=yT)
    kb.barrier()
    print("instructions", kb.ninst, "waits", kb.nwaits)
    return nc


_NC_CACHE = {}


def kernel(**inputs):
    inp = {k: np.asarray(v) for k, v in inputs.items()}
    plan = {(s, l) for s in STAGES for l in range(DEPTH)} | {("final", DEPTH)}
    if "nc" not in _NC_CACHE:
        _NC_CACHE["nc"] = build(plan)
    nc = _NC_CACHE["nc"]
    sm, rw = host_tables(inp)
    shared = dict(sm_in=sm, rw_in=rw, **host_consts())
    per_layer = ["w_ffn1_in", "w_ffn1_out", "w_in", "pool_w", "sg_w", "w_gate", "w_branch", "w_out",
                 "w_mem_q", "w_mem_kv", "w_mem_o", "w_ffn2_in", "w_ffn2_out"]
    for l in range(DEPTH):
        for n in per_layer:
            shared["%s_%d" % (n, l)] = np.ascontiguousarray(inp[n][l], dtype=np.float32)
    B = inp["x"].shape[0]
    in_maps = []
    for b in range(B):
        m = dict(shared)
        m["xT"] = np.ascontiguousarray(inp["x"][b].T, dtype=np.float32)
        m["memT"] = np.ascontiguousarray(inp["mem"][b].T, dtype=np.float32)
        in_maps.append(m)
    res = run_bass_kernel_spmd(nc, in_maps, core_ids=list(range(B)))
    out = np.stack([np.ascontiguousarray(res.results[b]["yT"].T) for b in range(B)], axis=0)
    return out.astype(np.float32)
```
